# Optimizing a Trainium2 kernel written in Bass

```python
import math
import jax
import jax.numpy as jnp
from jax import lax
import numpy as np

D_MODEL = 1024
BATCH = 8
SEQ = 8192
DEPTH = 4

CHUNK = 64
Q_BLOCK = 128
HEAD_DIM = 64
N_MIX_HEADS = D_MODEL // HEAD_DIM
MLA_HEADS = N_MIX_HEADS // 4
RWKV_HEADS = (N_MIX_HEADS - MLA_HEADS) // 2
GDN_HEADS = N_MIX_HEADS - MLA_HEADS - RWKV_HEADS
RWKV_WIDTH = RWKV_HEADS * HEAD_DIM
MLA_WIDTH = MLA_HEADS * HEAD_DIM
GDN_WIDTH = GDN_HEADS * HEAD_DIM
MIX_WIDTH = RWKV_WIDTH + MLA_WIDTH + GDN_WIDTH

RWKV_DECAY_LORA = 64
RWKV_ICLR_LORA = 64
RWKV_GATE_LORA = 128
RWKV_GN_EPS = 64e-5

MLA_NOPE_DIM = 64
MLA_ROPE_DIM = 32
MLA_QK_DIM = MLA_NOPE_DIM + MLA_ROPE_DIM
MLA_V_DIM = HEAD_DIM
MLA_Q_LORA = 256
MLA_KV_LORA = 128
ROPE_BASE = 10000.0

GDN_CONV = 4

D_FF = 4 * D_MODEL
NORM_EPS = 1e-6

RWKV_SPLITS = (RWKV_WIDTH, RWKV_WIDTH, RWKV_WIDTH, RWKV_DECAY_LORA, RWKV_ICLR_LORA, RWKV_GATE_LORA)
MLA_SPLITS = (MLA_Q_LORA, MLA_KV_LORA, MLA_ROPE_DIM)
GDN_SPLITS = (GDN_WIDTH, GDN_WIDTH, GDN_WIDTH, GDN_WIDTH, GDN_HEADS, GDN_HEADS)
RWKV_IN = 3 * RWKV_WIDTH + RWKV_DECAY_LORA + RWKV_ICLR_LORA + RWKV_GATE_LORA
MLA_IN = MLA_Q_LORA + MLA_KV_LORA + MLA_ROPE_DIM
GDN_IN = 4 * GDN_WIDTH + 2 * GDN_HEADS
P_IN = RWKV_IN + MLA_IN + GDN_IN

kernel_name = 'hybrid_rwkv7_mla_gdn_block'


def _split(t, sizes):
    idx = np.cumsum(np.array(sizes))[:-1].tolist()
    return jnp.split(t, idx, axis=-1)


def _rms(x, g, eps=NORM_EPS):
    xf = x.astype(jnp.float32)
    y = xf * lax.rsqrt(jnp.mean(xf * xf, axis=-1, keepdims=True) + eps)
    return (y * g.astype(jnp.float32)).astype(x.dtype)


def _l2norm(x, eps=1e-6):
    xf = x.astype(jnp.float32)
    return xf * lax.rsqrt(jnp.sum(xf * xf, axis=-1, keepdims=True) + eps)


def _rope(t, cos, sin):
    t1, t2 = jnp.split(t, 2, axis=-1)
    return jnp.concatenate([t1 * cos - t2 * sin, t1 * sin + t2 * cos], axis=-1)


def rwkv7_time_mix(p, mu, w0, w2, a0, a2, g2, k_k, k_a, r_k, gn_g, gn_b):
    f32 = jnp.float32
    B, S, _ = p.shape
    p_prev = jnp.pad(p, ((0, 0), (1, 0), (0, 0)))[:, :-1]
    z = p + (p_prev - p) * mu
    r, k, v, zw, za, zg = _split(z, RWKV_SPLITS)
    w_logit = (w0 + jnp.tanh(zw) @ w2).astype(f32)
    decay = jnp.exp(-jnp.exp(-jax.nn.softplus(-w_logit) - 0.5))
    a = jax.nn.sigmoid((a0 + za @ a2).astype(f32))
    g = jax.nn.sigmoid(zg) @ g2
    hs = lambda t: t.reshape(B, S, RWKV_HEADS, HEAD_DIM).astype(f32)
    hp = lambda t: t.reshape(RWKV_HEADS, HEAD_DIM).astype(f32)
    r, k, v, a, decay = hs(r), hs(k), hs(v), hs(a), hs(decay)
    kk = _l2norm(k * hp(k_k))
    k = k * (1.0 + (a - 1.0) * hp(k_a))

    def step(state, inp):
        r_t, w_t, k_t, v_t, kk_t, b_t = inp
        sa = jnp.einsum('bhvk,bhk->bhv', state, -kk_t)
        state = (state * w_t[:, :, None, :] + sa[..., None] * b_t[:, :, None, :]
                 + v_t[..., None] * k_t[:, :, None, :])
        return state, jnp.einsum('bhvk,bhk->bhv', state, r_t)

    tm = lambda t: jnp.moveaxis(t, 1, 0)
    s0 = jnp.zeros((B, RWKV_HEADS, HEAD_DIM, HEAD_DIM), f32)
    _, y = lax.scan(step, s0, (tm(r), tm(decay), tm(k), tm(v), tm(kk), tm(kk * a)))
    y = jnp.moveaxis(y, 0, 1)
    mean = jnp.mean(y, axis=-1, keepdims=True)
    var = jnp.mean(jnp.square(y - mean), axis=-1, keepdims=True)
    y = ((y - mean) * lax.rsqrt(var + RWKV_GN_EPS)).reshape(B, S, RWKV_WIDTH)
    y = y * gn_g.astype(f32) + gn_b.astype(f32)
    bonus = jnp.sum(r * k * r_k.astype(f32), axis=-1, keepdims=True) * v
    y = y + bonus.reshape(B, S, RWKV_WIDTH)
    return (y * g.astype(f32)).astype(p.dtype)


def mla_attention(p, positions, q_norm_g, w_uq, kv_norm_g, w_ukv, q_qk_g, k_qk_g):
    f32 = jnp.float32
    B, S, _ = p.shape
    c_q, c_kv, k_rope = _split(p, MLA_SPLITS)
    q = (_rms(c_q, q_norm_g) @ w_uq).reshape(B, S, MLA_HEADS, MLA_QK_DIM)
    kv = (_rms(c_kv, kv_norm_g) @ w_ukv).reshape(B, S, MLA_HEADS, MLA_NOPE_DIM + MLA_V_DIM)
    k_nope, v = jnp.split(kv, [MLA_NOPE_DIM], axis=-1)
    k_rope = jnp.broadcast_to(k_rope[:, :, None, :], (B, S, MLA_HEADS, MLA_ROPE_DIM))
    k = jnp.concatenate([k_nope, k_rope], axis=-1)
    q = _rms(q, q_qk_g)
    k = _rms(k, k_qk_g)
    inv_freq = ROPE_BASE ** (-jnp.arange(0, MLA_ROPE_DIM, 2, dtype=f32) / MLA_ROPE_DIM)
    ang = positions.astype(f32)[..., None] * inv_freq
    cos = jnp.cos(ang)[:, :, None, :].astype(q.dtype)
    sin = jnp.sin(ang)[:, :, None, :].astype(q.dtype)
    q = jnp.concatenate([q[..., :MLA_NOPE_DIM], _rope(q[..., MLA_NOPE_DIM:], cos, sin)], axis=-1)
    k = jnp.concatenate([k[..., :MLA_NOPE_DIM], _rope(k[..., MLA_NOPE_DIM:], cos, sin)], axis=-1)
    scale = MLA_QK_DIM ** -0.5
    nb = S // Q_BLOCK
    qb = jnp.moveaxis(q.reshape(B, nb, Q_BLOCK, MLA_HEADS, MLA_QK_DIM), 1, 0)
    k_chunk = jnp.arange(S) // CHUNK

    def block(args):
        q_blk, i = args
        s = jnp.einsum('bqhd,bkhd->bhqk', q_blk, k).astype(f32) * scale
        q_chunk = (i * Q_BLOCK + jnp.arange(Q_BLOCK)) // CHUNK
        s = jnp.where(k_chunk[None, :] <= q_chunk[:, None], s, -jnp.inf)
        prob = jax.nn.softmax(s, axis=-1).astype(v.dtype)
        return jnp.einsum('bhqk,bkhd->bqhd', prob, v)

    o = lax.map(block, (qb, jnp.arange(nb)))
    return jnp.moveaxis(o, 0, 1).reshape(B, S, MLA_WIDTH)


def _chunk_gated_delta_rule(q, k, v, beta, g):
    B, S, H, Dk = q.shape
    Dv = v.shape[-1]
    NC, L = S // CHUNK, CHUNK
    ch = lambda t: jnp.moveaxis(t.reshape(B, NC, L, H, -1), 3, 1)
    q, k, v = ch(q), ch(k), ch(v)
    beta, g = ch(beta[..., None])[..., 0], ch(g[..., None])[..., 0]
    gc = jnp.cumsum(g, axis=-1)
    idx = jnp.arange(L)
    incl = idx[:, None] >= idx[None, :]
    strict = idx[:, None] > idx[None, :]
    decay = jnp.exp(jnp.where(incl, gc[..., :, None] - gc[..., None, :], -jnp.inf))
    A = jnp.where(strict, jnp.einsum('bhnld,bhnmd->bhnlm', k, k) * decay * beta[..., :, None], 0.0)
    rhs = jnp.concatenate([v * beta[..., None], k * (beta * jnp.exp(gc))[..., None]], axis=-1)
    sol = lax.linalg.triangular_solve(A + jnp.eye(L, dtype=A.dtype), rhs,
                                      left_side=True, lower=True, unit_diagonal=True)
    u, w = sol[..., :Dv], sol[..., Dv:]
    attn = jnp.einsum('bhnld,bhnmd->bhnlm', q, k) * decay
    q_dec = q * jnp.exp(gc)[..., None]
    k_dec = k * jnp.exp(gc[..., -1:] - gc)[..., None]
    chunk_decay = jnp.exp(gc[..., -1])

    def step(state, inp):
        u_c, w_c, attn_c, q_c, k_c, d_c = inp
        delta = u_c - jnp.einsum('bhlk,bhkv->bhlv', w_c, state)
        o_c = jnp.einsum('bhlk,bhkv->bhlv', q_c, state) + jnp.einsum('bhlm,bhmv->bhlv', attn_c, delta)
        state = d_c[..., None, None] * state + jnp.einsum('bhlk,bhlv->bhkv', k_c, delta)
        return state, o_c

    cm = lambda t: jnp.moveaxis(t, 2, 0)
    s0 = jnp.zeros((B, H, Dk, Dv), jnp.float32)
    _, o = lax.scan(step, s0, (cm(u), cm(w), cm(attn), cm(q_dec), cm(k_dec), cm(chunk_decay)))
    return jnp.transpose(o, (1, 0, 3, 2, 4)).reshape(B, S, H, Dv)


def gated_deltanet(p, conv_w, a_log, dt_bias, o_norm_g):
    f32 = jnp.float32
    B, S, _ = p.shape
    q, k, v, gate, b, a = _split(p, GDN_SPLITS)
    qkv = jnp.concatenate([q, k, v], axis=-1)
    qkv = lax.conv_general_dilated(qkv, conv_w[:, None, :].astype(qkv.dtype), window_strides=(1,),
                                   padding=[(GDN_CONV - 1, 0)], dimension_numbers=('NWC', 'WIO', 'NWC'),
                                   feature_group_count=3 * GDN_WIDTH)
    q, k, v = jnp.split(jax.nn.silu(qkv), 3, axis=-1)
    hd = lambda t: t.reshape(B, S, GDN_HEADS, HEAD_DIM)
    q = _l2norm(hd(q)) * (HEAD_DIM ** -0.5)
    k = _l2norm(hd(k))
    v = hd(v).astype(f32)
    beta = jax.nn.sigmoid(b.astype(f32))
    g = -jnp.exp(a_log.astype(f32)) * jax.nn.softplus(a.astype(f32) + dt_bias.astype(f32))
    o = _chunk_gated_delta_rule(q, k, v, beta, g)
    o = _rms(o, o_norm_g) * jax.nn.silu(hd(gate).astype(f32))
    return o.reshape(B, S, GDN_WIDTH).astype(p.dtype)


def setup_inputs(seed: int = 0) -> dict:
    key = jax.random.key(seed)
    keys = list(jax.random.split(key, 40))
    f32 = jnp.float32
    L = DEPTH

    def nrm(shape, scale):
        return jax.random.normal(keys.pop(), shape, f32) * scale

    def gain(shape):
        return 1.0 + 0.05 * jax.random.normal(keys.pop(), shape, f32)

    inp = {}
    inp['x'] = nrm((BATCH, SEQ, D_MODEL), 1.0)
    inp['positions'] = jnp.tile(jnp.arange(SEQ, dtype=jnp.int32)[None, :], (BATCH, 1))
    inp['ln_mix_g'] = gain((L, D_MODEL))
    inp['w_in'] = nrm((L, D_MODEL, P_IN), D_MODEL ** -0.5)
    inp['w_out'] = nrm((L, MIX_WIDTH, D_MODEL), MIX_WIDTH ** -0.5)
    inp['ln_ffn_g'] = gain((L, D_MODEL))
    inp['w_ff1'] = nrm((L, D_MODEL, D_FF), D_MODEL ** -0.5)
    inp['w_ff2'] = nrm((L, D_FF, D_MODEL), D_FF ** -0.5)
    inp['rwkv_mu'] = jax.random.uniform(keys.pop(), (L, RWKV_IN), f32, 0.0, 1.0)
    inp['rwkv_w0'] = jax.random.uniform(keys.pop(), (L, RWKV_WIDTH), f32, -6.0, 1.0)
    inp['rwkv_w2'] = nrm((L, RWKV_DECAY_LORA, RWKV_WIDTH), 0.5 * RWKV_DECAY_LORA ** -0.5)
    inp['rwkv_a0'] = nrm((L, RWKV_WIDTH), 0.1)
    inp['rwkv_a2'] = nrm((L, RWKV_ICLR_LORA, RWKV_WIDTH), 0.5 * RWKV_ICLR_LORA ** -0.5)
    inp['rwkv_g2'] = nrm((L, RWKV_GATE_LORA, RWKV_WIDTH), RWKV_GATE_LORA ** -0.5)
    inp['rwkv_k_k'] = 0.85 + nrm((L, RWKV_WIDTH), 0.05)
    inp['rwkv_k_a'] = 1.0 + nrm((L, RWKV_WIDTH), 0.05)
    inp['rwkv_r_k'] = nrm((L, RWKV_HEADS, HEAD_DIM), 0.1)
    inp['rwkv_gn_g'] = gain((L, RWKV_WIDTH))
    inp['rwkv_gn_b'] = nrm((L, RWKV_WIDTH), 0.02)
    inp['mla_q_norm_g'] = gain((L, MLA_Q_LORA))
    inp['mla_w_uq'] = nrm((L, MLA_Q_LORA, MLA_HEADS * MLA_QK_DIM), MLA_Q_LORA ** -0.5)
    inp['mla_kv_norm_g'] = gain((L, MLA_KV_LORA))
    inp['mla_w_ukv'] = nrm((L, MLA_KV_LORA, MLA_HEADS * (MLA_NOPE_DIM + MLA_V_DIM)), MLA_KV_LORA ** -0.5)
    inp['mla_q_qk_g'] = gain((L, MLA_QK_DIM))
    inp['mla_k_qk_g'] = gain((L, MLA_QK_DIM))
    inp['gdn_conv_w'] = nrm((L, GDN_CONV, 3 * GDN_WIDTH), GDN_CONV ** -0.5)
    inp['gdn_a_log'] = jnp.log(jax.random.uniform(keys.pop(), (L, GDN_HEADS), f32, 1.0, 16.0))
    dt = jnp.exp(jax.random.uniform(keys.pop(), (L, GDN_HEADS), f32, math.log(1e-3), math.log(1e-1)))
    inp['gdn_dt_bias'] = dt + jnp.log(-jnp.expm1(-dt))
    inp['gdn_o_norm_g'] = gain((L, HEAD_DIM))
    return inp


def reference(x, positions, ln_mix_g, w_in, w_out, ln_ffn_g, w_ff1, w_ff2,
              rwkv_mu, rwkv_w0, rwkv_w2, rwkv_a0, rwkv_a2, rwkv_g2, rwkv_k_k, rwkv_k_a,
              rwkv_r_k, rwkv_gn_g, rwkv_gn_b,
              mla_q_norm_g, mla_w_uq, mla_kv_norm_g, mla_w_ukv, mla_q_qk_g, mla_k_qk_g,
              gdn_conv_w, gdn_a_log, gdn_dt_bias, gdn_o_norm_g):
    h = x
    for l in range(DEPTH):
        u = _rms(h, ln_mix_g[l])
        p = u @ w_in[l]
        p_rwkv, p_mla, p_gdn = _split(p, (RWKV_IN, MLA_IN, GDN_IN))
        y_rwkv = rwkv7_time_mix(p_rwkv, rwkv_mu[l], rwkv_w0[l], rwkv_w2[l], rwkv_a0[l], rwkv_a2[l],
                                rwkv_g2[l], rwkv_k_k[l], rwkv_k_a[l], rwkv_r_k[l],
                                rwkv_gn_g[l], rwkv_gn_b[l])
        y_mla = mla_attention(p_mla, positions, mla_q_norm_g[l], mla_w_uq[l], mla_kv_norm_g[l],
                              mla_w_ukv[l], mla_q_qk_g[l], mla_k_qk_g[l])
        y_gdn = gated_deltanet(p_gdn, gdn_conv_w[l], gdn_a_log[l], gdn_dt_bias[l], gdn_o_norm_g[l])
        y = jnp.concatenate([y_rwkv, y_mla, y_gdn], axis=-1)
        h = h + y @ w_out[l]
        u = _rms(h, ln_ffn_g[l])
        h = h + jnp.square(jax.nn.relu(u @ w_ff1[l])) @ w_ff2[l]
    return h
```

```python
import contextlib
import numpy as np
import concourse.bass as bass
import concourse.mybir as mybir
from concourse.bass_utils import run_bass_kernel_spmd

F32 = mybir.dt.float32
BF16 = mybir.dt.bfloat16
I32 = mybir.dt.int32
AF = mybir.ActivationFunctionType
ALU = mybir.AluOpType
AX = mybir.AxisListType

D = 1024
DFF = 4096
P_IN = 3372
P_EXT = P_IN + 32
NORM_EPS = 1e-6
NCST = 128 * 8 + 768 * 2

ENGS = ("pe", "act", "dve", "pool", "sp")
CENG = ("pe", "act", "dve", "pool")
BLK = 8192
NROT = 8
NDMA = 12


class Op:
    __slots__ = ("eng", "fn", "reads", "writes", "idx", "deps", "signal", "dma",
                 "k", "dslot", "dval")

    def __init__(self, eng, fn, reads, writes, dma):
        self.eng, self.fn, self.reads, self.writes, self.dma = eng, fn, reads, writes, dma
        self.deps = ()
        self.signal = False
        self.k = -1


class Sched:
    def __init__(self, nc):
        self.nc = nc
        self.ops = []
        self.last_w = {}
        self.readers = {}
        self.nsig = {e: 0 for e in ENGS}
        self.ndma = {e: 0 for e in ENGS}
        self.waited_c = {e: {} for e in ENGS}
        self.waited_d = {e: {} for e in ENGS}
        self.nbar = 0
        self.ntot = {e: 0 for e in ENGS}
        self.csem = {e: [nc.semaphore(f"c_{e}_{i}").__enter__() for i in range(NROT)] for e in CENG}
        self.dsem = {e: [nc.semaphore(f"d_{e}_{i}").__enter__() for i in range(NDMA)]
                     for e in ("sp", "pool")}
        self.bsem = nc.semaphore("bar").__enter__()

    def add(self, eng, fn, reads=(), writes=(), dma=False):
        op = Op(eng, fn, tuple(reads), tuple(writes), dma)
        op.idx = len(self.ops)
        deps = set()
        lw, rd = self.last_w, self.readers
        for r in op.reads:
            p = lw.get(r)
            if p is not None:
                deps.add(p)
        for w in op.writes:
            p = lw.get(w)
            if p is not None:
                deps.add(p)
            for q in rd.get(w, ()):
                deps.add(q)
        ops = self.ops
        keep = []
        for d in deps:
            p = ops[d]
            if p.eng == eng and not p.dma and not dma:
                if eng == "pe":
                    continue
            keep.append(d)
        op.deps = tuple(sorted(keep))
        for d in op.deps:
            ops[d].signal = True
        for r in op.reads:
            rd.setdefault(r, []).append(op.idx)
        for w in op.writes:
            lw[w] = op.idx
            rd[w] = []
        ops.append(op)
        return op

    def pe(self, fn, reads=(), writes=()):
        return self.add("pe", fn, reads, writes)

    def act(self, fn, reads=(), writes=()):
        return self.add("act", fn, reads, writes)

    def dve(self, fn, reads=(), writes=()):
        return self.add("dve", fn, reads, writes)

    def pool(self, fn, reads=(), writes=()):
        return self.add("pool", fn, reads, writes)

    def dma(self, out, in_, reads=(), writes=(), q="sp", **kw):
        op = self.add(q, lambda e: e.dma_start(out=out, in_=in_, **kw), reads, writes, dma=True)
        op.signal = True
        return op

    def _csig(self, op):
        j = op.k // BLK
        return self.csem[op.eng][j % NROT], (j // NROT) * BLK + (op.k % BLK) + 1

    def flush(self):
        nc = self.nc
        ops = self.ops
        per = {e: [] for e in ENGS}
        for op in ops:
            per[op.eng].append(op)
        for e in CENG:
            for op in reversed(per[e]):
                if not op.dma:
                    op.signal = True
                    break
        for op in ops:
            if op.dma:
                n = self.ndma[op.eng]
                op.dslot = n % NDMA
                op.dval = 16 * (n // NDMA + 1)
                self.ndma[op.eng] = n + 1
            elif op.signal:
                op.k = self.nsig[op.eng]
                self.nsig[op.eng] += 1
        for e in ENGS:
            self.ntot[e] += len(per[e])
        self.nbar += 1
        nbar = self.nbar
        dsem, bsem = self.dsem, self.bsem

        def run(engname, e):
            waited_c = self.waited_c[engname]
            waited_d = self.waited_d[engname]

            def wait_for(p):
                if p.dma:
                    key = (p.eng, p.dslot)
                    if waited_d.get(key, 0) >= p.dval:
                        return
                    waited_d[key] = p.dval
                    e.wait_ge(dsem[p.eng][p.dslot], p.dval)
                else:
                    if waited_c.get(p.eng, -1) >= p.k:
                        return
                    waited_c[p.eng] = p.k
                    s, v = self._csig(p)
                    e.wait_ge(s, v)

            last_c = None
            for op in per[engname]:
                for d in op.deps:
                    wait_for(ops[d])
                if op.dma:
                    if op.dval > 16:
                        key = (op.eng, op.dslot)
                        if waited_d.get(key, 0) < op.dval - 16:
                            waited_d[key] = op.dval - 16
                            e.wait_ge(dsem[op.eng][op.dslot], op.dval - 16)
                    op.fn(e).then_inc(dsem[op.eng][op.dslot], 16)
                else:
                    ins = op.fn(e)
                    if op.signal:
                        s, v = self._csig(op)
                        ins.then_inc(s, 1)
                        last_c = op
            if last_c is not None:
                wait_for(last_c)
            lastd = {}
            for op in per[engname]:
                if op.dma:
                    lastd[op.dslot] = op
            for op in lastd.values():
                wait_for(op)
            e.sem_inc(bsem, 1)
            e.wait_ge(bsem, 5 * nbar)

        with nc.Block() as block:
            block.tensor(lambda e: run("pe", e))
            block.scalar(lambda e: run("act", e))
            block.vector(lambda e: run("dve", e))
            block.gpsimd(lambda e: run("pool", e))
            block.sync(lambda e: run("sp", e))
        self.ops = []
        self.last_w = {}
        self.readers = {}


IN_CHUNKS = ([(i * 128, 128) for i in range(14)] + [(1792, 32)] +
             [(1824 + i * 128, 128) for i in range(12)] + [(3360, 12), (3372, 32)])


class Prog:
    def __init__(self, S, L, dbg=(), mix=("gdn", "rwkv", "mla")):
        self.S, self.L, self.dbg = S, L, set(dbg)
        self.mix = set(mix)
        self.stage = 9
        self.sub = 9
        nc = self.nc = bass.Bass("TRN2", target_bir_lowering=False)
        self.sc = Sched(nc)
        dt = nc.dram_tensor
        self.xT = dt("xT", [D, S], F32, kind="ExternalInput").ap()
        self.w_in = dt("w_in", [L, D, P_IN], F32, kind="ExternalInput").ap()
        self.w_out = dt("w_out", [L, D, D], F32, kind="ExternalInput").ap()
        self.w_ff1 = dt("w_ff1", [L, D, DFF], F32, kind="ExternalInput").ap()
        self.w_ff2 = dt("w_ff2", [L, DFF, D], F32, kind="ExternalInput").ap()
        self.g_mix = dt("g_mix", [128, L * 8], F32, kind="ExternalInput").ap()
        self.g_ffn = dt("g_ffn", [128, L * 8], F32, kind="ExternalInput").ap()
        self.oT = dt("oT", [D, S], F32, kind="ExternalOutput").ap()
        self.cst = dt("cst", [128, NCST], F32, kind="ExternalInput").ap()
        self.cw_d = dt("cw", [128, L * 36], F32, kind="ExternalInput").ap()
        self.gsm_d = dt("gsm", [128, L * 13], F32, kind="ExternalInput").ap()
        self.baT = dt("baT", [S, 12], F32).ap()
        self.rws_d = dt("rws", [128, L * 32], F32, kind="ExternalInput").ap()
        self.rww = dt("rww", [128, L * 1152], F32, kind="ExternalInput").ap()
        self.segm_d = dt("segm", [128, 512], F32, kind="ExternalInput").ap()
        self.w_uq = dt("w_uq", [L, 256, 384], F32, kind="ExternalInput").ap()
        self.w_ukv = dt("w_ukv", [L, 128, 512], F32, kind="ExternalInput").ap()
        self.mls_d = dt("mls", [128, L * 8], F32, kind="ExternalInput").ap()
        self.mlc_d = dt("mlc", [128, 104], F32, kind="ExternalInput").ap()
        self.pos96 = dt("pos96", [96, S], I32, kind="ExternalInput").ap()
        self.mmask = dt("mmask", [128, 2048], F32, kind="ExternalInput").ap()
        self.cosT = dt("cosT", [96, S], F32).ap()
        self.sinT = dt("sinT", [96, S], F32).ap()
        self.qfT = dt("qfT", [4, 96, S], BF16).ap()
        self.kfT = dt("kfT", [4, 96, S], BF16).ap()
        self.vxT = dt("vxT", [4, S, 128], BF16).ap()
        self.pT = dt("pT", [P_EXT, S], F32).ap()
        self.yT = dt("yT", [D, S], BF16).ap()
        self.hT = [dt(f"hT{i}", [D, S], F32).ap() for i in range(2)]
        self.dbg_out = {}
        if "pT" in self.dbg:
            self.dbg_out["pT"] = dt("dbg_pT", [P_EXT, S], F32, kind="ExternalOutput").ap()
        if "yT" in self.dbg:
            self.dbg_out["yT"] = dt("dbg_yT", [D, S], BF16, kind="ExternalOutput").ap()
        self.ps = [nc.alloc_psum_tensor(f"ps{i}", [128, 512], F32) for i in range(8)]
        self.ps_i = 0
        self.ps_n = 8

    def psum(self):
        i = self.ps_i % self.ps_n
        self.ps_i += 1
        return self.ps[i], ("ps", i)

    def consts(self):
        nc, sc = self.nc, self.sc
        L = self.L
        self.ones_bf = nc.alloc_sbuf_tensor("ones_bf", [128, 128], BF16)
        self.gm = nc.alloc_sbuf_tensor("gm", [128, L * 8], F32)
        self.gf = nc.alloc_sbuf_tensor("gf", [128, L * 8], F32)
        sc.pool(lambda e: e.memset(self.ones_bf[:], 1.0), [], ["ones_bf"])
        self.C = nc.alloc_sbuf_tensor("cstt", [128, NCST], F32)
        self.cw = nc.alloc_sbuf_tensor("cwt", [128, L * 36], F32)
        self.gsm = nc.alloc_sbuf_tensor("gsmt", [128, L * 13], F32)
        self.onesbd_bf = nc.alloc_sbuf_tensor("onesbd_bf", [128, 128], BF16)
        self.rws = nc.alloc_sbuf_tensor("rwst", [128, L * 32], F32)
        self.segm = nc.alloc_sbuf_tensor("segmt", [128, 512], F32)
        sc.dma(self.rws[:], self.rws_d, writes=["rws"])
        sc.dma(self.segm[:], self.segm_d, writes=["segm"])
        self.mls = nc.alloc_sbuf_tensor("mlst", [128, L * 8], F32)
        self.mlc = nc.alloc_sbuf_tensor("mlct", [128, 104], F32)
        sc.dma(self.mls[:], self.mls_d, writes=["mls"])
        sc.dma(self.mlc[:], self.mlc_d, writes=["mlc"])
        sc.dma(self.C[:], self.cst, writes=["C"])
        sc.dma(self.cw[:], self.cw_d, writes=["cw"])
        sc.dma(self.gsm[:], self.gsm_d, writes=["gsm"])
        sc.dve(lambda e: e.tensor_copy(out=self.onesbd_bf[:], in_=self.C[:, 256:384]), ["C"], ["onesbd_bf"])
        sc.dma(self.gm[:], self.g_mix, writes=["gm"])
        sc.dma(self.gf[:], self.g_ffn, writes=["gf"])
        sc.flush()

    def rms_stats(self, hx, hx_tok, sq, sq_tok, rs, rs_tok, T):
        sc = self.sc
        sc.act(lambda e: e.activation(out=sq[:, :, 0:T], in_=hx[:, :, 0:T], func=AF.Square), [hx_tok], [sq_tok])
        p, ptok = self.psum()
        for kc in range(8):
            sc.pe(lambda e, kc=kc: e.matmul(p[:, 0:T], lhsT=self.ones_bf[:], rhs=sq[:, kc, 0:T],
                                              start=(kc == 0), stop=(kc == 7)), [sq_tok, "ones_bf"], [ptok])
        sc.act(lambda e: e.activation(out=rs[:, 0:T], in_=p[:, 0:T], func=AF.Sqrt, bias=NORM_EPS, scale=1.0 / D),
               [ptok], [rs_tok])
        sc.dve(lambda e: e.reciprocal(out=rs[:, 0:T], in_=rs[:, 0:T]), [rs_tok], [rs_tok])

    def load_weight(self, dst, dst_name, src, KC, cols, gcol, gtok, stg, col_off=0):
        sc = self.sc
        n = 0
        CH = stg[0][0].shape[1]
        for kc in range(KC):
            for c0 in range(0, cols, CH):
                c1 = min(cols, c0 + CH)
                st, sttok = stg[n % len(stg)]
                n += 1
                sc.dma(st[:, 0:c1 - c0], src[kc * 128:(kc + 1) * 128, c0:c1], writes=[sttok])
                eng = sc.dve if n % 2 == 0 else sc.pool
                if gcol is not None:
                    eng(lambda e, st=st, kc=kc, c0=c0, c1=c1: e.tensor_scalar(
                        out=dst[:, kc, col_off + c0:col_off + c1], in0=st[:, 0:c1 - c0],
                        scalar1=gcol[:, kc:kc + 1], scalar2=None, op0=ALU.mult),
                        [sttok, gtok], [(dst_name, kc)])
                else:
                    eng(lambda e, st=st, kc=kc, c0=c0, c1=c1: e.tensor_copy(
                        out=dst[:, kc, col_off + c0:col_off + c1], in_=st[:, 0:c1 - c0]),
                        [sttok], [(dst_name, kc)])

    def phase_a(self, l, h_src):
        nc, sc, S = self.nc, self.sc, self.S
        with contextlib.ExitStack() as st:
            sb = lambda n, s, d=F32: st.enter_context(nc.sbuf_tensor(f"{n}_L{l}", s, d))
            wi = sb("wi", [128, 8, P_EXT], BF16)
            stg = [(sb(f"stgA{i}", [128, 2048]), f"stgA{i}") for i in range(2)]
            hx = [sb(f"hxA{i}", [128, 8, 512]) for i in range(2)]
            sq = [sb(f"sqA{i}", [128, 8, 512], BF16) for i in range(2)]
            xb = [sb(f"xbA{i}", [128, 8, 512], BF16) for i in range(2)]
            rs = [sb(f"rsA{i}", [128, 512]) for i in range(2)]
            ev = [sb(f"evA{i}", [128, 512]) for i in range(4)]
            bat = [sb(f"batA{i}", [128, 64]) for i in range(2)]
            gcol = self.gm[:, l * 8:(l + 1) * 8]
            self.load_weight(wi, "wi", self.w_in[l], 8, P_IN, gcol, "gm", stg)
            allwi = [("wi", kc) for kc in range(8)]
            sc.dve(lambda e: e.tensor_scalar(out=wi[:, :, 3372:3388], in0=wi[:, :, 1808:1824], scalar1=-1.0,
                                              scalar2=None, op0=ALU.mult), allwi, allwi)
            sc.dve(lambda e: e.tensor_copy(out=wi[:, :, 3388:3404], in_=wi[:, :, 1792:1808]), allwi, allwi)
            nev = 0
            for t in range(S // 512):
                b = t % 2
                t0 = t * 512
                sc.dma(hx[b][:], h_src[:, t0:t0 + 512].rearrange("(k p) s -> p k s", p=128), writes=[f"hxA{b}"])
                self.rms_stats(hx[b], f"hxA{b}", sq[b], f"sqA{b}", rs[b], f"rsA{b}", 512)
                for kc in range(8):
                    eng = sc.dve if kc % 2 == 0 else sc.pool
                    eng(lambda e, kc=kc, b=b: e.tensor_tensor(out=xb[b][:, kc, :], in0=hx[b][:, kc, :], in1=rs[b][:],
                                                               op=ALU.mult), [f"hxA{b}", f"rsA{b}"], [(f"xbA{b}", kc)])
                p, ptok = self.psum()
                for n in range(4):
                    for kc in range(8):
                        sc.pe(lambda e, p=p, kc=kc, n=n, b=b: e.matmul(
                            p[:, n * 16:n * 16 + 12], lhsT=xb[b][:, kc, n * 128:(n + 1) * 128], rhs=wi[:, kc, 3360:3372],
                            start=(kc == 0), stop=(kc == 7)), [("wi", kc), (f"xbA{b}", kc)], [ptok])
                sc.dve(lambda e, p=p, b=b: e.tensor_copy(out=bat[b][:].rearrange("p (n c) -> p n c", c=16)[:, :, 0:12], in_=p[:, 0:64].rearrange("p (n c) -> p n c", c=16)[:, :, 0:12]), [ptok], [f"batA{b}"])
                sc.dma(self.baT[t0:t0 + 512, :].rearrange("(n p) c -> p n c", p=128),
                       bat[b][:].rearrange("p (n c) -> p n c", c=16)[:, :, 0:12], reads=[f"batA{b}"], writes=[("baT", t)], q="pool")
                for (c0, m) in IN_CHUNKS:
                    p, ptok = self.psum()
                    for kc in range(8):
                        sc.pe(lambda e, p=p, kc=kc, c0=c0, m=m, b=b: e.matmul(
                            p[0:m, :], lhsT=wi[:, kc, c0:c0 + m], rhs=xb[b][:, kc, :], start=(kc == 0), stop=(kc == 7)),
                            [("wi", kc), (f"xbA{b}", kc)], [ptok])
                    e_i = nev % 4
                    evt = ev[e_i]
                    if nev % 2 == 0:
                        sc.dve(lambda e, p=p, m=m, evt=evt: e.tensor_copy(out=evt[0:m, :], in_=p[0:m, :]), [ptok], [f"evA{e_i}"])
                    else:
                        sc.act(lambda e, p=p, m=m, evt=evt: e.copy(out=evt[0:m, :], in_=p[0:m, :]), [ptok], [f"evA{e_i}"])
                    nev += 1
                    sc.dma(self.pT[c0:c0 + m, t0:t0 + 512], evt[0:m, :], reads=[f"evA{e_i}"], writes=[("pT", c0, t)], q="pool")
            sc.flush()

    def phase_c(self, l, h_src, h_dst):
        nc, sc, S = self.nc, self.sc, self.S
        T = 256
        with contextlib.ExitStack() as st:
            sb = lambda n, s, d=F32: st.enter_context(nc.sbuf_tensor(f"{n}_L{l}", s, d))
            wo = sb("wo", [128, 8, D], BF16)
            w1 = sb("w1", [128, 8, DFF], BF16)
            w2 = sb("w2", [128, 32, D], BF16)
            hid = sb("hid", [128, 32, T], BF16)
            hx = sb("hxC", [128, 8, T])
            sq = sb("sqC", [128, 8, T], BF16)
            xb = sb("xbC", [128, 8, T], BF16)
            yb = sb("ybC", [128, 8, T], BF16)
            rs = sb("rsC", [128, T])
            relu_t = [sb(f"reluC{i}", [128, T]) for i in range(2)]
            stg = [(sb(f"stgC{i}", [128, 1024]), f"stgC{i}") for i in range(2)]
            self.load_weight(wo, "wo", self.w_out[l], 8, D, None, None, stg)
            self.load_weight(w1, "w1", self.w_ff1[l], 8, DFF, self.gf[:, l * 8:(l + 1) * 8], "gf", stg)
            self.load_weight(w2, "w2", self.w_ff2[l], 32, D, None, None, stg)
            for t in range(S // T):
                t0 = t * T
                sc.dma(hx[:], h_src[:, t0:t0 + T].rearrange("(k p) s -> p k s", p=128), writes=["hxC"])
                sc.dma(yb[:], self.yT[:, t0:t0 + T].rearrange("(k p) s -> p k s", p=128), writes=["ybC"], q="pool")
                for oc in range(8):
                    p, ptok = self.psum()
                    for kc in range(8):
                        sc.pe(lambda e, p=p, kc=kc, oc=oc: e.matmul(
                            p[:, 0:T], lhsT=wo[:, kc, oc * 128:(oc + 1) * 128], rhs=yb[:, kc, :], start=(kc == 0), stop=(kc == 7)),
                            [("wo", kc), "ybC"], [ptok])
                    sc.dve(lambda e, p=p, oc=oc: e.tensor_tensor(out=hx[:, oc, :], in0=hx[:, oc, :], in1=p[:, 0:T], op=ALU.add),
                           [ptok, ("hxC", oc), "hxC"], [("hxC", oc)])
                hx_all = ["hxC"] + [("hxC", oc) for oc in range(8)]
                sc.act(lambda e: e.activation(out=sq[:], in_=hx[:], func=AF.Square), hx_all, ["sqC"])
                p, ptok = self.psum()
                for kc in range(8):
                    sc.pe(lambda e, p=p, kc=kc: e.matmul(p[:, 0:T], lhsT=self.ones_bf[:], rhs=sq[:, kc, :],
                                                          start=(kc == 0), stop=(kc == 7)), ["sqC", "ones_bf"], [ptok])
                sc.act(lambda e, p=p: e.activation(out=rs[:], in_=p[:, 0:T], func=AF.Sqrt, bias=NORM_EPS, scale=1.0 / D),
                       [ptok], ["rsC"])
                sc.dve(lambda e: e.reciprocal(out=rs[:], in_=rs[:]), ["rsC"], ["rsC"])
                for kc in range(8):
                    eng = sc.dve if kc % 2 == 0 else sc.pool
                    eng(lambda e, kc=kc: e.tensor_tensor(out=xb[:, kc, :], in0=hx[:, kc, :], in1=rs[:], op=ALU.mult),
                        hx_all + ["rsC"], [("xbC", kc)])
                for oc in range(32):
                    p, ptok = self.psum()
                    for kc in range(8):
                        sc.pe(lambda e, p=p, kc=kc, oc=oc: e.matmul(
                            p[:, 0:T], lhsT=w1[:, kc, oc * 128:(oc + 1) * 128], rhs=xb[:, kc, :], start=(kc == 0), stop=(kc == 7)),
                            [("w1", kc), ("xbC", kc)], [ptok])
                    rl, rltok = relu_t[oc % 2], f"reluC{oc % 2}"
                    sc.act(lambda e, p=p, rl=rl: e.activation(out=rl[:], in_=p[:, 0:T], func=AF.Relu), [ptok], [rltok])
                    sc.pool(lambda e, oc=oc, rl=rl: e.tensor_tensor(out=hid[:, oc, :], in0=rl[:], in1=rl[:], op=ALU.mult),
                            [rltok], [("hid", oc)])
                for oc in range(8):
                    p, ptok = self.psum()
                    for kc in range(32):
                        sc.pe(lambda e, p=p, kc=kc, oc=oc: e.matmul(
                            p[:, 0:T], lhsT=w2[:, kc, oc * 128:(oc + 1) * 128], rhs=hid[:, kc, :], start=(kc == 0), stop=(kc == 31)),
                            [("w2", kc), ("hid", kc)], [ptok])
                    sc.dve(lambda e, p=p, oc=oc: e.tensor_tensor(out=hx[:, oc, :], in0=hx[:, oc, :], in1=p[:, 0:T], op=ALU.add),
                           [ptok, ("hxC", oc), "hxC"], [("hxC", oc)])
                sc.dma(h_dst[:, t0:t0 + T].rearrange("(k p) s -> p k s", p=128), hx[:], reads=hx_all, writes=[("hdst", t)], q="pool")
            sc.flush()

    def phase_gdn(self, l):
        nc, sc, S = self.nc, self.sc, self.S
        C = self.C
        ident, ones, onesbd = C[:, 0:128], C[:, 128:256], C[:, 256:384]
        mneg, useg = C[:, 512:640], C[:, 896:1024]
        offd6, ident6 = C[:, 1024:1792], C[:, 1792:2560]
        self.ps_n = 5
        psO = [(self.ps[5 + i], f"psO{i}") for i in range(3)]
        cwl = self.cw[:, l * 36:(l + 1) * 36]
        gs = self.gsm[:, l * 13:(l + 1) * 13]
        with contextlib.ExitStack() as st:
            sb = lambda n, s, d=F32: st.enter_context(nc.sbuf_tensor(f"{n}_L{l}", s, d))
            xin = [sb(f"g_xin{i}", [128, 515]) for i in range(2)]
            acc = [sb(f"g_acc{i}", [128, 512]) for i in range(2)]
            qf32 = [sb(f"g_qf{i}", [128, 512]) for i in range(3)]
            kf32 = [sb(f"g_kf{i}", [128, 512]) for i in range(3)]
            vf32 = [sb(f"g_vf{i}", [128, 512]) for i in range(3)]
            qfb = [sb(f"g_qb{i}", [128, 512], BF16) for i in range(3)]
            kfb = [sb(f"g_kb{i}", [128, 512], BF16) for i in range(3)]
            sqt = sb("g_sq", [128, 512], BF16)
            rinv = sb("g_rinv", [128, 512])
            batm = sb("g_batm", [128, 4, 12])
            beta = sb("g_beta", [128, 4, 6]); nbeta = sb("g_nbeta", [128, 4, 6]); gtm = sb("g_gtm", [128, 4, 6])
            tmp46 = sb("g_tmp46", [128, 4, 6])
            nA = sb("g_nA", [128, 6])
            H32 = sb("g_H32", [128, 3 * 128]); Hbd = sb("g_Hbd", [128, 3 * 128], BF16)
            kz = [[sb(f"g_kz{i}_{hp}", [128, 512], BF16) for hp in range(2)] for i in range(3)]
            osb = sb("g_osb", [128, 512]); gate = sb("g_gate", [128, 512]); ybf = sb("g_ybf", [128, 512], BF16)
            B = []
            for pb in range(2):
                d = {}
                for nm, shp, dtp in [("gcc", [128, 6], F32), ("gct", [128, 6], F32), ("egc", [128, 6], F32), ("eend", [128, 6], F32),
                                     ("GU", [128, 768], F32), ("Egc", [128, 768], F32), ("dd", [128, 768], F32), ("DmT", [128, 768], F32),
                                     ("DmS", [128, 768], F32), ("Ktm", [128, 384], F32), ("Vtm", [128, 384], BF16),
                                     ("Kez", [128, 768], BF16), ("Bhc0", [128, 384], BF16), ("Bhc1", [128, 384], BF16),
                                     ("eendc0", [128, 6], F32), ("eendc1", [128, 6], F32), ("Mm", [128, 768], F32), ("Mt", [128, 768], F32),
                                     ("R", [128, 768], F32), ("Pa", [128, 768], F32), ("Pta", [128, 768], F32), ("Rbf", [128, 768], BF16),
                                     ("AqT", [128, 768], BF16), ("WtT", [128, 384], BF16), ("Utb", [128, 384], F32),
                                     ("qbar", [128, 384], BF16), ("Ubz", [128, 768], BF16)]:
                    d[nm] = sb(f"g_{nm}{pb}", shp, dtp)
                B.append(d)
            sc.act(lambda e: e.activation(out=nA[:], in_=gs[:, 6:12], func=AF.Exp), ["gsm"], ["g_nA"])
            sc.dve(lambda e: e.tensor_scalar(out=nA[:], in0=nA[:], scalar1=-1.0, scalar2=None, op0=ALU.mult), ["g_nA"], ["g_nA"])
            sc.dve(lambda e: e.memset(H32[:], 0.0), [], [("g_H32", h) for h in range(6)])
            sc.pool(lambda e: e.memset(Hbd[:], 0.0), [], [("g_Hbd", h) for h in range(6)])
            for i in range(3):
                for hp in range(2):
                    sc.pool(lambda e, i=i, hp=hp: e.memset(kz[i][hp][:], 0.0), [], [f"g_kz{i}"])
            for pb in range(2):
                sc.pool(lambda e, pb=pb: e.memset(B[pb]["Kez"][:], 0.0), [], [(f"g_Kez{pb}", h) for h in range(6)])
                sc.pool(lambda e, pb=pb: e.memset(B[pb]["Ubz"][:], 0.0), [], [(f"g_Ubz{pb}", h) for h in range(6)])
            nblk = 0
            for sbi in range(S // 512):
                t0 = sbi * 512
                for rc in range(9):
                    xi = xin[rc % 2]; xt = f"g_xin{rc % 2}"
                    ac = acc[rc % 2]; at_ = f"g_acc{rc % 2}"
                    r0 = 1824 + rc * 128
                    if sbi == 0:
                        sc.pool(lambda e, xi=xi: e.memset(xi[:, 0:3], 0.0), [], [xt])
                        sc.dma(xi[:, 3:515], self.pT[r0:r0 + 128, 0:512], reads=[xt], writes=[xt])
                    else:
                        sc.dma(xi[:], self.pT[r0:r0 + 128, t0 - 3:t0 + 512], writes=[xt])
                    cb = rc * 4
                    sc.dve(lambda e, xi=xi, ac=ac, cb=cb: e.tensor_scalar(out=ac[:], in0=xi[:, 3:515], scalar1=cwl[:, cb + 3:cb + 4],
                                                                         scalar2=None, op0=ALU.mult), [xt, "cw"], [at_])
                    for j in range(3):
                        sc.dve(lambda e, xi=xi, ac=ac, cb=cb, j=j: e.scalar_tensor_tensor(
                            out=ac[:], in0=xi[:, j:j + 512], scalar=cwl[:, cb + j:cb + j + 1], in1=ac[:], op0=ALU.mult, op1=ALU.add),
                            [xt, "cw", at_], [at_])
                    kind, i3 = rc // 3, rc % 3
                    if kind == 2:
                        sc.act(lambda e, ac=ac, i3=i3: e.activation(out=vf32[i3][:], in_=ac[:], func=AF.Silu), [at_], [f"g_vf{i3}"])
                        continue
                    sc.act(lambda e, ac=ac: e.activation(out=ac[:], in_=ac[:], func=AF.Silu), [at_], [at_])
                    sc.pool(lambda e, ac=ac: e.tensor_tensor(out=sqt[:], in0=ac[:], in1=ac[:], op=ALU.mult), [at_], ["g_sq"])
                    p, ptok = self.psum()
                    sc.pe(lambda e, p=p: e.matmul(p[:], lhsT=self.onesbd_bf[:], rhs=sqt[:], start=True, stop=True), ["g_sq", "onesbd_bf"], [ptok])
                    sc.act(lambda e, p=p: e.activation(out=rinv[:], in_=p[:], func=AF.Sqrt, bias=1e-6, scale=1.0), [ptok], ["g_rinv"])
                    sc.dve(lambda e: e.reciprocal(out=rinv[:], in_=rinv[:]), ["g_rinv"], ["g_rinv"])
                    if kind == 0:
                        sc.dve(lambda e, ac=ac, i3=i3: e.scalar_tensor_tensor(out=qf32[i3][:], in0=ac[:], scalar=0.125, in1=rinv[:],
                                                                              op0=ALU.mult, op1=ALU.mult), [at_, "g_rinv"], [f"g_qf{i3}"])
                        sc.pool(lambda e, i3=i3: e.tensor_copy(out=qfb[i3][:], in_=qf32[i3][:]), [f"g_qf{i3}"], [f"g_qb{i3}"])
                    else:
                        sc.dve(lambda e, ac=ac, i3=i3: e.tensor_tensor(out=kf32[i3][:], in0=ac[:], in1=rinv[:], op=ALU.mult),
                               [at_, "g_rinv"], [f"g_kf{i3}"])
                        sc.pool(lambda e, i3=i3: e.tensor_copy(out=kfb[i3][:], in_=kf32[i3][:]), [f"g_kf{i3}"], [f"g_kb{i3}"])
                        for hp in range(2):
                            sc.pool(lambda e, i3=i3, hp=hp: e.tensor_copy(out=kz[i3][hp][hp * 64:hp * 64 + 64, :], in_=kf32[i3][hp * 64:hp * 64 + 64, :]),
                                    [f"g_kf{i3}"], [f"g_kz{i3}"])
                sc.dma(batm[:], self.baT[t0:t0 + 512, :].rearrange("(n p) c -> p n c", p=128), writes=["g_batm"])
                sc.act(lambda e: e.activation(out=beta[:], in_=batm[:, :, 0:6], func=AF.Sigmoid), ["g_batm"], ["g_beta"])
                sc.dve(lambda e: e.tensor_scalar(out=nbeta[:], in0=beta[:], scalar1=-1.0, scalar2=None, op0=ALU.mult), ["g_beta"], ["g_nbeta"])
                for n in range(4):
                    sc.dve(lambda e, n=n: e.tensor_tensor(out=tmp46[:, n, :], in0=batm[:, n, 6:12], in1=gs[:, 0:6], op=ALU.add),
                           ["g_batm", "gsm"], ["g_tmp46"])
                sc.act(lambda e: e.activation(out=tmp46[:], in_=tmp46[:], func=AF.Exp), ["g_tmp46"], ["g_tmp46"])
                sc.act(lambda e: e.activation(out=tmp46[:], in_=tmp46[:], func=AF.Ln, bias=1.0), ["g_tmp46"], ["g_tmp46"])
                for n in range(4):
                    sc.dve(lambda e, n=n: e.tensor_tensor(out=gtm[:, n, :], in0=tmp46[:, n, :], in1=nA[:], op=ALU.mult),
                           ["g_tmp46", "g_nA"], ["g_gtm"])
                for n in range(4 if self.stage >= 2 else 0):
                    pb = nblk % 2
                    nblk += 1
                    b = B[pb]
                    tk = lambda nm, pb=pb: f"g_{nm}{pb}"
                    cs = slice(n * 128, (n + 1) * 128)
                    g_n = gtm[:, n, :]
                    p1, t1 = self.psum()
                    sc.pe(lambda e, p1=p1, g_n=g_n: e.matmul(p1[:, 0:6], lhsT=useg, rhs=g_n, start=True, stop=True), ["g_gtm", "C"], [t1])
                    sc.pe(lambda e, p1=p1, g_n=g_n: e.matmul(p1[:, 8:14], lhsT=onesbd, rhs=g_n, start=True, stop=True), ["g_gtm", "C"], [t1])
                    sc.dve(lambda e, p1=p1, b=b: e.tensor_copy(out=b["gcc"][:], in_=p1[:, 0:6]), [t1], [tk("gcc")])
                    sc.dve(lambda e, p1=p1, b=b: e.tensor_tensor(out=b["gct"][:], in0=p1[:, 8:14], in1=b["gcc"][:], op=ALU.subtract),
                           [t1, tk("gcc")], [tk("gct")])
                    sc.act(lambda e, b=b: e.activation(out=b["egc"][:], in_=b["gcc"][:], func=AF.Exp), [tk("gcc")], [tk("egc")])
                    sc.act(lambda e, b=b: e.activation(out=b["eend"][:], in_=b["gct"][:], func=AF.Exp), [tk("gct")], [tk("eend")])
                    if self.sub < 2:
                        continue
                    for h in range(6):
                        eng = sc.dve if h % 2 == 0 else sc.pool
                        eng(lambda e, b=b, h=h, g_n=g_n: e.tensor_scalar(out=b["GU"][:, h * 128:(h + 1) * 128], in0=useg, scalar1=g_n[:, h:h + 1],
                                                                       scalar2=None, op0=ALU.mult), ["C", "g_gtm"], [(tk("GU"), h)])
                    p2, t2 = self.psum()
                    p3, t3 = self.psum()
                    allGU = [(tk("GU"), h) for h in range(6)]
                    sc.pe(lambda e, p2=p2, b=b: e.matmul(p2[:, 0:512], lhsT=ones, rhs=b["GU"][:, 0:512], start=True, stop=True), allGU + ["C"], [t2])
                    sc.pe(lambda e, p3=p3, b=b: e.matmul(p3[:, 0:256], lhsT=ones, rhs=b["GU"][:, 512:768], start=True, stop=True), allGU + ["C"], [t3])
                    sc.act(lambda e, p2=p2, b=b: e.activation(out=b["Egc"][:, 0:512], in_=p2[:, 0:512], func=AF.Exp), [t2], [tk("Egc")])
                    sc.act(lambda e, p3=p3, b=b: e.activation(out=b["Egc"][:, 512:768], in_=p3[:, 0:256], func=AF.Exp), [t3], [tk("Egc")])
                    for h in range(6):
                        src = p2[:, h * 128:(h + 1) * 128] if h < 4 else p3[:, (h - 4) * 128:(h - 3) * 128]
                        sc.dve(lambda e, b=b, h=h, src=src: e.scalar_tensor_tensor(
                            out=b["dd"][:, h * 128:(h + 1) * 128], in0=src, scalar=b["gcc"][:, h:h + 1], in1=mneg,
                            op0=ALU.subtract, op1=ALU.add), [t2, t3, tk("gcc"), "C"], [tk("dd")])
                    sc.act(lambda e, b=b: e.activation(out=b["DmT"][:], in_=b["dd"][:], func=AF.Exp), [tk("dd")], [tk("DmT")])
                    sc.pool(lambda e, b=b: e.tensor_tensor(out=b["DmS"][:], in0=b["DmT"][:], in1=offd6, op=ALU.mult), [tk("DmT"), "C"], [tk("DmS")])
                    for rc in range(3):
                        pk, tkk = self.psum()
                        sc.pe(lambda e, pk=pk, rc=rc, cs=cs: e.matmul(pk[:, 0:128], lhsT=kf32[rc][:, cs], rhs=ident, start=True, stop=True), [f"g_kf{rc}", "C"], [tkk])
                        sc.pe(lambda e, pk=pk, rc=rc, cs=cs: e.matmul(pk[:, 128:256], lhsT=vf32[rc][:, cs], rhs=ident, start=True, stop=True), [f"g_vf{rc}", "C"], [tkk])
                        sc.act(lambda e, pk=pk, rc=rc, b=b: e.copy(out=b["Ktm"][:, rc * 128:(rc + 1) * 128], in_=pk[:, 0:128]), [tkk], [(tk("Ktm"), rc)])
                        sc.act(lambda e, pk=pk, rc=rc, b=b: e.copy(out=b["Vtm"][:, rc * 128:(rc + 1) * 128], in_=pk[:, 128:256]), [tkk], [(tk("Vtm"), rc)])
                    for c in range(2):
                        sc.dve(lambda e, b=b, c=c: e.tensor_scalar(out=b[f"eendc{c}"][:], in0=b["eend"][:], scalar1=C[:, 256 + 64 * c:257 + 64 * c],
                                                                   scalar2=None, op0=ALU.mult), [tk("eend"), "C"], [tk(f"eendc{c}")])
                    for h in range(6):
                        eng = sc.dve if h % 2 == 0 else sc.pool
                        hs = slice(h * 64, (h + 1) * 64)
                        kz_c = slice(h * 128 + (h % 2) * 64, h * 128 + (h % 2) * 64 + 64)
                        eng(lambda e, b=b, h=h, hs=hs, kz_c=kz_c: e.tensor_scalar(out=b["Kez"][:, kz_c], in0=b["Ktm"][:, hs], scalar1=b["egc"][:, h:h + 1],
                                                                       scalar2=None, op0=ALU.mult), [(tk("Ktm"), h // 2), tk("egc")], [(tk("Kez"), h)])
                        for c in range(2):
                            eng(lambda e, b=b, h=h, hs=hs, c=c: e.tensor_scalar(out=b[f"Bhc{c}"][:, hs], in0=b["Ktm"][:, hs], scalar1=b[f"eendc{c}"][:, h:h + 1],
                                                                           scalar2=None, op0=ALU.mult), [(tk("Ktm"), h // 2), tk(f"eendc{c}")], [(tk(f"Bhc{c}"), h)])
                    pKa, tKa = self.psum(); pKb, tKb = self.psum()
                    pQa, tQa = self.psum(); pQb, tQb = self.psum()
                    def bank(h, pa, ta, pb_, tb):
                        return (pa[:, h * 128:(h + 1) * 128], ta) if h < 4 else (pb_[:, (h - 4) * 128:(h - 3) * 128], tb)
                    for h in range(6):
                        rc, hp = h // 2, h % 2
                        o1, to1 = bank(h, pKa, tKa, pKb, tKb)
                        sc.pe(lambda e, o1=o1, rc=rc, hp=hp, cs=cs: e.matmul(o1, lhsT=kz[rc][hp][:, cs], rhs=kfb[rc][:, cs], start=True, stop=True),
                              [f"g_kb{rc}", f"g_kz{rc}"], [to1])
                        o2, to2 = bank(h, pQa, tQa, pQb, tQb)
                        sc.pe(lambda e, o2=o2, rc=rc, hp=hp, cs=cs: e.matmul(o2, lhsT=kz[rc][hp][:, cs], rhs=qfb[rc][:, cs], start=True, stop=True),
                              [f"g_kz{rc}", f"g_qb{rc}"], [to2])
                    for h in range(6):
                        hc = slice(h * 128, (h + 1) * 128)
                        o1, to1 = bank(h, pKa, tKa, pKb, tKb)
                        sc.dve(lambda e, b=b, h=h, hc=hc, o1=o1, n=n: e.scalar_tensor_tensor(
                            out=b["Mm"][:, hc], in0=o1, scalar=nbeta[:, n, h:h + 1], in1=b["DmS"][:, hc], op0=ALU.mult, op1=ALU.mult),
                            [to1, "g_nbeta", tk("DmS")], [(tk("Mm"), h)])
                        o2, to2 = bank(h, pQa, tQa, pQb, tQb)
                        sc.dve(lambda e, b=b, hc=hc, o2=o2: e.tensor_tensor(out=b["AqT"][:, hc], in0=o2, in1=b["DmT"][:, hc], op=ALU.mult),
                               [to2, tk("DmT")], [(tk("AqT"), h)])
                    self.neumann(b, tk, ident, ident6)
                    sc.pool(lambda e, b=b: e.tensor_copy(out=b["Rbf"][:], in_=b["R"][:]), [tk("R")], [tk("Rbf")])
                    pW, tW = self.psum()
                    pU, tU = self.psum()
                    for h in range(6):
                        rc, hp = h // 2, h % 2
                        hs = slice(h * 64, (h + 1) * 64); hc = slice(h * 128, (h + 1) * 128)
                        sc.pe(lambda e, pW=pW, b=b, rc=rc, hp=hp, hc=hc: e.matmul(
                            pW[:, rc * 128:(rc + 1) * 128], lhsT=b["Kez"][:, hc], rhs=b["Rbf"][:, hc], start=(hp == 0), stop=(hp == 1)),
                            [(tk("Kez"), h), tk("Rbf")], [tW])
                        sc.pe(lambda e, pU=pU, b=b, hs=hs, hc=hc: e.matmul(pU[:, hs], lhsT=b["Rbf"][:, hc], rhs=b["Vtm"][:, hs], start=True, stop=True),
                              [(tk("Vtm"), h // 2), tk("Rbf")], [tU])
                    sc.act(lambda e, pW=pW, b=b: e.mul(out=b["WtT"][:], in_=pW[:, 0:384], mul=-1.0), [tW], [tk("WtT")])
                    for h in range(6):
                        hs = slice(h * 64, (h + 1) * 64)
                        sc.dve(lambda e, pU=pU, b=b, h=h, hs=hs, n=n: e.tensor_scalar(out=b["Utb"][:, hs], in0=pU[:, hs], scalar1=beta[:, n, h:h + 1],
                                                                                 scalar2=None, op0=ALU.mult), [tU, "g_beta"], [tk("Utb")])
                    for h in range(6):
                        rc, hp = h // 2, h % 2
                        rows = slice(hp * 64, hp * 64 + 64)
                        eng = sc.pool if h % 2 == 0 else sc.dve
                        eng(lambda e, b=b, rc=rc, rows=rows, h=h, cs=cs: e.tensor_tensor(
                            out=b["qbar"][rows, rc * 128:(rc + 1) * 128], in0=qf32[rc][rows, cs], in1=b["Egc"][rows, h * 128:(h + 1) * 128], op=ALU.mult),
                            [f"g_qf{rc}", tk("Egc")], [tk("qbar")])
                    gl_ap = lambda h, c, b=b: b["Egc"][(h % 2) * 64:(h % 2) * 64 + 64, h * 128 + c * 64 + 63:h * 128 + c * 64 + 64]
                    self.chunk_loop(b, tk, n, psO, H32, Hbd, "g", gl_ap, [tk("Egc")], beta=beta)
                for rc in range(3 if self.stage >= 6 else 0):
                    po, pot = psO[rc]
                    sc.act(lambda e, po=po: e.copy(out=osb[:], in_=po[:]), [pot], ["g_osb"])
                    sc.pool(lambda e: e.tensor_tensor(out=sqt[:], in0=osb[:], in1=osb[:], op=ALU.mult), ["g_osb"], ["g_sq"])
                    p, ptok = self.psum()
                    sc.pe(lambda e, p=p: e.matmul(p[:], lhsT=self.onesbd_bf[:], rhs=sqt[:], start=True, stop=True), ["g_sq", "onesbd_bf"], [ptok])
                    sc.act(lambda e, p=p: e.activation(out=rinv[:], in_=p[:], func=AF.Sqrt, bias=NORM_EPS, scale=1.0 / 64), [ptok], ["g_rinv"])
                    sc.dve(lambda e: e.reciprocal(out=rinv[:], in_=rinv[:]), ["g_rinv"], ["g_rinv"])
                    r0 = 2976 + rc * 128
                    sc.dma(gate[:], self.pT[r0:r0 + 128, t0:t0 + 512], writes=["g_gate"])
                    sc.act(lambda e: e.activation(out=gate[:], in_=gate[:], func=AF.Silu), ["g_gate"], ["g_gate"])
                    sc.dve(lambda e: e.tensor_tensor(out=osb[:], in0=osb[:], in1=rinv[:], op=ALU.mult), ["g_osb", "g_rinv"], ["g_osb"])
                    sc.dve(lambda e: e.scalar_tensor_tensor(out=ybf[:], in0=osb[:], scalar=gs[:, 12:13], in1=gate[:], op0=ALU.mult, op1=ALU.mult),
                           ["g_osb", "gsm", "g_gate"], ["g_ybf"])
                    y0 = 640 + rc * 128
                    sc.dma(self.yT[y0:y0 + 128, t0:t0 + 512], ybf[:], reads=["g_ybf"], writes=[("yT", y0, sbi)], q="pool")
            sc.flush()
        self.ps_n = 8

    def mla_tables(self):
        nc, sc, S = self.nc, self.sc, self.S
        with contextlib.ExitStack() as st:
            T = 2048 if S % 2048 == 0 else 512
            pi_ = st.enter_context(nc.sbuf_tensor("m_posi", [96, T], I32))
            pf = st.enter_context(nc.sbuf_tensor("m_posf", [96, T], F32))
            t1 = st.enter_context(nc.sbuf_tensor("m_tt1", [96, T], F32))
            t2 = st.enter_context(nc.sbuf_tensor("m_tt2", [96, T], F32))
            ifq = self.mlc[0:96, 0:1]
            for t in range(S // T):
                t0 = t * T
                sc.dma(pi_[:], self.pos96[:, t0:t0 + T], writes=["m_posi"])
                sc.dve(lambda e: e.tensor_copy(out=pf[:], in_=pi_[:]), ["m_posi"], ["m_posf"])
                sc.dve(lambda e: e.tensor_scalar(out=pf[:], in0=pf[:], scalar1=ifq, scalar2=None, op0=ALU.mult), ["m_posf", "mlc"], ["m_posf"])
                sc.dve(lambda e: e.tensor_scalar(out=pf[:], in0=pf[:], scalar1=float(1.0 / (2.0 * np.pi)), scalar2=None, op0=ALU.mult), ["m_posf"], ["m_posf"])
                sc.dve(lambda e: e.tensor_copy(out=pi_[:], in_=pf[:]), ["m_posf", "m_posi"], ["m_posi"])
                sc.dve(lambda e: e.tensor_copy(out=t1[:], in_=pi_[:]), ["m_posi"], ["m_tt1"])
                sc.dve(lambda e: e.tensor_tensor(out=pf[:], in0=pf[:], in1=t1[:], op=ALU.subtract), ["m_posf", "m_tt1"], ["m_posf"])
                sc.act(lambda e: e.activation(out=t1[:], in_=pf[:], func=AF.Sin, scale=float(np.pi)), ["m_posf"], ["m_tt1"])
                sc.act(lambda e: e.activation(out=t2[:], in_=pf[:], func=AF.Sin, scale=float(np.pi / 2)), ["m_posf"], ["m_tt2"])
                sc.dve(lambda e: e.tensor_tensor(out=t2[:], in0=t2[:], in1=t2[:], op=ALU.mult), ["m_tt2"], ["m_tt2"])
                sc.dve(lambda e: e.tensor_scalar(out=t2[:], in0=t2[:], scalar1=-2.0, scalar2=1.0, op0=ALU.mult, op1=ALU.add), ["m_tt2"], ["m_tt2"])
                sc.dve(lambda e: e.scalar_tensor_tensor(out=t2[:], in0=t1[:], scalar=2.0, in1=t2[:], op0=ALU.mult, op1=ALU.mult), ["m_tt1", "m_tt2"], ["m_tt2"])
                sc.dma(self.sinT[:, t0:t0 + T], t2[:], reads=["m_tt2"], writes=[("tab", 1, t)], q="pool")
                sc.dve(lambda e: e.tensor_tensor(out=t1[:], in0=t1[:], in1=t1[:], op=ALU.mult), ["m_tt1"], ["m_tt1"])
                sc.dve(lambda e: e.tensor_scalar(out=t1[:], in0=t1[:], scalar1=-2.0, scalar2=1.0, op0=ALU.mult, op1=ALU.add), ["m_tt1"], ["m_tt1"])
                sc.dma(self.cosT[:, t0:t0 + T], t1[:], reads=["m_tt1"], writes=[("tab", 0, t)], q="pool")
            sc.flush()

    def phase_mla(self, l):
        nc, sc, S = self.nc, self.sc, self.S
        C = self.C
        T = 512
        ml = self.mls[:, l * 8:(l + 1) * 8]
        with contextlib.ExitStack() as st:
            sb = lambda n, s, d=F32: st.enter_context(nc.sbuf_tensor(f"{n}_L{l}", s, d))
            wst = sb("m_wst", [128, 512])
            wuq = sb("m_wuq", [128, 2, 384], BF16); wuqr = sb("m_wuqr", [128, 2, 384], BF16)
            wkn = sb("m_wkn", [128, 384], BF16); wv = sb("m_wv", [128, 256], BF16)
            sel = sb("m_sel", [32, 96], BF16); ones96 = sb("m_ones96", [96, 96], BF16)
            cq = sb("m_cq", [128, 2, T]); ckv = sb("m_ckv", [128, T]); kr = sb("m_kr", [32, T]); krt = sb("m_krt", [32, T])
            sq2 = sb("m_sq2", [128, 2, T], BF16); sq1 = sb("m_sq1", [128, T], BF16)
            rq = sb("m_rq", [128, T]); rkv = sb("m_rkv", [128, T])
            cqn = sb("m_cqn", [128, 2, T], BF16); ckvn = sb("m_ckvn", [128, T], BF16)
            krb = sb("m_krb", [32, T], BF16); krtb = sb("m_krtb", [32, T], BF16)
            cosb = sb("m_cos", [96, T]); sinb = sb("m_sin", [96, T])
            raw = sb("m_raw", [96, T]); rot = sb("m_rot", [96, T]); sq96 = sb("m_sq96", [96, T], BF16); rs96 = sb("m_rs96", [96, T])
            fin = [sb(f"m_fin{i}", [96, T], BF16) for i in range(2)]
            vx = [sb(f"m_vx{i}", [128, 4, 128], BF16) for i in range(2)]
            sc.dma(wst[:, 0:384], self.w_uq[l, 0:128, :], writes=["m_wst"])
            sc.dve(lambda e: e.tensor_scalar(out=wuq[:, 0, :], in0=wst[:, 0:384], scalar1=ml[:, 0:1], scalar2=None, op0=ALU.mult), ["m_wst", "mls"], ["m_wuq"])
            sc.dma(wst[:, 0:384], self.w_uq[l, 128:256, :], reads=["m_wst"], writes=["m_wst"])
            sc.dve(lambda e: e.tensor_scalar(out=wuq[:, 1, :], in0=wst[:, 0:384], scalar1=ml[:, 1:2], scalar2=None, op0=ALU.mult), ["m_wst", "mls"], ["m_wuq"])
            sc.pool(lambda e: e.memset(wuqr[:], 0.0), [], ["m_wuqr"])
            for h in range(4):
                b0 = h * 96
                sc.dve(lambda e, b0=b0: e.tensor_scalar(out=wuqr[:, :, b0 + 64:b0 + 80], in0=wuq[:, :, b0 + 80:b0 + 96], scalar1=-1.0, scalar2=None, op0=ALU.mult),
                       ["m_wuq", "m_wuqr"], ["m_wuqr"])
                sc.dve(lambda e, b0=b0: e.tensor_copy(out=wuqr[:, :, b0 + 80:b0 + 96], in_=wuq[:, :, b0 + 64:b0 + 80]), ["m_wuq", "m_wuqr"], ["m_wuqr"])
            sc.dma(wst[:], self.w_ukv[l], reads=["m_wst"], writes=["m_wst"])
            sc.pool(lambda e: e.memset(wkn[:], 0.0), [], ["m_wkn"])
            for h in range(4):
                sc.dve(lambda e, h=h: e.tensor_scalar(out=wkn[:, h * 96:h * 96 + 64], in0=wst[:, h * 128:h * 128 + 64], scalar1=ml[:, 2:3], scalar2=None, op0=ALU.mult),
                       ["m_wst", "mls", "m_wkn"], ["m_wkn"])
                sc.dve(lambda e, h=h: e.tensor_scalar(out=wv[:, h * 64:h * 64 + 64], in0=wst[:, h * 128 + 64:h * 128 + 128], scalar1=ml[:, 2:3], scalar2=None, op0=ALU.mult),
                       ["m_wst", "mls"], ["m_wv"])
            sc.dve(lambda e: e.tensor_copy(out=sel[:], in_=self.mlc[0:32, 8:104]), ["mlc"], ["m_sel"])
            sc.pool(lambda e: e.memset(ones96[:], 1.0), [], ["m_ones96"])
            for i in range(2):
                sc.pool(lambda e, i=i: e.memset(vx[i][:], 1.0), [], [f"m_vx{i}"])
            nfin = 0
            for t in range(S // T):
                t0 = t * T
                sc.dma(cq[:], self.pT[1408:1664, t0:t0 + T].rearrange("(k p) s -> p k s", p=128), writes=["m_cq"])
                sc.dma(ckv[:], self.pT[1664:1792, t0:t0 + T], writes=["m_ckv"])
                sc.dma(kr[:], self.pT[1792:1824, t0:t0 + T], writes=["m_kr"])
                sc.dma(krt[:], self.pT[3372:3404, t0:t0 + T], writes=["m_krt"])
                sc.dma(cosb[:], self.cosT[:, t0:t0 + T], writes=["m_cos"])
                sc.dma(sinb[:], self.sinT[:, t0:t0 + T], writes=["m_sin"])
                sc.act(lambda e: e.activation(out=sq2[:], in_=cq[:], func=AF.Square), ["m_cq"], ["m_sq2"])
                sc.act(lambda e: e.activation(out=sq1[:], in_=ckv[:], func=AF.Square), ["m_ckv"], ["m_sq1"])
                p, ptok = self.psum()
                for kc in range(2):
                    sc.pe(lambda e, p=p, kc=kc: e.matmul(p[:], lhsT=self.ones_bf[:], rhs=sq2[:, kc, :], start=(kc == 0), stop=(kc == 1)), ["m_sq2", "ones_bf"], [ptok])
                sc.act(lambda e, p=p: e.activation(out=rq[:], in_=p[:], func=AF.Sqrt, bias=NORM_EPS, scale=1.0 / 256), [ptok], ["m_rq"])
                sc.dve(lambda e: e.reciprocal(out=rq[:], in_=rq[:]), ["m_rq"], ["m_rq"])
                p, ptok = self.psum()
                sc.pe(lambda e, p=p: e.matmul(p[:], lhsT=self.ones_bf[:], rhs=sq1[:], start=True, stop=True), ["m_sq1", "ones_bf"], [ptok])
                sc.act(lambda e, p=p: e.activation(out=rkv[:], in_=p[:], func=AF.Sqrt, bias=NORM_EPS, scale=1.0 / 128), [ptok], ["m_rkv"])
                sc.dve(lambda e: e.reciprocal(out=rkv[:], in_=rkv[:]), ["m_rkv"], ["m_rkv"])
                for kc in range(2):
                    sc.dve(lambda e, kc=kc: e.tensor_tensor(out=cqn[:, kc, :], in0=cq[:, kc, :], in1=rq[:], op=ALU.mult), ["m_cq", "m_rq"], ["m_cqn"])
                sc.dve(lambda e: e.tensor_tensor(out=ckvn[:], in0=ckv[:], in1=rkv[:], op=ALU.mult), ["m_ckv", "m_rkv"], ["m_ckvn"])
                sc.pool(lambda e: e.tensor_copy(out=krb[:], in_=kr[:]), ["m_kr"], ["m_krb"])
                sc.pool(lambda e: e.tensor_copy(out=krtb[:], in_=krt[:]), ["m_krt"], ["m_krtb"])
                for h in range(4):
                    hc = slice(h * 96, (h + 1) * 96)
                    for isk in range(2):
                        pr, prt = self.psum()
                        pro, prot = self.psum()
                        if isk == 0:
                            for kc in range(2):
                                sc.pe(lambda e, pr=pr, kc=kc, hc=hc: e.matmul(pr[0:96, :], lhsT=wuq[:, kc, hc], rhs=cqn[:, kc, :], start=(kc == 0), stop=(kc == 1)),
                                      ["m_wuq", "m_cqn"], [prt])
                            for kc in range(2):
                                sc.pe(lambda e, pro=pro, kc=kc, hc=hc: e.matmul(pro[0:96, :], lhsT=wuqr[:, kc, hc], rhs=cqn[:, kc, :], start=(kc == 0), stop=(kc == 1)),
                                      ["m_wuqr", "m_cqn"], [prot])
                            gcol, gpcol, fscale = ml[0:96, 3:4], ml[0:96, 4:5], float(96 ** -0.5)
                        else:
                            sc.pe(lambda e, pr=pr, hc=hc: e.matmul(pr[0:96, :], lhsT=wkn[:, hc], rhs=ckvn[:], start=True, stop=False), ["m_wkn", "m_ckvn"], [prt])
                            sc.pe(lambda e, pr=pr: e.matmul(pr[0:96, :], lhsT=sel[:], rhs=krb[:], start=False, stop=True), ["m_sel", "m_krb"], [prt])
                            sc.pe(lambda e, pro=pro: e.matmul(pro[0:96, :], lhsT=sel[:], rhs=krtb[:], start=True, stop=True), ["m_sel", "m_krtb"], [prot])
                            gcol, gpcol, fscale = ml[0:96, 5:6], ml[0:96, 6:7], 1.0
                        sc.act(lambda e, pr=pr: e.copy(out=raw[:], in_=pr[0:96, :]), [prt], ["m_raw"])
                        sc.act(lambda e, pro=pro: e.copy(out=rot[:], in_=pro[0:96, :]), [prot], ["m_rot"])
                        sc.pool(lambda e: e.tensor_tensor(out=sq96[:], in0=raw[:], in1=raw[:], op=ALU.mult), ["m_raw"], ["m_sq96"])
                        p, ptok = self.psum()
                        sc.pe(lambda e, p=p: e.matmul(p[0:96, :], lhsT=ones96[:], rhs=sq96[:], start=True, stop=True), ["m_sq96", "m_ones96"], [ptok])
                        sc.act(lambda e, p=p: e.activation(out=rs96[:], in_=p[0:96, :], func=AF.Sqrt, bias=NORM_EPS, scale=1.0 / 96), [ptok], ["m_rs96"])
                        sc.dve(lambda e: e.reciprocal(out=rs96[:], in_=rs96[:]), ["m_rs96"], ["m_rs96"])
                        sc.dve(lambda e, gcol=gcol: e.scalar_tensor_tensor(out=raw[:], in0=raw[:], scalar=gcol, in1=cosb[:], op0=ALU.mult, op1=ALU.mult),
                               ["m_raw", "mls", "m_cos"], ["m_raw"])
                        sc.dve(lambda e, gpcol=gpcol: e.scalar_tensor_tensor(out=rot[:], in0=rot[:], scalar=gpcol, in1=sinb[:], op0=ALU.mult, op1=ALU.mult),
                               ["m_rot", "mls", "m_sin"], ["m_rot"])
                        sc.dve(lambda e: e.tensor_tensor(out=raw[:], in0=raw[:], in1=rot[:], op=ALU.add), ["m_raw", "m_rot"], ["m_raw"])
                        sc.dve(lambda e: e.tensor_tensor(out=raw[:], in0=raw[:], in1=rs96[:], op=ALU.mult), ["m_raw", "m_rs96"], ["m_raw"])
                        fi = nfin % 2
                        nfin += 1
                        sc.act(lambda e, fi=fi, fscale=fscale: e.mul(out=fin[fi][:], in_=raw[:], mul=fscale), ["m_raw"], [f"m_fin{fi}"])
                        dstT = self.qfT if isk == 0 else self.kfT
                        sc.dma(dstT[h, :, t0:t0 + T], fin[fi][:], reads=[f"m_fin{fi}"], writes=[("qk", isk, h, t)], q="pool")
                for n in range(4):
                    vi = (t * 4 + n) % 2
                    p, ptok = self.psum()
                    sc.pe(lambda e, p=p, n=n: e.matmul(p[:, 0:256], lhsT=ckvn[:, n * 128:(n + 1) * 128], rhs=wv[:], start=True, stop=True), ["m_ckvn", "m_wv"], [ptok])
                    sc.act(lambda e, p=p, vi=vi: e.copy(out=vx[vi][:, :, 0:64], in_=p[:, 0:256].rearrange("p (h d) -> p h d", d=64)), [ptok], [f"m_vx{vi}"])
                    r0 = t0 + n * 128
                    sc.dma(self.vxT[:, r0:r0 + 128, :].rearrange("h p d -> p h d"), vx[vi][:], reads=[f"m_vx{vi}"], writes=[("vx", t, n)], q="pool")
            sc.flush()
        self.ps_n = 6
        psOD = [(self.ps[6 + i], f"psOD{i}") for i in range(2)]
        NB = S // 128
        with contextlib.ExitStack() as st:
            sb = lambda n, s, d=F32: st.enter_context(nc.sbuf_tensor(f"{n}_L{l}", s, d))
            kf = sb("m_kf", [96, S], BF16); qf = sb("m_qf", [96, S], BF16)
            vxa = sb("m_vxa", [128, NB, 128], BF16)
            mk32 = sb("m_mk32", [128, 2048]); mk = sb("m_mk", [128, 2048], BF16)
            pt = [sb(f"m_pt{i}", [128, T], BF16) for i in range(3)]
            od = sb("m_od", [128, T]); den = sb("m_den", [64, T]); yb = [sb(f"m_yb{i}", [64, T], BF16) for i in range(2)]
            sc.dma(mk32[:], self.mmask, writes=["m_mk32"])
            sc.dve(lambda e: e.tensor_copy(out=mk[:], in_=mk32[:]), ["m_mk32"], ["m_mk"])
            npt = 0
            ng = 0
            for h in range(4):
                sc.dma(kf[:], self.kfT[h], writes=["m_kf"])
                sc.dma(qf[:], self.qfT[h], writes=["m_qf"])
                sc.dma(vxa[:], self.vxT[h].rearrange("(n p) d -> p n d", p=128), writes=["m_vxa"])
                for g in range(S // T):
                    po, pot = psOD[ng % 2]
                    nkb = 4 * g + 4
                    for kb in range(nkb):
                        ps_, pst = self.psum()
                        sc.pe(lambda e, ps_=ps_, kb=kb, g=g: e.matmul(ps_[:], lhsT=kf[:, kb * 128:(kb + 1) * 128], rhs=qf[:, g * T:(g + 1) * T], start=True, stop=True),
                              ["m_kf", "m_qf"], [pst])
                        pi = npt % 3
                        npt += 1
                        sc.act(lambda e, ps_=ps_, pi=pi: e.activation(out=pt[pi][:], in_=ps_[:], func=AF.Exp), [pst], [f"m_pt{pi}"])
                        j = kb - 4 * g
                        if j >= 0:
                            sc.pool(lambda e, pi=pi, j=j: e.tensor_tensor(out=pt[pi][:], in0=pt[pi][:], in1=mk[:, j * T:(j + 1) * T], op=ALU.mult),
                                    [f"m_pt{pi}", "m_mk"], [f"m_pt{pi}"])
                        sc.pe(lambda e, po=po, kb=kb, pi=pi, nkb=nkb: e.matmul(po[:], lhsT=vxa[:, kb, :], rhs=pt[pi][:], start=(kb == 0), stop=(kb == nkb - 1)),
                              ["m_vxa", f"m_pt{pi}"], [pot])
                    sc.act(lambda e, po=po: e.copy(out=od[:], in_=po[:]), [pot], ["m_od"])
                    sc.dve(lambda e: e.tensor_copy(out=den[:], in_=od[64:128, :]), ["m_od"], ["m_den"])
                    sc.dve(lambda e: e.reciprocal(out=den[:], in_=den[:]), ["m_den"], ["m_den"])
                    yi = ng % 2
                    sc.dve(lambda e, yi=yi: e.tensor_tensor(out=yb[yi][:], in0=od[0:64, :], in1=den[:], op=ALU.mult), ["m_od", "m_den"], [f"m_yb{yi}"])
                    y0 = 384 + h * 64
                    sc.dma(self.yT[y0:y0 + 64, g * T:(g + 1) * T], yb[yi][:], reads=[f"m_yb{yi}"], writes=[("yT", y0, g)], q="pool")
                    ng += 1
            sc.flush()
        self.ps_n = 8

    def phase_rwkv(self, l):
        nc, sc, S = self.nc, self.sc, self.S
        C = self.C
        ident, onesbd = C[:, 0:128], C[:, 256:384]
        mstrict, mincl, nmstrict = C[:, 768:896], C[:, 896:1024], C[:, 384:512]
        ident6 = C[:, 1792:2560]
        segm = self.segm
        self.ps_n = 5
        psO = [(self.ps[5 + i], f"psO{i}") for i in range(3)]
        rs_ = self.rws[:, l * 32:(l + 1) * 32]
        MU, W0, A0, KK_, KA, RK, GG, GB = 0, 11, 14, 17, 20, 23, 26, 29
        with contextlib.ExitStack() as st:
            sb = lambda n, s, d=F32: st.enter_context(nc.sbuf_tensor(f"{n}_L{l}", s, d))
            T = 512
            wst = sb("r_wst", [128, 1152])
            wbf = sb("r_wbf", [128, 1152], BF16)
            xin = [sb(f"r_xin{i}", [128, 513]) for i in range(2)]
            dtmp = sb("r_dtmp", [128, T])
            f3 = lambda nm, d=F32: [sb(f"r_{nm}{i}", [128, T], d) for i in range(3)]
            rf, kraw, vf, gf_, epos = (f3(n_) for n_ in ("rf", "kraw", "vf", "gf", "epos"))
            af, gc, kk, kmod, bvec, eneg = ([sb(f"r_{n_}", [128, T])] * 3 for n_ in ("af", "gc", "kk", "kmod", "bvec", "eneg"))
            ap32, Bh32, Kh32, bonus = f3("ap32"), f3("Bh32"), f3("Kh32"), f3("bonus")
            apb, rbar, btb, ktb = f3("apb", BF16), f3("rbar", BF16), f3("btb", BF16), f3("ktb", BF16)
            btz = [[sb(f"r_btz{i}_{hp}", [128, T], BF16) for hp in range(2)] for i in range(3)]
            ktz = [[sb(f"r_ktz{i}_{hp}", [128, T], BF16) for hp in range(2)] for i in range(3)]
            z9, zg = sb("r_z9", [128, T]), sb("r_zg", [128, T])
            act9, sigzg = sb("r_act9", [128, T], BF16), sb("r_sigzg", [128, T], BF16)
            tA, tB = sb("r_tA", [128, T]), sb("r_tB", [128, T])
            sqt = sb("r_sq", [128, T], BF16)
            H32 = sb("r_H32", [128, 384]); Hbd = sb("r_Hbd", [128, 384], BF16)
            B = []
            for pb in range(2):
                d = {}
                for nm, shp, dtp in [("Mm", [128, 768], F32), ("Mt", [128, 768], F32), ("R", [128, 768], F32), ("Pa", [128, 768], F32),
                                     ("Pta", [128, 768], F32), ("Rbf", [128, 768], BF16), ("AqT", [128, 768], BF16), ("AqkT", [128, 768], BF16),
                                     ("AakT", [128, 768], BF16), ("Kez", [128, 768], BF16), ("Vz", [128, 768], BF16), ("Vtm", [128, 384], BF16),
                                     ("Xb", [128, 384], BF16), ("Ubz", [128, 768], BF16), ("WtT", [128, 384], BF16), ("Utb", [128, 384], F32),
                                     ("qbar", [128, 384], BF16), ("Bhc0", [128, 384], BF16), ("Bhc1", [128, 384], BF16),
                                     ("Khc0", [128, 384], BF16), ("Khc1", [128, 384], BF16), ("BKtm", [128, 256], F32)]:
                    d[nm] = sb(f"r_{nm}{pb}", shp, dtp)
                B.append(d)
            sc.dma(wst[:], self.rww[:, l * 1152:(l + 1) * 1152], writes=["r_wst"])
            sc.dve(lambda e: e.tensor_copy(out=wbf[:], in_=wst[:]), ["r_wst"], ["r_wbf"])
            sc.dve(lambda e: e.memset(H32[:], 0.0), [], [("r_H32", h) for h in range(6)])
            sc.pool(lambda e: e.memset(Hbd[:], 0.0), [], [("r_Hbd", h) for h in range(6)])
            for i in range(3):
                for hp in range(2):
                    sc.pool(lambda e, i=i, hp=hp: e.memset(btz[i][hp][:], 0.0), [], [f"r_btz{i}"])
                    sc.pool(lambda e, i=i, hp=hp: e.memset(ktz[i][hp][:], 0.0), [], [f"r_ktz{i}"])
            for pb in range(2):
                for nm in ("Kez", "Vz", "Ubz"):
                    sc.pool(lambda e, pb=pb, nm=nm: e.memset(B[pb][nm][:], 0.0), [], [(f"r_{nm}{pb}", h) for h in range(6)])
            nblk = 0
            for sbi in range(S // T):
                t0 = sbi * T
                dests = rf + kraw + vf + [z9, zg]
                dtoks = [f"r_rf{i}" for i in range(3)] + [f"r_kraw{i}" for i in range(3)] + [f"r_vf{i}" for i in range(3)] + ["r_z9", "r_zg"]
                for rc in range(11):
                    xi, xt = xin[rc % 2], f"r_xin{rc % 2}"
                    if sbi == 0:
                        sc.pool(lambda e, xi=xi: e.memset(xi[:, 0:1], 0.0), [], [xt])
                        sc.dma(xi[:, 1:513], self.pT[rc * 128:(rc + 1) * 128, 0:T], reads=[xt], writes=[xt])
                    else:
                        sc.dma(xi[:], self.pT[rc * 128:(rc + 1) * 128, t0 - 1:t0 + T], writes=[xt])
                    sc.dve(lambda e, xi=xi: e.tensor_tensor(out=dtmp[:], in0=xi[:, 0:512], in1=xi[:, 1:513], op=ALU.subtract), [xt], ["r_dtmp"])
                    dst = dests[rc]
                    sc.dve(lambda e, xi=xi, dst=dst, rc=rc: e.scalar_tensor_tensor(out=dst[:], in0=dtmp[:], scalar=rs_[:, MU + rc:MU + rc + 1], in1=xi[:, 1:513],
                                                                            op0=ALU.mult, op1=ALU.add), ["r_dtmp", xt, "rws"], [dtoks[rc]])
                if self.stage < 2:
                    continue
                sc.act(lambda e: e.activation(out=act9[0:64, :], in_=z9[0:64, :], func=AF.Tanh), ["r_z9"], ["r_act9"])
                sc.act(lambda e: e.copy(out=act9[64:128, :], in_=z9[64:128, :]), ["r_z9"], ["r_act9"])
                sc.act(lambda e: e.activation(out=sigzg[:], in_=zg[:], func=AF.Sigmoid), ["r_zg"], ["r_sigzg"])
                for c in range(3):
                    cc = slice(c * 128, (c + 1) * 128)
                    p, ptok = self.psum()
                    sc.pe(lambda e, p=p, cc=cc: e.matmul(p[:], lhsT=wbf[:, cc], rhs=act9[:], start=True, stop=True), ["r_wbf", "r_act9"], [ptok])
                    sc.act(lambda e, p=p, c=c: e.activation(out=gc[c][:], in_=p[:], func=AF.Sigmoid, bias=rs_[:, W0 + c:W0 + c + 1]), [ptok, "rws"], ["r_gc"])
                    sc.dve(lambda e, c=c: e.tensor_scalar(out=tA[:], in0=gc[c][:], scalar1=-0.6065306597126334, scalar2=None, op0=ALU.mult), ["r_gc"], ["r_tA"])
                    p2, p2tok = self.psum()
                    sc.pe(lambda e, p2=p2, c=c: e.matmul(p2[:], lhsT=wbf[:, 384 + c * 128:384 + (c + 1) * 128], rhs=act9[:], start=True, stop=True), ["r_wbf", "r_act9"], [p2tok])
                    sc.act(lambda e, p2=p2, c=c: e.activation(out=af[c][:], in_=p2[:], func=AF.Sigmoid, bias=rs_[:, A0 + c:A0 + c + 1]), [p2tok, "rws"], ["r_af"])
                    p3, p3tok = self.psum()
                    sc.pe(lambda e, p3=p3, c=c: e.matmul(p3[:], lhsT=wbf[:, 768 + c * 128:768 + (c + 1) * 128], rhs=sigzg[:], start=True, stop=True), ["r_wbf", "r_sigzg"], [p3tok])
                    sc.act(lambda e, p3=p3, c=c: e.copy(out=gf_[c][:], in_=p3[:]), [p3tok], [f"r_gf{c}"])
                    sc.dve(lambda e, c=c: e.tensor_tensor_scan(out=gc[c][:], data0=segm[:], data1=tA[:], initial=0.0, op0=ALU.mult, op1=ALU.add),
                           ["r_tA", "segm", "r_gc"], ["r_gc"])
                    sc.act(lambda e, c=c: e.activation(out=epos[c][:], in_=gc[c][:], func=AF.Exp), ["r_gc"], [f"r_epos{c}"])
                    sc.act(lambda e, c=c: e.activation(out=eneg[c][:], in_=gc[c][:], func=AF.Exp, scale=-1.0), ["r_gc"], ["r_eneg"])
                    sc.dve(lambda e, c=c: e.tensor_tensor(out=tB[:], in0=gc[c][:], in1=tA[:], op=ALU.subtract), ["r_gc", "r_tA"], ["r_tB"])
                    sc.act(lambda e: e.activation(out=tB[:], in_=tB[:], func=AF.Exp), ["r_tB"], ["r_tB"])
                    sc.dve(lambda e, c=c: e.tensor_scalar(out=kk[c][:], in0=kraw[c][:], scalar1=rs_[:, KK_ + c:KK_ + c + 1], scalar2=None, op0=ALU.mult),
                           [f"r_kraw{c}", "rws"], ["r_kk"])
                    sc.pool(lambda e, c=c: e.tensor_tensor(out=sqt[:], in0=kk[c][:], in1=kk[c][:], op=ALU.mult), ["r_kk"], ["r_sq"])
                    p4, p4tok = self.psum()
                    sc.pe(lambda e, p4=p4: e.matmul(p4[:], lhsT=self.onesbd_bf[:], rhs=sqt[:], start=True, stop=True), ["r_sq", "onesbd_bf"], [p4tok])
                    sc.act(lambda e, p4=p4: e.activation(out=tA[:], in_=p4[:], func=AF.Sqrt, bias=1e-6, scale=1.0), [p4tok, "r_tA"], ["r_tA"])
                    sc.dve(lambda e: e.reciprocal(out=tA[:], in_=tA[:]), ["r_tA"], ["r_tA"])
                    sc.dve(lambda e, c=c: e.tensor_tensor(out=kk[c][:], in0=kk[c][:], in1=tA[:], op=ALU.mult), ["r_kk", "r_tA"], ["r_kk"])
                    sc.dve(lambda e, c=c: e.tensor_tensor(out=ap32[c][:], in0=kk[c][:], in1=tB[:], op=ALU.mult), ["r_kk", "r_tB"], [f"r_ap32{c}"])
                    sc.pool(lambda e, c=c: e.tensor_copy(out=apb[c][:], in_=ap32[c][:]), [f"r_ap32{c}"], [f"r_apb{c}"])
                    sc.dve(lambda e, c=c: e.tensor_scalar(out=tA[:], in0=af[c][:], scalar1=-1.0, scalar2=None, op0=ALU.add), ["r_af", "r_tA"], ["r_tA"])
                    sc.dve(lambda e, c=c: e.tensor_scalar(out=tA[:], in0=tA[:], scalar1=rs_[:, KA + c:KA + c + 1], scalar2=None, op0=ALU.mult), ["rws", "r_tA"], ["r_tA"])
                    sc.dve(lambda e, c=c: e.scalar_tensor_tensor(out=kmod[c][:], in0=tA[:], scalar=1.0, in1=kraw[c][:], op0=ALU.add, op1=ALU.mult),
                           ["r_tA", f"r_kraw{c}"], ["r_kmod"])
                    sc.pool(lambda e, c=c: e.tensor_tensor(out=bvec[c][:], in0=kk[c][:], in1=af[c][:], op=ALU.mult), ["r_kk", "r_af"], ["r_bvec"])
                    sc.dve(lambda e, c=c: e.scalar_tensor_tensor(out=sqt[:], in0=rf[c][:], scalar=rs_[:, RK + c:RK + c + 1], in1=kmod[c][:], op0=ALU.mult, op1=ALU.mult),
                           [f"r_rf{c}", "rws", "r_kmod", "r_sq"], ["r_sq"])
                    p5, p5tok = self.psum()
                    sc.pe(lambda e, p5=p5: e.matmul(p5[:], lhsT=self.onesbd_bf[:], rhs=sqt[:], start=True, stop=True), ["r_sq", "onesbd_bf"], [p5tok])
                    sc.dve(lambda e, p5=p5, c=c: e.tensor_tensor(out=bonus[c][:], in0=p5[:], in1=vf[c][:], op=ALU.mult), [p5tok, f"r_vf{c}"], [f"r_bonus{c}"])
                    sc.pool(lambda e, c=c: e.tensor_tensor(out=rbar[c][:], in0=rf[c][:], in1=epos[c][:], op=ALU.mult), [f"r_rf{c}", f"r_epos{c}"], [f"r_rbar{c}"])
                    sc.dve(lambda e, c=c: e.tensor_tensor(out=btb[c][:], in0=bvec[c][:], in1=eneg[c][:], op=ALU.mult), ["r_bvec", "r_eneg"], [f"r_btb{c}"])
                    sc.dve(lambda e, c=c: e.tensor_tensor(out=ktb[c][:], in0=kmod[c][:], in1=eneg[c][:], op=ALU.mult), ["r_kmod", "r_eneg"], [f"r_ktb{c}"])
                    for hp in range(2):
                        hr = slice(hp * 64, hp * 64 + 64)
                        sc.pool(lambda e, c=c, hp=hp, hr=hr: e.tensor_copy(out=btz[c][hp][hr, :], in_=btb[c][hr, :]), [f"r_btb{c}"], [f"r_btz{c}"])
                        sc.pool(lambda e, c=c, hp=hp, hr=hr: e.tensor_copy(out=ktz[c][hp][hr, :], in_=ktb[c][hr, :]), [f"r_ktb{c}"], [f"r_ktz{c}"])
                    for j in range(8):
                        js = slice(j * 64, (j + 1) * 64)
                        eng = sc.dve if j % 2 == 0 else sc.pool
                        eng(lambda e, c=c, j=j, js=js: e.tensor_scalar(out=tA[:, js], in0=eneg[c][:, js], scalar1=epos[c][:, j * 64 + 63:j * 64 + 64], scalar2=None,
                                                                        op0=ALU.mult), ["r_eneg", f"r_epos{c}", "r_tA"], ["r_tA"])
                    sc.dve(lambda e, c=c: e.tensor_tensor(out=Bh32[c][:], in0=bvec[c][:], in1=tA[:], op=ALU.mult), ["r_bvec", "r_tA"], [f"r_Bh32{c}"])
                    sc.pool(lambda e, c=c: e.tensor_tensor(out=Kh32[c][:], in0=kmod[c][:], in1=tA[:], op=ALU.mult), ["r_kmod", "r_tA"], [f"r_Kh32{c}"])
                for n in range(4 if self.stage >= 3 else 0):
                    pb = nblk % 2
                    nblk += 1
                    b = B[pb]
                    tk = lambda nm, pb=pb: f"r_{nm}{pb}"
                    cs = slice(n * 128, (n + 1) * 128)
                    for rc in range(3):
                        pk, tkk = self.psum()
                        for q_, (src, stok) in enumerate([(ap32, "r_ap32"), (vf, "r_vf"), (Bh32, "r_Bh32"), (Kh32, "r_Kh32")]):
                            sc.pe(lambda e, pk=pk, q_=q_, src=src, rc=rc, cs=cs: e.matmul(pk[:, q_ * 128:(q_ + 1) * 128], lhsT=src[rc][:, cs], rhs=ident, start=True, stop=True),
                                  [f"{stok}{rc}", "C"], [tkk])
                        pc = slice(rc * 128, (rc + 1) * 128)
                        sc.act(lambda e, pk=pk, pc=pc, b=b: e.copy(out=b["Vtm"][:, pc], in_=pk[:, 128:256]), [tkk], [(tk("Vtm"), rc)])
                        for hp in range(2):
                            h = 2 * rc + hp
                            zc = slice(h * 128 + hp * 64, h * 128 + hp * 64 + 64)
                            hs = slice(h * 64, (h + 1) * 64)
                            sc.act(lambda e, pk=pk, zc=zc, hp=hp, b=b: e.copy(out=b["Kez"][:, zc], in_=pk[:, hp * 64:hp * 64 + 64]), [tkk], [(tk("Kez"), h)])
                            sc.act(lambda e, pk=pk, zc=zc, hp=hp, b=b: e.copy(out=b["Vz"][:, zc], in_=pk[:, 128 + hp * 64:128 + hp * 64 + 64]), [tkk], [(tk("Vz"), h)])
                        sc.act(lambda e, pk=pk, b=b: e.copy(out=b["BKtm"][:], in_=pk[:, 256:512]), [tkk], [tk("BKtm")])
                        for hp in range(2):
                            h = 2 * rc + hp
                            hs = slice(h * 64, (h + 1) * 64)
                            for c in range(2):
                                ind = C[:, 256 + 64 * c:257 + 64 * c]
                                sc.dve(lambda e, hs=hs, hp=hp, c=c, ind=ind, b=b: e.tensor_scalar(
                                    out=b[f"Bhc{c}"][:, hs], in0=b["BKtm"][:, hp * 64:hp * 64 + 64], scalar1=ind, scalar2=None, op0=ALU.mult),
                                    [tk("BKtm"), "C"], [(tk(f"Bhc{c}"), h)])
                                sc.dve(lambda e, hs=hs, hp=hp, c=c, ind=ind, b=b: e.tensor_scalar(
                                    out=b[f"Khc{c}"][:, hs], in0=b["BKtm"][:, 128 + hp * 64:128 + hp * 64 + 64], scalar1=ind, scalar2=None, op0=ALU.mult),
                                    [tk("BKtm"), "C"], [(tk(f"Khc{c}"), h)])
                    banks = [[self.psum(), self.psum()] for _ in range(2)]
                    def dst(k, h):
                        (pa, ta), (pb_, tb) = banks[k]
                        return (pa[:, h * 128:(h + 1) * 128], ta) if h < 4 else (pb_[:, (h - 4) * 128:(h - 3) * 128], tb)
                    for h in range(6):
                        rc, hp = h // 2, h % 2
                        o1, to1 = dst(0, h)
                        sc.pe(lambda e, o1=o1, rc=rc, hp=hp, cs=cs: e.matmul(o1, lhsT=btz[rc][hp][:, cs], rhs=apb[rc][:, cs], start=True, stop=True),
                              [f"r_btz{rc}", f"r_apb{rc}"], [to1])
                        o2, to2 = dst(1, h)
                        sc.pe(lambda e, o2=o2, rc=rc, hp=hp, cs=cs: e.matmul(o2, lhsT=ktz[rc][hp][:, cs], rhs=apb[rc][:, cs], start=True, stop=True),
                              [f"r_ktz{rc}", f"r_apb{rc}"], [to2])
                    for h in range(6):
                        hc = slice(h * 128, (h + 1) * 128)
                        o1, to1 = dst(0, h)
                        sc.dve(lambda e, b=b, hc=hc, o1=o1: e.tensor_tensor(out=b["Mm"][:, hc], in0=o1, in1=nmstrict, op=ALU.mult),
                               [to1, "C"], [(tk("Mm"), h)])
                        o2, to2 = dst(1, h)
                        sc.dve(lambda e, b=b, hc=hc, o2=o2: e.tensor_tensor(out=b["AakT"][:, hc], in0=o2, in1=nmstrict, op=ALU.mult),
                               [to2, "C"], [(tk("AakT"), h)])
                    banks = [[self.psum(), self.psum()] for _ in range(2)]
                    for h in range(6):
                        rc, hp = h // 2, h % 2
                        o1, to1 = dst(0, h)
                        sc.pe(lambda e, o1=o1, rc=rc, hp=hp, cs=cs: e.matmul(o1, lhsT=btz[rc][hp][:, cs], rhs=rbar[rc][:, cs], start=True, stop=True),
                              [f"r_btz{rc}", f"r_rbar{rc}"], [to1])
                        o2, to2 = dst(1, h)
                        sc.pe(lambda e, o2=o2, rc=rc, hp=hp, cs=cs: e.matmul(o2, lhsT=ktz[rc][hp][:, cs], rhs=rbar[rc][:, cs], start=True, stop=True),
                              [f"r_ktz{rc}", f"r_rbar{rc}"], [to2])
                    for h in range(6):
                        hc = slice(h * 128, (h + 1) * 128)
                        o1, to1 = dst(0, h)
                        sc.dve(lambda e, b=b, hc=hc, o1=o1: e.tensor_tensor(out=b["AqT"][:, hc], in0=o1, in1=mincl, op=ALU.mult), [to1, "C"], [(tk("AqT"), h)])
                        o2, to2 = dst(1, h)
                        sc.dve(lambda e, b=b, hc=hc, o2=o2: e.tensor_tensor(out=b["AqkT"][:, hc], in0=o2, in1=mincl, op=ALU.mult), [to2, "C"], [(tk("AqkT"), h)])
                    if self.stage < 4:
                        continue
                    self.neumann(b, tk, ident, ident6)
                    sc.pool(lambda e, b=b: e.tensor_copy(out=b["Rbf"][:], in_=b["R"][:]), [tk("R")], [tk("Rbf")])
                    pW, tW = self.psum()
                    pX, tX = self.psum()
                    for h in range(6):
                        rc, hp = h // 2, h % 2
                        hs = slice(h * 64, (h + 1) * 64); hc = slice(h * 128, (h + 1) * 128)
                        sc.pe(lambda e, pW=pW, b=b, rc=rc, hp=hp, hc=hc: e.matmul(
                            pW[:, rc * 128:(rc + 1) * 128], lhsT=b["Kez"][:, hc], rhs=b["Rbf"][:, hc], start=(hp == 0), stop=(hp == 1)),
                            [(tk("Kez"), h), tk("Rbf")], [tW])
                        sc.pe(lambda e, pX=pX, b=b, hs=hs, hc=hc: e.matmul(pX[:, hs], lhsT=b["AakT"][:, hc], rhs=b["Vtm"][:, hs], start=True, stop=True),
                              [(tk("AakT"), h), (tk("Vtm"), h // 2)], [tX])
                    sc.act(lambda e, pW=pW, b=b: e.mul(out=b["WtT"][:], in_=pW[:, 0:384], mul=-1.0), [tW], [tk("WtT")])
                    sc.act(lambda e, pX=pX, b=b: e.copy(out=b["Xb"][:], in_=pX[:, 0:384]), [tX], [tk("Xb")])
                    pU, tU = self.psum()
                    for h in range(6):
                        hs = slice(h * 64, (h + 1) * 64); hc = slice(h * 128, (h + 1) * 128)
                        sc.pe(lambda e, pU=pU, b=b, hs=hs, hc=hc: e.matmul(pU[:, hs], lhsT=b["Rbf"][:, hc], rhs=b["Xb"][:, hs], start=True, stop=True),
                              [tk("Rbf"), tk("Xb")], [tU])
                    sc.act(lambda e, pU=pU, b=b: e.copy(out=b["Utb"][:], in_=pU[:, 0:384]), [tU], [tk("Utb")])
                    for rc in range(3):
                        sc.pool(lambda e, b=b, rc=rc, cs=cs: e.tensor_copy(out=b["qbar"][:, rc * 128:(rc + 1) * 128], in_=rbar[rc][:, cs]), [f"r_rbar{rc}"], [tk("qbar")])
                    gl_ap = lambda h, c, n=n: epos[h // 2][(h % 2) * 64:(h % 2) * 64 + 64, n * 128 + c * 64 + 63:n * 128 + c * 64 + 64]
                    if self.stage < 5:
                        continue
                    self.chunk_loop(b, tk, n, psO, H32, Hbd, "r", gl_ap, [f"r_epos{i}" for i in range(3)], beta=None, extra=True)
                for rc in range(3 if self.stage >= 6 else 0):
                    po, pot = psO[rc]
                    sc.act(lambda e, po=po: e.copy(out=tA[:], in_=po[:]), [pot, "r_tA"], ["r_tA"])
                    p, ptok = self.psum()
                    sc.pe(lambda e, p=p: e.matmul(p[:], lhsT=onesbd, rhs=tA[:], start=True, stop=True), ["r_tA", "C"], [ptok])
                    sc.dve(lambda e, p=p: e.scalar_tensor_tensor(out=tA[:], in0=p[:], scalar=-1.0 / 64, in1=tA[:], op0=ALU.mult, op1=ALU.add), [ptok, "r_tA"], ["r_tA"])
                    sc.pool(lambda e: e.tensor_tensor(out=tB[:], in0=tA[:], in1=tA[:], op=ALU.mult), ["r_tA", "r_tB"], ["r_tB"])
                    p2, p2tok = self.psum()
                    sc.pe(lambda e, p2=p2: e.matmul(p2[:], lhsT=onesbd, rhs=tB[:], start=True, stop=True), ["r_tB", "C"], [p2tok])
                    sc.act(lambda e, p2=p2: e.activation(out=tB[:], in_=p2[:], func=AF.Sqrt, bias=64e-5, scale=1.0 / 64), [p2tok, "r_tB"], ["r_tB"])
                    sc.dve(lambda e: e.reciprocal(out=tB[:], in_=tB[:]), ["r_tB"], ["r_tB"])
                    sc.dve(lambda e: e.tensor_tensor(out=tA[:], in0=tA[:], in1=tB[:], op=ALU.mult), ["r_tA", "r_tB"], ["r_tA"])
                    sc.dve(lambda e, rc=rc: e.tensor_scalar(out=tA[:], in0=tA[:], scalar1=rs_[:, GG + rc:GG + rc + 1], scalar2=None, op0=ALU.mult), ["r_tA", "rws"], ["r_tA"])
                    sc.dve(lambda e, rc=rc: e.tensor_scalar(out=tA[:], in0=tA[:], scalar1=rs_[:, GB + rc:GB + rc + 1], scalar2=None, op0=ALU.add), ["r_tA", "rws"], ["r_tA"])
                    sc.dve(lambda e, rc=rc: e.tensor_tensor(out=tA[:], in0=tA[:], in1=bonus[rc][:], op=ALU.add), ["r_tA", f"r_bonus{rc}"], ["r_tA"])
                    sc.dve(lambda e, rc=rc: e.tensor_tensor(out=sqt[:], in0=tA[:], in1=gf_[rc][:], op=ALU.mult), ["r_tA", f"r_gf{rc}", "r_sq"], ["r_sq"])
                    sc.dma(self.yT[rc * 128:(rc + 1) * 128, t0:t0 + T], sqt[:], reads=["r_sq"], writes=[("yT", rc, sbi)], q="pool")
            sc.flush()
        self.ps_n = 8

    def chunk_loop(self, b, tk, n, psO, H32, Hbd, pf, gl_ap, gl_toks, beta=None, extra=False):
        sc = self.sc
        for c in range(2):
            tr = slice(c * 64, c * 64 + 64)
            pSU, tSU = self.psum()
            for rc in range(3):
                pc = slice(rc * 128, (rc + 1) * 128)
                sc.pe(lambda e, pSU=pSU, pc=pc: e.matmul(pSU[:, pc], lhsT=b["WtT"][:, pc], rhs=Hbd[:, pc], start=True, stop=True),
                      [tk("WtT"), (f"{pf}_Hbd", 2 * rc), (f"{pf}_Hbd", 2 * rc + 1)], [tSU])
            for h in range(6):
                rc, hp = h // 2, h % 2
                hs = slice(h * 64, (h + 1) * 64)
                src = pSU[tr, rc * 128 + hp * 64:rc * 128 + hp * 64 + 64]
                dst = b["Ubz"][tr, h * 128 + hp * 64:h * 128 + hp * 64 + 64]
                if beta is not None:
                    sc.dve(lambda e, src=src, dst=dst, h=h, hs=hs, tr=tr: e.scalar_tensor_tensor(
                        out=dst, in0=src, scalar=beta[tr, n, h:h + 1], in1=b["Utb"][tr, hs], op0=ALU.mult, op1=ALU.add),
                        [tSU, f"{pf}_beta", tk("Utb")], [(tk("Ubz"), h)])
                else:
                    sc.dve(lambda e, src=src, dst=dst, hs=hs, tr=tr: e.tensor_tensor(out=dst, in0=src, in1=b["Utb"][tr, hs], op=ALU.add),
                           [tSU, tk("Utb")], [(tk("Ubz"), h)])
            pSH, tSH = self.psum()
            for rc in range(3):
                pc = slice(rc * 128, (rc + 1) * 128)
                po, pot = psO[rc]
                oc = slice(n * 128 + c * 64, n * 128 + c * 64 + 64)
                qc = slice(rc * 128 + c * 64, rc * 128 + c * 64 + 64)
                hbt = [(f"{pf}_Hbd", 2 * rc), (f"{pf}_Hbd", 2 * rc + 1)]
                sc.pe(lambda e, po=po, pc=pc, oc=oc, qc=qc: e.matmul(po[:, oc], lhsT=Hbd[:, pc], rhs=b["qbar"][:, qc], start=True, stop=False),
                      hbt + [tk("qbar")], [pot])
                for hp in range(2):
                    h = 2 * rc + hp
                    hc = slice(h * 128, (h + 1) * 128)
                    ac = slice(h * 128 + c * 64, h * 128 + c * 64 + 64)
                    last = (hp == 1) and not extra
                    sc.pe(lambda e, po=po, hc=hc, oc=oc, ac=ac, last=last: e.matmul(po[:, oc], lhsT=b["Ubz"][:, hc], rhs=b["AqT"][:, ac], start=False, stop=last),
                          [(tk("Ubz"), h), (tk("AqT"), h)], [pot])
                    if extra:
                        sc.pe(lambda e, po=po, hc=hc, oc=oc, ac=ac, hp=hp: e.matmul(po[:, oc], lhsT=b["Vz"][:, hc], rhs=b["AqkT"][:, ac], start=False, stop=(hp == 1)),
                              [(tk("Vz"), h), (tk("AqkT"), h)], [pot])
                nmm = 4 if extra else 2
                k = 0
                for hp in range(2):
                    h = 2 * rc + hp
                    hc = slice(h * 128, (h + 1) * 128)
                    sc.pe(lambda e, pSH=pSH, pc=pc, hc=hc, c=c, k=k, nmm=nmm: e.matmul(pSH[:, pc], lhsT=b[f"Bhc{c}"][:, pc], rhs=b["Ubz"][:, hc], start=(k == 0), stop=(k == nmm - 1)),
                          [(tk(f"Bhc{c}"), h), (tk(f"Bhc{c}"), h ^ 1), (tk("Ubz"), h)], [tSH])
                    k += 1
                    if extra:
                        sc.pe(lambda e, pSH=pSH, pc=pc, hc=hc, c=c, k=k, nmm=nmm: e.matmul(pSH[:, pc], lhsT=b[f"Khc{c}"][:, pc], rhs=b["Vz"][:, hc], start=False, stop=(k == nmm - 1)),
                              [(tk(f"Khc{c}"), h), (tk(f"Khc{c}"), h ^ 1), (tk("Vz"), h)], [tSH])
                        k += 1
            for h in range(6):
                rc, hp = h // 2, h % 2
                rows = slice(hp * 64, hp * 64 + 64)
                cols = slice(rc * 128 + hp * 64, rc * 128 + hp * 64 + 64)
                gl = gl_ap(h, c)
                sc.dve(lambda e, pSH=pSH, rows=rows, cols=cols, gl=gl: e.scalar_tensor_tensor(
                    out=H32[rows, cols], in0=H32[rows, cols], scalar=gl, in1=pSH[rows, cols], op0=ALU.mult, op1=ALU.add),
                    [tSH, (f"{pf}_H32", h)] + list(gl_toks), [(f"{pf}_H32", h)])
                sc.act(lambda e, rows=rows, cols=cols: e.copy(out=Hbd[rows, cols], in_=H32[rows, cols]), [(f"{pf}_H32", h)], [(f"{pf}_Hbd", h)])

    def neumann(self, b, tk, ident, ident6):
        sc = self.sc
        allM = [(tk("Mm"), h) for h in range(6)]
        pa, ta = self.psum(); pb_, tb = self.psum()
        for h in range(6):
            o = pa[:, h * 128:(h + 1) * 128] if h < 4 else pb_[:, (h - 4) * 128:(h - 3) * 128]
            sc.pe(lambda e, o=o, h=h: e.matmul(o, lhsT=b["Mm"][:, h * 128:(h + 1) * 128], rhs=ident, start=True, stop=True), [(tk("Mm"), h), "C"], [ta if h < 4 else tb])
        sc.act(lambda e, pa=pa: e.copy(out=b["Mt"][:, 0:512], in_=pa[:, 0:512]), [ta], [tk("Mt")])
        sc.act(lambda e, pb_=pb_: e.copy(out=b["Mt"][:, 512:768], in_=pb_[:, 0:256]), [tb], [tk("Mt")])
        sc.pool(lambda e: e.tensor_tensor(out=b["R"][:], in0=b["Mm"][:], in1=ident6, op=ALU.add), allM + ["C"], [tk("R")])
        P, Pt, Ptok, Pttok = b["Mm"], b["Mt"], allM, [tk("Mt")]
        for lev in range(5):
            last = lev == 4
            pa, ta = self.psum(); pb_, tb = self.psum()
            for h in range(6):
                hc = slice(h * 128, (h + 1) * 128)
                o = pa[:, hc] if h < 4 else pb_[:, (h - 4) * 128:(h - 3) * 128]
                sc.pe(lambda e, o=o, hc=hc, P=P, Pt=Pt: e.matmul(o, lhsT=P[:, hc], rhs=Pt[:, hc], start=True, stop=True),
                      list(Ptok) + list(Pttok), [ta if h < 4 else tb])
            n2t = b["Pta"] if lev % 2 == 0 else b["Mt"]
            n2ttok = tk("Pta") if lev % 2 == 0 else tk("Mt")
            sc.act(lambda e, pa=pa, n2t=n2t: e.copy(out=n2t[:, 0:512], in_=pa[:, 0:512]), [ta], [n2ttok])
            sc.act(lambda e, pb_=pb_, n2t=n2t: e.copy(out=n2t[:, 512:768], in_=pb_[:, 0:256]), [tb], [n2ttok])
            if not last:
                pc, tc = self.psum(); pd, td = self.psum()
                for h in range(6):
                    hc = slice(h * 128, (h + 1) * 128)
                    o = pc[:, hc] if h < 4 else pd[:, (h - 4) * 128:(h - 3) * 128]
                    sc.pe(lambda e, o=o, hc=hc, P=P, Pt=Pt: e.matmul(o, lhsT=Pt[:, hc], rhs=P[:, hc], start=True, stop=True),
                          list(Ptok) + list(Pttok), [tc if h < 4 else td])
                n2 = b["Pa"] if lev % 2 == 0 else b["Mm"]
                n2tok = tk("Pa") if lev % 2 == 0 else tk("Mm_all")
            pe_, te = self.psum(); pf, tf = self.psum()
            for h in range(6):
                hc = slice(h * 128, (h + 1) * 128)
                o = pe_[:, hc] if h < 4 else pf[:, (h - 4) * 128:(h - 3) * 128]
                sc.pe(lambda e, o=o, hc=hc, n2t=n2t: e.matmul(o, lhsT=n2t[:, hc], rhs=b["R"][:, hc], start=True, stop=True),
                      [n2ttok, tk("R")], [te if h < 4 else tf])
            if not last:
                sc.act(lambda e, pc=pc, n2=n2: e.copy(out=n2[:, 0:512], in_=pc[:, 0:512]), [tc] + (list(Ptok) if n2 is P else []), [n2tok] + (list(Ptok) if n2 is P else []))
                sc.act(lambda e, pd=pd, n2=n2: e.copy(out=n2[:, 512:768], in_=pd[:, 0:256]), [td], [n2tok] + (list(Ptok) if n2 is P else []))
            sc.dve(lambda e, pe_=pe_: e.tensor_tensor(out=b["R"][:, 0:512], in0=b["R"][:, 0:512], in1=pe_[:, 0:512], op=ALU.add), [te, tk("R")], [tk("R")])
            sc.dve(lambda e, pf=pf: e.tensor_tensor(out=b["R"][:, 512:768], in0=b["R"][:, 512:768], in1=pf[:, 0:256], op=ALU.add), [tf, tk("R")], [tk("R")])
            if not last:
                P, Pt = n2, n2t
                Ptok, Pttok = [n2tok], [n2ttok]


    def build(self, mixers=True):
        self.consts()
        h_src = self.xT
        for l in range(self.L):
            self.phase_a(l, h_src)
            if "pT" in self.dbg:
                self.sc.dma(self.dbg_out["pT"], self.pT, reads=[], writes=[])
                self.sc.flush()
            self.phase_b(l)
            if "yT" in self.dbg:
                self.sc.dma(self.dbg_out["yT"], self.yT, reads=[], writes=[])
                self.sc.flush()
            h_dst = self.oT if l == self.L - 1 else self.hT[l % 2]
            self.phase_c(l, h_src, h_dst)
            h_src = h_dst
        return self.nc

    def phase_b(self, l):
        if len(self.mix) < 3:
            self.phase_stub(l)
        if "gdn" in self.mix:
            self.phase_gdn(l)
        if "rwkv" in self.mix:
            self.phase_rwkv(l)
        if "mla" in self.mix:
            if l == 0:
                self.mla_tables()
            self.phase_mla(l)

    def phase_stub(self, l):
        nc, sc, S = self.nc, self.sc, self.S
        with contextlib.ExitStack() as st:
            z = st.enter_context(nc.sbuf_tensor(f"stZ_L{l}", [128, 8, 512], BF16))
            sc.pool(lambda e: e.memset(z[:], 0.0), [], ["stZ"])
            for t in range(S // 512):
                t0 = t * 512
                sc.dma(self.yT[:, t0:t0 + 512].rearrange("(k p) s -> p k s", p=128), z[:], reads=["stZ"])
            sc.flush()


def _consts():
    C = np.zeros((128, NCST), np.float32)
    i = np.arange(128)
    same = (i[:, None] // 64) == (i[None, :] // 64)
    incl = same & (i[:, None] <= i[None, :])
    strict = same & (i[:, None] < i[None, :])
    C[:, 0:128] = np.eye(128)
    C[:, 128:256] = 1.0
    C[:, 256:384] = same
    C[:, 384:512] = -1.0 * strict
    C[:, 512:640] = np.where(incl, 0.0, -30000.0)
    C[:, 640:768] = 1.0 - np.eye(128)
    C[:, 768:896] = strict
    C[:, 896:1024] = incl
    C[:, 1024:1792] = np.tile(1.0 - np.eye(128), (1, 6))
    C[:, 1792:2560] = np.tile(np.eye(128), (1, 6))
    return C


def rwkv_host(mu, w0, w2, a0, a2, g2, k_k, k_a, r_k, gn_g, gn_b):
    L = mu.shape[0]
    c3 = lambda v: v.reshape(L, 3, 128).transpose(2, 0, 1)
    rws = np.zeros((128, L, 32), np.float32)
    rws[:, :, 0:11] = mu.reshape(L, 11, 128).transpose(2, 0, 1)
    for off, v in ((11, w0), (14, a0), (17, k_k), (20, k_a), (23, r_k.reshape(L, 384)), (26, gn_g), (29, gn_b)):
        rws[:, :, off:off + 3] = c3(v)
    rww = np.zeros((128, L, 1152), np.float32)
    rww[0:64, :, 0:384] = w2.transpose(1, 0, 2)
    rww[64:128, :, 384:768] = a2.transpose(1, 0, 2)
    rww[:, :, 768:1152] = g2.transpose(1, 0, 2)
    segm = np.ones((128, 512), np.float32)
    segm[:, ::64] = 0.0
    return {"rws": np.ascontiguousarray(rws.reshape(128, L * 32)), "rww": np.ascontiguousarray(rww.reshape(128, L * 1152)), "segm": segm}


def mla_host(q_norm_g, kv_norm_g, q_qk_g, k_qk_g, positions_row, S):
    L = q_norm_g.shape[0]
    mls = np.zeros((128, L, 8), np.float32)
    mls[:, :, 0:2] = q_norm_g.reshape(L, 2, 128).transpose(2, 0, 1)
    mls[:, :, 2] = kv_norm_g.T
    perm = np.concatenate([np.arange(64), 64 + (np.arange(32) + 16) % 32])
    mls[0:96, :, 3] = q_qk_g.T
    mls[0:96, :, 4] = q_qk_g[:, perm].T
    mls[0:96, :, 5] = k_qk_g.T
    mls[0:96, :, 6] = k_qk_g[:, perm].T
    mlc = np.zeros((128, 104), np.float32)
    inv_freq = (10000.0 ** (-np.arange(0, 32, 2, dtype=np.float32) / 32)).astype(np.float32)
    mlc[64:96, 0] = np.tile(inv_freq, 2)
    mlc[0:32, 8 + 64:8 + 96] = np.eye(32, dtype=np.float32)
    kk = np.arange(128)[:, None]
    qo = np.arange(512)[None, :]
    mm = np.concatenate([((2 * j + (kk >= 64)) <= (qo // 64)).astype(np.float32) for j in range(4)], axis=1)
    pos96 = np.ascontiguousarray(np.broadcast_to(positions_row.astype(np.int32)[None, :], (96, S)))
    return {"mls": np.ascontiguousarray(mls.reshape(128, L * 8)), "mlc": mlc, "mmask": np.ascontiguousarray(mm), "pos96": pos96}


def kernel(**inputs):
    x = np.asarray(inputs["x"], np.float32)
    B, S, _ = x.shape
    L = int(inputs["w_in"].shape[0])
    f = lambda k: np.ascontiguousarray(np.asarray(inputs[k], np.float32))
    col8 = lambda g: np.ascontiguousarray(g.reshape(L, 8, 128).transpose(2, 0, 1).reshape(128, L * 8))
    conv_w, a_log, dtb, og = f("gdn_conv_w"), f("gdn_a_log"), f("gdn_dt_bias"), f("gdn_o_norm_g")
    gsm = np.zeros((128, L, 13), np.float32)
    gsm[:, :, 0:6] = dtb[None]
    gsm[:, :, 6:12] = a_log[None]
    gsm[:, :, 12] = np.tile(og, (1, 2)).T
    shared = {
        "w_in": f("w_in"), "w_out": f("w_out"), "w_ff1": f("w_ff1"), "w_ff2": f("w_ff2"),
        "g_mix": col8(f("ln_mix_g")), "g_ffn": col8(f("ln_ffn_g")), "cst": _consts(),
        "cw": np.ascontiguousarray(conv_w.reshape(L, 4, 9, 128).transpose(3, 0, 2, 1).reshape(128, L * 36)),
        "gsm": np.ascontiguousarray(gsm.reshape(128, L * 13)),
        "w_uq": f("mla_w_uq"), "w_ukv": f("mla_w_ukv"),
    }
    shared.update(rwkv_host(f("rwkv_mu"), f("rwkv_w0"), f("rwkv_w2"), f("rwkv_a0"), f("rwkv_a2"), f("rwkv_g2"), f("rwkv_k_k"),
                            f("rwkv_k_a"), f("rwkv_r_k"), f("rwkv_gn_g"), f("rwkv_gn_b")))
    positions = np.asarray(inputs["positions"]).astype(np.int32)
    prog = Prog(S, L)
    nc = prog.build()
    in_maps = []
    for b in range(B):
        m = dict(shared, xT=np.ascontiguousarray(x[b].T))
        m.update(mla_host(f("mla_q_norm_g"), f("mla_kv_norm_g"), f("mla_q_qk_g"), f("mla_k_qk_g"), positions[b], S))
        in_maps.append(m)
    res = run_bass_kernel_spmd(nc, in_maps, core_ids=list(range(B)))
    return np.stack([np.ascontiguousarray(r["oT"].T) for r in res.results], axis=0).astype(np.float32)
```

```python
import contextlib
import numpy as np
import concourse.bass as bass
import concourse.mybir as mybir
from concourse.bass_utils import run_bass_kernel_spmd

F32 = mybir.dt.float32
BF16 = mybir.dt.bfloat16
I32 = mybir.dt.int32
AF = mybir.ActivationFunctionType
ALU = mybir.AluOpType
AX = mybir.AxisListType

D = 1024
DFF = 4096
P_IN = 3372
P_EXT = P_IN + 32
NORM_EPS = 1e-6
NCST = 128 * 8 + 768 * 2

ENGS = ("pe", "act", "dve", "pool", "sp")
CENG = ("pe", "act", "dve", "pool")
BLK = 8192
NROT = 8
NDMA = 12


class Op:
    __slots__ = ("eng", "fn", "reads", "writes", "idx", "deps", "signal", "dma",
                 "k", "dslot", "dval")

    def __init__(self, eng, fn, reads, writes, dma):
        self.eng, self.fn, self.reads, self.writes, self.dma = eng, fn, reads, writes, dma
        self.deps = ()
        self.signal = False
        self.k = -1


class Sched:
    def __init__(self, nc):
        self.nc = nc
        self.ops = []
        self.last_w = {}
        self.readers = {}
        self.nsig = {e: 0 for e in ENGS}
        self.ndma = {e: 0 for e in ENGS}
        self.waited_c = {e: {} for e in ENGS}
        self.waited_d = {e: {} for e in ENGS}
        self.nbar = 0
        self.ntot = {e: 0 for e in ENGS}
        self.csem = {e: [nc.semaphore(f"c_{e}_{i}").__enter__() for i in range(NROT)] for e in CENG}
        self.dsem = {e: [nc.semaphore(f"d_{e}_{i}").__enter__() for i in range(NDMA)]
                     for e in ("sp", "pool")}
        self.bsem = nc.semaphore("bar").__enter__()
        self.cap = None

    def cap_begin(self):
        self.cap = []

    def cap_end(self):
        c, self.cap = self.cap, None
        return c

    def replay(self, a, b=()):
        na, nb = len(a), len(b)
        i = j = 0
        while i < na or j < nb:
            if j >= nb or (i < na and i * nb <= j * na):
                self.add(*a[i]); i += 1
            else:
                self.add(*b[j]); j += 1

    def add(self, eng, fn, reads=(), writes=(), dma=False):
        if self.cap is not None:
            self.cap.append((eng, fn, tuple(reads), tuple(writes), dma))
            return None
        op = Op(eng, fn, tuple(reads), tuple(writes), dma)
        if dma:
            op.signal = True
        op.idx = len(self.ops)
        deps = set()
        lw, rd = self.last_w, self.readers
        for r in op.reads:
            p = lw.get(r)
            if p is not None:
                deps.add(p)
        for w in op.writes:
            p = lw.get(w)
            if p is not None:
                deps.add(p)
            for q in rd.get(w, ()):
                deps.add(q)
        ops = self.ops
        keep = []
        for d in deps:
            p = ops[d]
            if p.eng == eng and not p.dma and not dma:
                if eng == "pe":
                    continue
            keep.append(d)
        op.deps = tuple(sorted(keep))
        for d in op.deps:
            ops[d].signal = True
        for r in op.reads:
            rd.setdefault(r, []).append(op.idx)
        for w in op.writes:
            lw[w] = op.idx
            rd[w] = []
        ops.append(op)
        return op

    def pe(self, fn, reads=(), writes=()):
        return self.add("pe", fn, reads, writes)

    def act(self, fn, reads=(), writes=()):
        return self.add("act", fn, reads, writes)

    def dve(self, fn, reads=(), writes=()):
        return self.add("dve", fn, reads, writes)

    def pool(self, fn, reads=(), writes=()):
        return self.add("pool", fn, reads, writes)

    def dma(self, out, in_, reads=(), writes=(), q="sp", **kw):
        return self.add(q, lambda e: e.dma_start(out=out, in_=in_, **kw), reads, writes, dma=True)

    def _csig(self, op):
        j = op.k // BLK
        return self.csem[op.eng][j % NROT], (j // NROT) * BLK + (op.k % BLK) + 1

    def flush(self):
        nc = self.nc
        ops = self.ops
        per = {e: [] for e in ENGS}
        for op in ops:
            per[op.eng].append(op)
        for e in CENG:
            for op in reversed(per[e]):
                if not op.dma:
                    op.signal = True
                    break
        for op in ops:
            if op.dma:
                n = self.ndma[op.eng]
                op.dslot = n % NDMA
                op.dval = 16 * (n // NDMA + 1)
                self.ndma[op.eng] = n + 1
            elif op.signal:
                op.k = self.nsig[op.eng]
                self.nsig[op.eng] += 1
        for e in ENGS:
            self.ntot[e] += len(per[e])
        self.nbar += 1
        nbar = self.nbar
        dsem, bsem = self.dsem, self.bsem

        def run(engname, e):
            waited_c = self.waited_c[engname]
            waited_d = self.waited_d[engname]

            def wait_for(p):
                if p.dma:
                    key = (p.eng, p.dslot)
                    if waited_d.get(key, 0) >= p.dval:
                        return
                    waited_d[key] = p.dval
                    e.wait_ge(dsem[p.eng][p.dslot], p.dval)
                else:
                    if waited_c.get(p.eng, -1) >= p.k:
                        return
                    waited_c[p.eng] = p.k
                    s, v = self._csig(p)
                    e.wait_ge(s, v)

            last_c = None
            for op in per[engname]:
                for d in op.deps:
                    wait_for(ops[d])
                if op.dma:
                    if op.dval > 16:
                        key = (op.eng, op.dslot)
                        if waited_d.get(key, 0) < op.dval - 16:
                            waited_d[key] = op.dval - 16
                            e.wait_ge(dsem[op.eng][op.dslot], op.dval - 16)
                    op.fn(e).then_inc(dsem[op.eng][op.dslot], 16)
                else:
                    ins = op.fn(e)
                    if op.signal:
                        s, v = self._csig(op)
                        ins.then_inc(s, 1)
                        last_c = op
            if last_c is not None:
                wait_for(last_c)
            lastd = {}
            for op in per[engname]:
                if op.dma:
                    lastd[op.dslot] = op
            for op in lastd.values():
                wait_for(op)
            e.sem_inc(bsem, 1)
            e.wait_ge(bsem, 5 * nbar)

        with nc.Block() as block:
            block.tensor(lambda e: run("pe", e))
            block.scalar(lambda e: run("act", e))
            block.vector(lambda e: run("dve", e))
            block.gpsimd(lambda e: run("pool", e))
            block.sync(lambda e: run("sp", e))
        self.ops = []
        self.last_w = {}
        self.readers = {}


IN_CHUNKS = ([(i * 128, 128) for i in range(14)] + [(1792, 32)] +
             [(1824 + i * 128, 128) for i in range(12)] + [(3360, 12), (3372, 32)])


class Prog:
    def __init__(self, S, L, dbg=(), mix=("gdn", "rwkv", "mla")):
        self.S, self.L, self.dbg = S, L, set(dbg)
        self.mix = set(mix)
        self.stage = 9
        self.sub = 9
        nc = self.nc = bass.Bass("TRN2", target_bir_lowering=False)
        self.sc = Sched(nc)
        dt = nc.dram_tensor
        self.xT = dt("xT", [D, S], F32, kind="ExternalInput").ap()
        self.w_in = dt("w_in", [L, D, P_IN], F32, kind="ExternalInput").ap()
        self.w_out = dt("w_out", [L, D, D], F32, kind="ExternalInput").ap()
        self.w_ff1 = dt("w_ff1", [L, D, DFF], F32, kind="ExternalInput").ap()
        self.w_ff2 = dt("w_ff2", [L, DFF, D], F32, kind="ExternalInput").ap()
        self.g_mix = dt("g_mix", [128, L * 8], F32, kind="ExternalInput").ap()
        self.g_ffn = dt("g_ffn", [128, L * 8], F32, kind="ExternalInput").ap()
        self.oT = dt("oT", [D, S], F32, kind="ExternalOutput").ap()
        self.cst = dt("cst", [128, NCST], F32, kind="ExternalInput").ap()
        self.cw_d = dt("cw", [128, L * 36], F32, kind="ExternalInput").ap()
        self.gsm_d = dt("gsm", [128, L * 13], F32, kind="ExternalInput").ap()
        self.baT = dt("baT", [S, 12], F32).ap()
        self.rws_d = dt("rws", [128, L * 32], F32, kind="ExternalInput").ap()
        self.rww = dt("rww", [128, L * 1152], F32, kind="ExternalInput").ap()
        self.segm_d = dt("segm", [128, 512], F32, kind="ExternalInput").ap()
        self.w_uq = dt("w_uq", [L, 256, 384], F32, kind="ExternalInput").ap()
        self.w_ukv = dt("w_ukv", [L, 128, 512], F32, kind="ExternalInput").ap()
        self.mls_d = dt("mls", [128, L * 8], F32, kind="ExternalInput").ap()
        self.mlc_d = dt("mlc", [128, 104], F32, kind="ExternalInput").ap()
        self.pos96 = dt("pos96", [96, S], I32, kind="ExternalInput").ap()
        self.mmask = dt("mmask", [128, 2048], F32, kind="ExternalInput").ap()
        self.cosT = dt("cosT", [96, S], F32).ap()
        self.sinT = dt("sinT", [96, S], F32).ap()
        self.qfT = dt("qfT", [4, 96, S], BF16).ap()
        self.kfT = dt("kfT", [4, 96, S], BF16).ap()
        self.vxT = dt("vxT", [4, S, 128], BF16).ap()
        self.pT = dt("pT", [P_EXT, S], F32).ap()
        self.yT = dt("yT", [D, S], BF16).ap()
        self.hT = [dt(f"hT{i}", [D, S], F32).ap() for i in range(2)]
        self.dbg_out = {}
        if "pT" in self.dbg:
            self.dbg_out["pT"] = dt("dbg_pT", [P_EXT, S], F32, kind="ExternalOutput").ap()
        if "yT" in self.dbg:
            self.dbg_out["yT"] = dt("dbg_yT", [D, S], BF16, kind="ExternalOutput").ap()
        self.ps = [nc.alloc_psum_tensor(f"ps{i}", [128, 512], F32) for i in range(8)]
        self.ps_i = 0
        self.ps_lo, self.ps_n = 0, 8
        self.ps_lo = 0

    def psum(self):
        i = self.ps_lo + self.ps_i % self.ps_n
        self.ps_i += 1
        return self.ps[i], ("ps", i)

    def consts(self):
        nc, sc = self.nc, self.sc
        L = self.L
        self.ones_bf = nc.alloc_sbuf_tensor("ones_bf", [128, 128], BF16)
        self.gm = nc.alloc_sbuf_tensor("gm", [128, L * 8], F32)
        self.gf = nc.alloc_sbuf_tensor("gf", [128, L * 8], F32)
        sc.pool(lambda e: e.memset(self.ones_bf[:], 1.0), [], ["ones_bf"])
        self.C = nc.alloc_sbuf_tensor("cstt", [128, NCST], F32)
        self.cw = nc.alloc_sbuf_tensor("cwt", [128, L * 36], F32)
        self.gsm = nc.alloc_sbuf_tensor("gsmt", [128, L * 13], F32)
        self.onesbd_bf = nc.alloc_sbuf_tensor("onesbd_bf", [128, 128], BF16)
        self.rws = nc.alloc_sbuf_tensor("rwst", [128, L * 32], F32)
        self.segm = nc.alloc_sbuf_tensor("segmt", [128, 512], F32)
        sc.dma(self.rws[:], self.rws_d, writes=["rws"])
        sc.dma(self.segm[:], self.segm_d, writes=["segm"])
        self.mls = nc.alloc_sbuf_tensor("mlst", [128, L * 8], F32)
        self.mlc = nc.alloc_sbuf_tensor("mlct", [128, 104], F32)
        sc.dma(self.mls[:], self.mls_d, writes=["mls"])
        sc.dma(self.mlc[:], self.mlc_d, writes=["mlc"])
        sc.dma(self.C[:], self.cst, writes=["C"])
        sc.dma(self.cw[:], self.cw_d, writes=["cw"])
        sc.dma(self.gsm[:], self.gsm_d, writes=["gsm"])
        sc.dve(lambda e: e.tensor_copy(out=self.onesbd_bf[:], in_=self.C[:, 256:384]), ["C"], ["onesbd_bf"])
        sc.dma(self.gm[:], self.g_mix, writes=["gm"])
        sc.dma(self.gf[:], self.g_ffn, writes=["gf"])
        sc.flush()

    def rms_stats(self, hx, hx_tok, sq, sq_tok, rs, rs_tok, T):
        sc = self.sc
        sc.act(lambda e: e.activation(out=sq[:, :, 0:T], in_=hx[:, :, 0:T], func=AF.Square), [hx_tok], [sq_tok])
        p, ptok = self.psum()
        for kc in range(8):
            sc.pe(lambda e, kc=kc: e.matmul(p[:, 0:T], lhsT=self.ones_bf[:], rhs=sq[:, kc, 0:T],
                                              start=(kc == 0), stop=(kc == 7)), [sq_tok, "ones_bf"], [ptok])
        sc.act(lambda e: e.activation(out=rs[:, 0:T], in_=p[:, 0:T], func=AF.Sqrt, bias=NORM_EPS, scale=1.0 / D),
               [ptok], [rs_tok])
        sc.dve(lambda e: e.reciprocal(out=rs[:, 0:T], in_=rs[:, 0:T]), [rs_tok], [rs_tok])

    def load_weight(self, dst, dst_name, src, KC, cols, gcol, gtok, stg, col_off=0):
        sc = self.sc
        n = 0
        CH = stg[0][0].shape[1]
        for kc in range(KC):
            for c0 in range(0, cols, CH):
                c1 = min(cols, c0 + CH)
                st, sttok = stg[n % len(stg)]
                n += 1
                sc.dma(st[:, 0:c1 - c0], src[kc * 128:(kc + 1) * 128, c0:c1], writes=[sttok])
                if n % 3 == 0:
                    if gcol is not None:
                        sc.act(lambda e, st=st, kc=kc, c0=c0, c1=c1: e.mul(out=dst[:, kc, col_off + c0:col_off + c1], in_=st[:, 0:c1 - c0],
                                                                           mul=gcol[:, kc:kc + 1]), [sttok, gtok], [(dst_name, kc)])
                    else:
                        sc.act(lambda e, st=st, kc=kc, c0=c0, c1=c1: e.copy(out=dst[:, kc, col_off + c0:col_off + c1], in_=st[:, 0:c1 - c0]),
                               [sttok], [(dst_name, kc)])
                elif gcol is not None:
                    sc.dve(lambda e, st=st, kc=kc, c0=c0, c1=c1: e.tensor_scalar(
                        out=dst[:, kc, col_off + c0:col_off + c1], in0=st[:, 0:c1 - c0],
                        scalar1=gcol[:, kc:kc + 1], scalar2=None, op0=ALU.mult),
                        [sttok, gtok], [(dst_name, kc)])
                else:
                    sc.dve(lambda e, st=st, kc=kc, c0=c0, c1=c1: e.tensor_copy(
                        out=dst[:, kc, col_off + c0:col_off + c1], in_=st[:, 0:c1 - c0]),
                        [sttok], [(dst_name, kc)])

    def phase_a(self, l, h_src):
        nc, sc, S = self.nc, self.sc, self.S
        with contextlib.ExitStack() as st:
            sb = lambda n, s, d=F32: st.enter_context(nc.sbuf_tensor(f"{n}_L{l}", s, d))
            wi = sb("wi", [128, 8, P_EXT], BF16)
            stg = [(sb(f"stgA{i}", [128, 2048]), f"stgA{i}") for i in range(2)]
            hx = [sb(f"hxA{i}", [128, 8, 512]) for i in range(2)]
            sq = [sb(f"sqA{i}", [128, 8, 512], BF16) for i in range(2)]
            xb = [sb(f"xbA{i}", [128, 8, 512], BF16) for i in range(2)]
            rs = [sb(f"rsA{i}", [128, 512]) for i in range(2)]
            ev = [sb(f"evA{i}", [128, 512]) for i in range(4)]
            bat = [sb(f"batA{i}", [128, 64]) for i in range(2)]
            gcol = self.gm[:, l * 8:(l + 1) * 8]
            self.load_weight(wi, "wi", self.w_in[l], 8, P_IN, gcol, "gm", stg)
            allwi = [("wi", kc) for kc in range(8)]
            sc.dve(lambda e: e.tensor_scalar(out=wi[:, :, 3372:3388], in0=wi[:, :, 1808:1824], scalar1=-1.0,
                                              scalar2=None, op0=ALU.mult), allwi, allwi)
            sc.dve(lambda e: e.tensor_copy(out=wi[:, :, 3388:3404], in_=wi[:, :, 1792:1808]), allwi, allwi)
            nev = 0
            for t in range(S // 512):
                b = t % 2
                t0 = t * 512
                sc.dma(hx[b][:], h_src[:, t0:t0 + 512].rearrange("(k p) s -> p k s", p=128), writes=[f"hxA{b}"])
                self.rms_stats(hx[b], f"hxA{b}", sq[b], f"sqA{b}", rs[b], f"rsA{b}", 512)
                for kc in range(8):
                    eng = sc.dve if kc % 2 == 0 else sc.pool
                    eng(lambda e, kc=kc, b=b: e.tensor_tensor(out=xb[b][:, kc, :], in0=hx[b][:, kc, :], in1=rs[b][:],
                                                               op=ALU.mult), [f"hxA{b}", f"rsA{b}"], [(f"xbA{b}", kc)])
                p, ptok = self.psum()
                for n in range(4):
                    for kc in range(8):
                        sc.pe(lambda e, p=p, kc=kc, n=n, b=b: e.matmul(
                            p[:, n * 16:n * 16 + 12], lhsT=xb[b][:, kc, n * 128:(n + 1) * 128], rhs=wi[:, kc, 3360:3372],
                            start=(kc == 0), stop=(kc == 7)), [("wi", kc), (f"xbA{b}", kc)], [ptok])
                sc.dve(lambda e, p=p, b=b: e.tensor_copy(out=bat[b][:].rearrange("p (n c) -> p n c", c=16)[:, :, 0:12], in_=p[:, 0:64].rearrange("p (n c) -> p n c", c=16)[:, :, 0:12]), [ptok], [f"batA{b}"])
                sc.dma(self.baT[t0:t0 + 512, :].rearrange("(n p) c -> p n c", p=128),
                       bat[b][:].rearrange("p (n c) -> p n c", c=16)[:, :, 0:12], reads=[f"batA{b}"], writes=[("baT", t)], q="pool")
                for (c0, m) in IN_CHUNKS:
                    p, ptok = self.psum()
                    for kc in range(8):
                        sc.pe(lambda e, p=p, kc=kc, c0=c0, m=m, b=b: e.matmul(
                            p[0:m, :], lhsT=wi[:, kc, c0:c0 + m], rhs=xb[b][:, kc, :], start=(kc == 0), stop=(kc == 7)),
                            [("wi", kc), (f"xbA{b}", kc)], [ptok])
                    e_i = nev % 4
                    evt = ev[e_i]
                    if nev % 2 == 0:
                        sc.dve(lambda e, p=p, m=m, evt=evt: e.tensor_copy(out=evt[0:m, :], in_=p[0:m, :]), [ptok], [f"evA{e_i}"])
                    else:
                        sc.act(lambda e, p=p, m=m, evt=evt: e.copy(out=evt[0:m, :], in_=p[0:m, :]), [ptok], [f"evA{e_i}"])
                    nev += 1
                    sc.dma(self.pT[c0:c0 + m, t0:t0 + 512], evt[0:m, :], reads=[f"evA{e_i}"], writes=[("pT", c0, t)], q="pool")
            sc.flush()

    def phase_c(self, l, h_src, h_dst):
        nc, sc, S = self.nc, self.sc, self.S
        T = 256
        with contextlib.ExitStack() as st:
            sb = lambda n, s, d=F32: st.enter_context(nc.sbuf_tensor(f"{n}_L{l}", s, d))
            wo = sb("wo", [128, 8, D], BF16)
            w1 = sb("w1", [128, 8, DFF], BF16)
            w2 = sb("w2", [128, 32, D], BF16)
            hid = sb("hid", [128, 32, T], BF16)
            hx = sb("hxC", [128, 8, T])
            sq = sb("sqC", [128, 8, T], BF16)
            xb = sb("xbC", [128, 8, T], BF16)
            yb = sb("ybC", [128, 8, T], BF16)
            rs = sb("rsC", [128, T])
            relu_t = [sb(f"reluC{i}", [128, T]) for i in range(2)]
            stg = [(sb(f"stgC{i}", [128, 1024]), f"stgC{i}") for i in range(2)]
            self.load_weight(wo, "wo", self.w_out[l], 8, D, None, None, stg)
            self.load_weight(w1, "w1", self.w_ff1[l], 8, DFF, self.gf[:, l * 8:(l + 1) * 8], "gf", stg)
            self.load_weight(w2, "w2", self.w_ff2[l], 32, D, None, None, stg)
            for t in range(S // T):
                t0 = t * T
                sc.dma(hx[:], h_src[:, t0:t0 + T].rearrange("(k p) s -> p k s", p=128), writes=["hxC"])
                sc.dma(yb[:], self.yT[:, t0:t0 + T].rearrange("(k p) s -> p k s", p=128), writes=["ybC"], q="pool")
                for oc in range(8):
                    p, ptok = self.psum()
                    for kc in range(8):
                        sc.pe(lambda e, p=p, kc=kc, oc=oc: e.matmul(
                            p[:, 0:T], lhsT=wo[:, kc, oc * 128:(oc + 1) * 128], rhs=yb[:, kc, :], start=(kc == 0), stop=(kc == 7)),
                            [("wo", kc), "ybC"], [ptok])
                    sc.dve(lambda e, p=p, oc=oc: e.tensor_tensor(out=hx[:, oc, :], in0=hx[:, oc, :], in1=p[:, 0:T], op=ALU.add),
                           [ptok, ("hxC", oc), "hxC"], [("hxC", oc)])
                hx_all = ["hxC"] + [("hxC", oc) for oc in range(8)]
                sc.act(lambda e: e.activation(out=sq[:], in_=hx[:], func=AF.Square), hx_all, ["sqC"])
                p, ptok = self.psum()
                for kc in range(8):
                    sc.pe(lambda e, p=p, kc=kc: e.matmul(p[:, 0:T], lhsT=self.ones_bf[:], rhs=sq[:, kc, :],
                                                          start=(kc == 0), stop=(kc == 7)), ["sqC", "ones_bf"], [ptok])
                sc.act(lambda e, p=p: e.activation(out=rs[:], in_=p[:, 0:T], func=AF.Sqrt, bias=NORM_EPS, scale=1.0 / D),
                       [ptok], ["rsC"])
                sc.dve(lambda e: e.reciprocal(out=rs[:], in_=rs[:]), ["rsC"], ["rsC"])
                for kc in range(8):
                    eng = sc.dve if kc % 2 == 0 else sc.pool
                    eng(lambda e, kc=kc: e.tensor_tensor(out=xb[:, kc, :], in0=hx[:, kc, :], in1=rs[:], op=ALU.mult),
                        hx_all + ["rsC"], [("xbC", kc)])
                for oc in range(32):
                    p, ptok = self.psum()
                    for kc in range(8):
                        sc.pe(lambda e, p=p, kc=kc, oc=oc: e.matmul(
                            p[:, 0:T], lhsT=w1[:, kc, oc * 128:(oc + 1) * 128], rhs=xb[:, kc, :], start=(kc == 0), stop=(kc == 7)),
                            [("w1", kc), ("xbC", kc)], [ptok])
                    rl, rltok = relu_t[oc % 2], f"reluC{oc % 2}"
                    sc.act(lambda e, p=p, rl=rl: e.activation(out=rl[:], in_=p[:, 0:T], func=AF.Relu), [ptok], [rltok])
                    sc.pool(lambda e, oc=oc, rl=rl: e.tensor_tensor(out=hid[:, oc, :], in0=rl[:], in1=rl[:], op=ALU.mult),
                            [rltok], [("hid", oc)])
                for oc in range(8):
                    p, ptok = self.psum()
                    for kc in range(32):
                        sc.pe(lambda e, p=p, kc=kc, oc=oc: e.matmul(
                            p[:, 0:T], lhsT=w2[:, kc, oc * 128:(oc + 1) * 128], rhs=hid[:, kc, :], start=(kc == 0), stop=(kc == 31)),
                            [("w2", kc), ("hid", kc)], [ptok])
                    sc.dve(lambda e, p=p, oc=oc: e.tensor_tensor(out=hx[:, oc, :], in0=hx[:, oc, :], in1=p[:, 0:T], op=ALU.add),
                           [ptok, ("hxC", oc), "hxC"], [("hxC", oc)])
                sc.dma(h_dst[:, t0:t0 + T].rearrange("(k p) s -> p k s", p=128), hx[:], reads=hx_all, writes=[("hdst", t)], q="pool")
            sc.flush()

    def phase_gdn(self, l):
        nc, sc, S = self.nc, self.sc, self.S
        C = self.C
        ident, ones, onesbd = C[:, 0:128], C[:, 128:256], C[:, 256:384]
        mneg, useg = C[:, 512:640], C[:, 896:1024]
        offd6, ident6 = C[:, 1024:1792], C[:, 1792:2560]
        self.ps_n = 5
        psO = [(self.ps[5 + i], f"psO{i}") for i in range(3)]
        cwl = self.cw[:, l * 36:(l + 1) * 36]
        gs = self.gsm[:, l * 13:(l + 1) * 13]
        with contextlib.ExitStack() as st:
            sb = lambda n, s, d=F32: st.enter_context(nc.sbuf_tensor(f"{n}_L{l}", s, d))
            xin = [sb(f"g_xin{i}", [128, 515]) for i in range(2)]
            acc = [sb(f"g_acc{i}", [128, 512]) for i in range(2)]
            qf32 = [sb(f"g_qf{i}", [128, 512]) for i in range(3)]
            kf32 = [sb(f"g_kf{i}", [128, 512]) for i in range(3)]
            vf32 = [sb(f"g_vf{i}", [128, 512]) for i in range(3)]
            qfb = [sb(f"g_qb{i}", [128, 512], BF16) for i in range(3)]
            kfb = [sb(f"g_kb{i}", [128, 512], BF16) for i in range(3)]
            sqt = sb("g_sq", [128, 512], BF16)
            rinv = sb("g_rinv", [128, 512])
            batm = sb("g_batm", [128, 4, 12])
            beta = sb("g_beta", [128, 4, 6]); nbeta = sb("g_nbeta", [128, 4, 6]); gtm = sb("g_gtm", [128, 4, 6])
            tmp46 = sb("g_tmp46", [128, 4, 6])
            nA = sb("g_nA", [128, 6])
            H32 = sb("g_H32", [128, 3 * 128]); Hbd = sb("g_Hbd", [128, 3 * 128], BF16)
            kz = [[sb(f"g_kz{i}_{hp}", [128, 512], BF16) for hp in range(2)] for i in range(3)]
            osb = sb("g_osb", [128, 512]); gate = sb("g_gate", [128, 512]); ybf = sb("g_ybf", [128, 512], BF16)
            B = []
            for pb in range(2):
                d = {}
                for nm, shp, dtp in [("gcc", [128, 6], F32), ("gct", [128, 6], F32), ("egc", [128, 6], F32), ("eend", [128, 6], F32),
                                     ("GU", [128, 768], F32), ("Egc", [128, 768], F32), ("dd", [128, 768], F32), ("DmT", [128, 768], F32),
                                     ("DmS", [128, 768], F32), ("Ktm", [128, 384], F32), ("Vtm", [128, 384], BF16),
                                     ("Kez", [128, 768], BF16), ("Bhc0", [128, 384], BF16), ("Bhc1", [128, 384], BF16),
                                     ("eendc0", [128, 6], F32), ("eendc1", [128, 6], F32), ("Mm", [128, 768], F32), ("Mt", [128, 768], F32),
                                     ("R", [128, 768], F32), ("Pa", [128, 768], F32), ("Pta", [128, 768], F32), ("Rbf", [128, 768], BF16),
                                     ("AqT", [128, 768], BF16), ("WtT", [128, 384], BF16), ("Utb", [128, 384], F32),
                                     ("qbar", [128, 384], BF16), ("Ubz", [128, 768], BF16)]:
                    d[nm] = sb(f"g_{nm}{pb}", shp, dtp)
                B.append(d)
            sc.act(lambda e: e.activation(out=nA[:], in_=gs[:, 6:12], func=AF.Exp), ["gsm"], ["g_nA"])
            sc.dve(lambda e: e.tensor_scalar(out=nA[:], in0=nA[:], scalar1=-1.0, scalar2=None, op0=ALU.mult), ["g_nA"], ["g_nA"])
            sc.dve(lambda e: e.memset(H32[:], 0.0), [], [("g_H32", h) for h in range(6)])
            sc.pool(lambda e: e.memset(Hbd[:], 0.0), [], [("g_Hbd", h) for h in range(6)])
            for i in range(3):
                for hp in range(2):
                    sc.pool(lambda e, i=i, hp=hp: e.memset(kz[i][hp][:], 0.0), [], [f"g_kz{i}"])
            for pb in range(2):
                sc.pool(lambda e, pb=pb: e.memset(B[pb]["Kez"][:], 0.0), [], [(f"g_Kez{pb}", h) for h in range(6)])
                sc.pool(lambda e, pb=pb: e.memset(B[pb]["Ubz"][:], 0.0), [], [(f"g_Ubz{pb}", h) for h in range(6)])
            nblk = 0
            pend_loop = []
            for sbi in range(S // 512):
                t0 = sbi * 512
                for rc in range(9):
                    xi = xin[rc % 2]; xt = f"g_xin{rc % 2}"
                    ac = acc[rc % 2]; at_ = f"g_acc{rc % 2}"
                    r0 = 1824 + rc * 128
                    if sbi == 0:
                        sc.pool(lambda e, xi=xi: e.memset(xi[:, 0:3], 0.0), [], [xt])
                        sc.dma(xi[:, 3:515], self.pT[r0:r0 + 128, 0:512], reads=[xt], writes=[xt])
                    else:
                        sc.dma(xi[:], self.pT[r0:r0 + 128, t0 - 3:t0 + 512], writes=[xt])
                    cb = rc * 4
                    sc.dve(lambda e, xi=xi, ac=ac, cb=cb: e.tensor_scalar(out=ac[:], in0=xi[:, 3:515], scalar1=cwl[:, cb + 3:cb + 4],
                                                                         scalar2=None, op0=ALU.mult), [xt, "cw"], [at_])
                    for j in range(3):
                        sc.dve(lambda e, xi=xi, ac=ac, cb=cb, j=j: e.scalar_tensor_tensor(
                            out=ac[:], in0=xi[:, j:j + 512], scalar=cwl[:, cb + j:cb + j + 1], in1=ac[:], op0=ALU.mult, op1=ALU.add),
                            [xt, "cw", at_], [at_])
                    kind, i3 = rc // 3, rc % 3
                    if kind == 2:
                        sc.act(lambda e, ac=ac, i3=i3: e.activation(out=vf32[i3][:], in_=ac[:], func=AF.Silu), [at_], [f"g_vf{i3}"])
                        continue
                    sc.act(lambda e, ac=ac: e.activation(out=ac[:], in_=ac[:], func=AF.Silu), [at_], [at_])
                    sc.act(lambda e, ac=ac: e.activation(out=sqt[:], in_=ac[:], func=AF.Square), [at_], ["g_sq"])
                    p, ptok = self.psum()
                    sc.pe(lambda e, p=p: e.matmul(p[:], lhsT=self.onesbd_bf[:], rhs=sqt[:], start=True, stop=True), ["g_sq", "onesbd_bf"], [ptok])
                    sc.act(lambda e, p=p: e.activation(out=rinv[:], in_=p[:], func=AF.Sqrt, bias=1e-6, scale=1.0), [ptok], ["g_rinv"])
                    sc.dve(lambda e: e.reciprocal(out=rinv[:], in_=rinv[:]), ["g_rinv"], ["g_rinv"])
                    if kind == 0:
                        sc.dve(lambda e, ac=ac, i3=i3: e.scalar_tensor_tensor(out=qf32[i3][:], in0=ac[:], scalar=0.125, in1=rinv[:],
                                                                              op0=ALU.mult, op1=ALU.mult), [at_, "g_rinv"], [f"g_qf{i3}"])
                        sc.act(lambda e, i3=i3: e.copy(out=qfb[i3][:], in_=qf32[i3][:]), [f"g_qf{i3}"], [f"g_qb{i3}"])
                    else:
                        sc.dve(lambda e, ac=ac, i3=i3: e.tensor_tensor(out=kf32[i3][:], in0=ac[:], in1=rinv[:], op=ALU.mult),
                               [at_, "g_rinv"], [f"g_kf{i3}"])
                        sc.act(lambda e, i3=i3: e.copy(out=kfb[i3][:], in_=kf32[i3][:]), [f"g_kf{i3}"], [f"g_kb{i3}"])
                        for hp in range(2):
                            sc.act(lambda e, i3=i3, hp=hp: e.copy(out=kz[i3][hp][hp * 64:hp * 64 + 64, :], in_=kf32[i3][hp * 64:hp * 64 + 64, :]),
                                    [f"g_kf{i3}"], [f"g_kz{i3}"])
                sc.dma(batm[:], self.baT[t0:t0 + 512, :].rearrange("(n p) c -> p n c", p=128), writes=["g_batm"])
                sc.act(lambda e: e.activation(out=beta[:], in_=batm[:, :, 0:6], func=AF.Sigmoid), ["g_batm"], ["g_beta"])
                sc.dve(lambda e: e.tensor_scalar(out=nbeta[:], in0=beta[:], scalar1=-1.0, scalar2=None, op0=ALU.mult), ["g_beta"], ["g_nbeta"])
                for n in range(4):
                    sc.dve(lambda e, n=n: e.tensor_tensor(out=tmp46[:, n, :], in0=batm[:, n, 6:12], in1=gs[:, 0:6], op=ALU.add),
                           ["g_batm", "gsm"], ["g_tmp46"])
                sc.act(lambda e: e.activation(out=tmp46[:], in_=tmp46[:], func=AF.Exp), ["g_tmp46"], ["g_tmp46"])
                sc.act(lambda e: e.activation(out=tmp46[:], in_=tmp46[:], func=AF.Ln, bias=1.0), ["g_tmp46"], ["g_tmp46"])
                for n in range(4):
                    sc.dve(lambda e, n=n: e.tensor_tensor(out=gtm[:, n, :], in0=tmp46[:, n, :], in1=nA[:], op=ALU.mult),
                           ["g_tmp46", "g_nA"], ["g_gtm"])
                for n in range(4 if self.stage >= 2 else 0):
                    pb = nblk % 2
                    nblk += 1
                    b = B[pb]
                    tk = lambda nm, pb=pb: f"g_{nm}{pb}"
                    cs = slice(n * 128, (n + 1) * 128)
                    g_n = gtm[:, n, :]
                    sc.cap_begin(); self.ps_lo, self.ps_n = 0, 4
                    p1, t1 = self.psum()
                    sc.pe(lambda e, p1=p1, g_n=g_n: e.matmul(p1[:, 0:6], lhsT=useg, rhs=g_n, start=True, stop=True), ["g_gtm", "C"], [t1])
                    sc.pe(lambda e, p1=p1, g_n=g_n: e.matmul(p1[:, 8:14], lhsT=onesbd, rhs=g_n, start=True, stop=True), ["g_gtm", "C"], [t1])
                    sc.dve(lambda e, p1=p1, b=b: e.tensor_copy(out=b["gcc"][:], in_=p1[:, 0:6]), [t1], [tk("gcc")])
                    sc.dve(lambda e, p1=p1, b=b: e.tensor_tensor(out=b["gct"][:], in0=p1[:, 8:14], in1=b["gcc"][:], op=ALU.subtract),
                           [t1, tk("gcc")], [tk("gct")])
                    sc.act(lambda e, b=b: e.activation(out=b["egc"][:], in_=b["gcc"][:], func=AF.Exp), [tk("gcc")], [tk("egc")])
                    sc.act(lambda e, b=b: e.activation(out=b["eend"][:], in_=b["gct"][:], func=AF.Exp), [tk("gct")], [tk("eend")])
                    for h in range(6):
                        eng = sc.dve
                        eng(lambda e, b=b, h=h, g_n=g_n: e.tensor_scalar(out=b["GU"][:, h * 128:(h + 1) * 128], in0=useg, scalar1=g_n[:, h:h + 1],
                                                                       scalar2=None, op0=ALU.mult), ["C", "g_gtm"], [(tk("GU"), h)])
                    p2, t2 = self.psum()
                    p3, t3 = self.psum()
                    allGU = [(tk("GU"), h) for h in range(6)]
                    sc.pe(lambda e, p2=p2, b=b: e.matmul(p2[:, 0:512], lhsT=ones, rhs=b["GU"][:, 0:512], start=True, stop=True), allGU + ["C"], [t2])
                    sc.pe(lambda e, p3=p3, b=b: e.matmul(p3[:, 0:256], lhsT=ones, rhs=b["GU"][:, 512:768], start=True, stop=True), allGU + ["C"], [t3])
                    sc.act(lambda e, p2=p2, b=b: e.activation(out=b["Egc"][:, 0:512], in_=p2[:, 0:512], func=AF.Exp), [t2], [tk("Egc")])
                    sc.act(lambda e, p3=p3, b=b: e.activation(out=b["Egc"][:, 512:768], in_=p3[:, 0:256], func=AF.Exp), [t3], [tk("Egc")])
                    for h in range(6):
                        src = p2[:, h * 128:(h + 1) * 128] if h < 4 else p3[:, (h - 4) * 128:(h - 3) * 128]
                        sc.dve(lambda e, b=b, h=h, src=src: e.scalar_tensor_tensor(
                            out=b["dd"][:, h * 128:(h + 1) * 128], in0=src, scalar=b["gcc"][:, h:h + 1], in1=mneg,
                            op0=ALU.subtract, op1=ALU.add), [t2, t3, tk("gcc"), "C"], [tk("dd")])
                    sc.act(lambda e, b=b: e.activation(out=b["DmT"][:], in_=b["dd"][:], func=AF.Exp), [tk("dd")], [tk("DmT")])
                    sc.dve(lambda e, b=b: e.tensor_tensor(out=b["DmS"][:], in0=b["DmT"][:], in1=offd6, op=ALU.mult), [tk("DmT"), "C"], [tk("DmS")])
                    for rc in range(3):
                        pk, tkk = self.psum()
                        sc.pe(lambda e, pk=pk, rc=rc, cs=cs: e.matmul(pk[:, 0:128], lhsT=kf32[rc][:, cs], rhs=ident, start=True, stop=True), [f"g_kf{rc}", "C"], [tkk])
                        sc.pe(lambda e, pk=pk, rc=rc, cs=cs: e.matmul(pk[:, 128:256], lhsT=vf32[rc][:, cs], rhs=ident, start=True, stop=True), [f"g_vf{rc}", "C"], [tkk])
                        sc.act(lambda e, pk=pk, rc=rc, b=b: e.copy(out=b["Ktm"][:, rc * 128:(rc + 1) * 128], in_=pk[:, 0:128]), [tkk], [(tk("Ktm"), rc)])
                        sc.act(lambda e, pk=pk, rc=rc, b=b: e.copy(out=b["Vtm"][:, rc * 128:(rc + 1) * 128], in_=pk[:, 128:256]), [tkk], [(tk("Vtm"), rc)])
                    for c in range(2):
                        sc.dve(lambda e, b=b, c=c: e.tensor_scalar(out=b[f"eendc{c}"][:], in0=b["eend"][:], scalar1=C[:, 256 + 64 * c:257 + 64 * c],
                                                                   scalar2=None, op0=ALU.mult), [tk("eend"), "C"], [tk(f"eendc{c}")])
                    for h in range(6):
                        eng = sc.dve
                        hs = slice(h * 64, (h + 1) * 64)
                        kz_c = slice(h * 128 + (h % 2) * 64, h * 128 + (h % 2) * 64 + 64)
                        eng(lambda e, b=b, h=h, hs=hs, kz_c=kz_c: e.tensor_scalar(out=b["Kez"][:, kz_c], in0=b["Ktm"][:, hs], scalar1=b["egc"][:, h:h + 1],
                                                                       scalar2=None, op0=ALU.mult), [(tk("Ktm"), h // 2), tk("egc")], [(tk("Kez"), h)])
                        for c in range(2):
                            eng(lambda e, b=b, h=h, hs=hs, c=c: e.tensor_scalar(out=b[f"Bhc{c}"][:, hs], in0=b["Ktm"][:, hs], scalar1=b[f"eendc{c}"][:, h:h + 1],
                                                                           scalar2=None, op0=ALU.mult), [(tk("Ktm"), h // 2), tk(f"eendc{c}")], [(tk(f"Bhc{c}"), h)])
                    pKa, tKa = self.psum(); pKb, tKb = self.psum()
                    pQa, tQa = self.psum(); pQb, tQb = self.psum()
                    def bank(h, pa, ta, pb_, tb):
                        return (pa[:, h * 128:(h + 1) * 128], ta) if h < 4 else (pb_[:, (h - 4) * 128:(h - 3) * 128], tb)
                    for h in range(6):
                        rc, hp = h // 2, h % 2
                        o1, to1 = bank(h, pKa, tKa, pKb, tKb)
                        sc.pe(lambda e, o1=o1, rc=rc, hp=hp, cs=cs: e.matmul(o1, lhsT=kz[rc][hp][:, cs], rhs=kfb[rc][:, cs], start=True, stop=True),
                              [f"g_kb{rc}", f"g_kz{rc}"], [to1])
                        o2, to2 = bank(h, pQa, tQa, pQb, tQb)
                        sc.pe(lambda e, o2=o2, rc=rc, hp=hp, cs=cs: e.matmul(o2, lhsT=kz[rc][hp][:, cs], rhs=qfb[rc][:, cs], start=True, stop=True),
                              [f"g_kz{rc}", f"g_qb{rc}"], [to2])
                    for h in range(6):
                        hc = slice(h * 128, (h + 1) * 128)
                        o1, to1 = bank(h, pKa, tKa, pKb, tKb)
                        sc.dve(lambda e, b=b, h=h, hc=hc, o1=o1, n=n: e.scalar_tensor_tensor(
                            out=b["Mm"][:, hc], in0=o1, scalar=nbeta[:, n, h:h + 1], in1=b["DmS"][:, hc], op0=ALU.mult, op1=ALU.mult),
                            [to1, "g_nbeta", tk("DmS")], [(tk("Mm"), h)])
                        o2, to2 = bank(h, pQa, tQa, pQb, tQb)
                        sc.dve(lambda e, b=b, hc=hc, o2=o2: e.tensor_tensor(out=b["AqT"][:, hc], in0=o2, in1=b["DmT"][:, hc], op=ALU.mult),
                               [to2, tk("DmT")], [(tk("AqT"), h)])
                    self.neumann(b, tk, ident, ident6)
                    sc.act(lambda e, b=b: e.copy(out=b["Rbf"][:], in_=b["R"][:]), [tk("R")], [tk("Rbf")])
                    pW, tW = self.psum()
                    pU, tU = self.psum()
                    for h in range(6):
                        rc, hp = h // 2, h % 2
                        hs = slice(h * 64, (h + 1) * 64); hc = slice(h * 128, (h + 1) * 128)
                        sc.pe(lambda e, pW=pW, b=b, rc=rc, hp=hp, hc=hc: e.matmul(
                            pW[:, rc * 128:(rc + 1) * 128], lhsT=b["Kez"][:, hc], rhs=b["Rbf"][:, hc], start=(hp == 0), stop=(hp == 1)),
                            [(tk("Kez"), h), tk("Rbf")], [tW])
                        sc.pe(lambda e, pU=pU, b=b, hs=hs, hc=hc: e.matmul(pU[:, hs], lhsT=b["Rbf"][:, hc], rhs=b["Vtm"][:, hs], start=True, stop=True),
                              [(tk("Vtm"), h // 2), tk("Rbf")], [tU])
                    sc.act(lambda e, pW=pW, b=b: e.mul(out=b["WtT"][:], in_=pW[:, 0:384], mul=-1.0), [tW], [tk("WtT")])
                    for h in range(6):
                        hs = slice(h * 64, (h + 1) * 64)
                        sc.dve(lambda e, pU=pU, b=b, h=h, hs=hs, n=n: e.tensor_scalar(out=b["Utb"][:, hs], in0=pU[:, hs], scalar1=beta[:, n, h:h + 1],
                                                                                 scalar2=None, op0=ALU.mult), [tU, "g_beta"], [tk("Utb")])
                    for h in range(6):
                        rc, hp = h // 2, h % 2
                        rows = slice(hp * 64, hp * 64 + 64)
                        eng = sc.dve
                        eng(lambda e, b=b, rc=rc, rows=rows, h=h, cs=cs: e.tensor_tensor(
                            out=b["qbar"][rows, rc * 128:(rc + 1) * 128], in0=qf32[rc][rows, cs], in1=b["Egc"][rows, h * 128:(h + 1) * 128], op=ALU.mult),
                            [f"g_qf{rc}", tk("Egc")], [tk("qbar")])
                    gl_ap = lambda h, c, b=b: b["Egc"][(h % 2) * 64:(h % 2) * 64 + 64, h * 128 + c * 64 + 63:h * 128 + c * 64 + 64]
                    pre_ops = sc.cap_end()
                    sc.cap_begin(); self.ps_lo, self.ps_n = 4, 1
                    self.chunk_loop(b, tk, n, psO, H32, Hbd, "g", gl_ap, [tk("Egc")], beta=beta)
                    loop_ops = sc.cap_end()
                    self.ps_lo, self.ps_n = 0, 5
                    sc.replay(pend_loop, pre_ops)
                    pend_loop = loop_ops
                    if n == 3:
                        sc.replay(pend_loop)
                        pend_loop = []
                for rc in range(3 if self.stage >= 6 else 0):
                    po, pot = psO[rc]
                    sc.act(lambda e, po=po: e.copy(out=osb[:], in_=po[:]), [pot], ["g_osb"])
                    sc.act(lambda e: e.activation(out=sqt[:], in_=osb[:], func=AF.Square), ["g_osb"], ["g_sq"])
                    p, ptok = self.psum()
                    sc.pe(lambda e, p=p: e.matmul(p[:], lhsT=self.onesbd_bf[:], rhs=sqt[:], start=True, stop=True), ["g_sq", "onesbd_bf"], [ptok])
                    sc.act(lambda e, p=p: e.activation(out=rinv[:], in_=p[:], func=AF.Sqrt, bias=NORM_EPS, scale=1.0 / 64), [ptok], ["g_rinv"])
                    sc.dve(lambda e: e.reciprocal(out=rinv[:], in_=rinv[:]), ["g_rinv"], ["g_rinv"])
                    r0 = 2976 + rc * 128
                    sc.dma(gate[:], self.pT[r0:r0 + 128, t0:t0 + 512], writes=["g_gate"])
                    sc.act(lambda e: e.activation(out=gate[:], in_=gate[:], func=AF.Silu), ["g_gate"], ["g_gate"])
                    sc.dve(lambda e: e.tensor_tensor(out=osb[:], in0=osb[:], in1=rinv[:], op=ALU.mult), ["g_osb", "g_rinv"], ["g_osb"])
                    sc.dve(lambda e: e.scalar_tensor_tensor(out=ybf[:], in0=osb[:], scalar=gs[:, 12:13], in1=gate[:], op0=ALU.mult, op1=ALU.mult),
                           ["g_osb", "gsm", "g_gate"], ["g_ybf"])
                    y0 = 640 + rc * 128
                    sc.dma(self.yT[y0:y0 + 128, t0:t0 + 512], ybf[:], reads=["g_ybf"], writes=[("yT", y0, sbi)], q="pool")
            sc.flush()
        self.ps_lo, self.ps_n = 0, 8

    def mla_tables(self):
        nc, sc, S = self.nc, self.sc, self.S
        with contextlib.ExitStack() as st:
            T = 2048 if S % 2048 == 0 else 512
            pi_ = st.enter_context(nc.sbuf_tensor("m_posi", [96, T], I32))
            pf = st.enter_context(nc.sbuf_tensor("m_posf", [96, T], F32))
            t1 = st.enter_context(nc.sbuf_tensor("m_tt1", [96, T], F32))
            t2 = st.enter_context(nc.sbuf_tensor("m_tt2", [96, T], F32))
            ifq = self.mlc[0:96, 0:1]
            for t in range(S // T):
                t0 = t * T
                sc.dma(pi_[:], self.pos96[:, t0:t0 + T], writes=["m_posi"])
                sc.dve(lambda e: e.tensor_copy(out=pf[:], in_=pi_[:]), ["m_posi"], ["m_posf"])
                sc.dve(lambda e: e.tensor_scalar(out=pf[:], in0=pf[:], scalar1=ifq, scalar2=None, op0=ALU.mult), ["m_posf", "mlc"], ["m_posf"])
                sc.dve(lambda e: e.tensor_scalar(out=pf[:], in0=pf[:], scalar1=float(1.0 / (2.0 * np.pi)), scalar2=None, op0=ALU.mult), ["m_posf"], ["m_posf"])
                sc.dve(lambda e: e.tensor_copy(out=pi_[:], in_=pf[:]), ["m_posf", "m_posi"], ["m_posi"])
                sc.dve(lambda e: e.tensor_copy(out=t1[:], in_=pi_[:]), ["m_posi"], ["m_tt1"])
                sc.dve(lambda e: e.tensor_tensor(out=pf[:], in0=pf[:], in1=t1[:], op=ALU.subtract), ["m_posf", "m_tt1"], ["m_posf"])
                sc.act(lambda e: e.activation(out=t1[:], in_=pf[:], func=AF.Sin, scale=float(np.pi)), ["m_posf"], ["m_tt1"])
                sc.act(lambda e: e.activation(out=t2[:], in_=pf[:], func=AF.Sin, scale=float(np.pi / 2)), ["m_posf"], ["m_tt2"])
                sc.dve(lambda e: e.tensor_tensor(out=t2[:], in0=t2[:], in1=t2[:], op=ALU.mult), ["m_tt2"], ["m_tt2"])
                sc.dve(lambda e: e.tensor_scalar(out=t2[:], in0=t2[:], scalar1=-2.0, scalar2=1.0, op0=ALU.mult, op1=ALU.add), ["m_tt2"], ["m_tt2"])
                sc.dve(lambda e: e.scalar_tensor_tensor(out=t2[:], in0=t1[:], scalar=2.0, in1=t2[:], op0=ALU.mult, op1=ALU.mult), ["m_tt1", "m_tt2"], ["m_tt2"])
                sc.dma(self.sinT[:, t0:t0 + T], t2[:], reads=["m_tt2"], writes=[("tab", 1, t)], q="pool")
                sc.dve(lambda e: e.tensor_tensor(out=t1[:], in0=t1[:], in1=t1[:], op=ALU.mult), ["m_tt1"], ["m_tt1"])
                sc.dve(lambda e: e.tensor_scalar(out=t1[:], in0=t1[:], scalar1=-2.0, scalar2=1.0, op0=ALU.mult, op1=ALU.add), ["m_tt1"], ["m_tt1"])
                sc.dma(self.cosT[:, t0:t0 + T], t1[:], reads=["m_tt1"], writes=[("tab", 0, t)], q="pool")
            sc.flush()

    def phase_mla(self, l):
        nc, sc, S = self.nc, self.sc, self.S
        C = self.C
        T = 512
        ml = self.mls[:, l * 8:(l + 1) * 8]
        with contextlib.ExitStack() as st:
            sb = lambda n, s, d=F32: st.enter_context(nc.sbuf_tensor(f"{n}_L{l}", s, d))
            wst = sb("m_wst", [128, 512])
            wuq = sb("m_wuq", [128, 2, 384], BF16); wuqr = sb("m_wuqr", [128, 2, 384], BF16)
            wkn = sb("m_wkn", [128, 384], BF16); wv = sb("m_wv", [128, 256], BF16)
            sel = sb("m_sel", [32, 96], BF16); ones96 = sb("m_ones96", [96, 96], BF16)
            cq = sb("m_cq", [128, 2, T]); ckv = sb("m_ckv", [128, T]); kr = sb("m_kr", [32, T]); krt = sb("m_krt", [32, T])
            sq2 = sb("m_sq2", [128, 2, T], BF16); sq1 = sb("m_sq1", [128, T], BF16)
            rq = sb("m_rq", [128, T]); rkv = sb("m_rkv", [128, T])
            cqn = sb("m_cqn", [128, 2, T], BF16); ckvn = sb("m_ckvn", [128, T], BF16)
            krb = sb("m_krb", [32, T], BF16); krtb = sb("m_krtb", [32, T], BF16)
            cosb = sb("m_cos", [96, T]); sinb = sb("m_sin", [96, T])
            raw = sb("m_raw", [96, T]); rot = sb("m_rot", [96, T]); sq96 = sb("m_sq96", [96, T], BF16); rs96 = sb("m_rs96", [96, T])
            fin = [sb(f"m_fin{i}", [96, T], BF16) for i in range(2)]
            vx = [sb(f"m_vx{i}", [128, 4, 128], BF16) for i in range(2)]
            sc.dma(wst[:, 0:384], self.w_uq[l, 0:128, :], writes=["m_wst"])
            sc.dve(lambda e: e.tensor_scalar(out=wuq[:, 0, :], in0=wst[:, 0:384], scalar1=ml[:, 0:1], scalar2=None, op0=ALU.mult), ["m_wst", "mls"], ["m_wuq"])
            sc.dma(wst[:, 0:384], self.w_uq[l, 128:256, :], reads=["m_wst"], writes=["m_wst"])
            sc.dve(lambda e: e.tensor_scalar(out=wuq[:, 1, :], in0=wst[:, 0:384], scalar1=ml[:, 1:2], scalar2=None, op0=ALU.mult), ["m_wst", "mls"], ["m_wuq"])
            sc.pool(lambda e: e.memset(wuqr[:], 0.0), [], ["m_wuqr"])
            for h in range(4):
                b0 = h * 96
                sc.dve(lambda e, b0=b0: e.tensor_scalar(out=wuqr[:, :, b0 + 64:b0 + 80], in0=wuq[:, :, b0 + 80:b0 + 96], scalar1=-1.0, scalar2=None, op0=ALU.mult),
                       ["m_wuq", "m_wuqr"], ["m_wuqr"])
                sc.dve(lambda e, b0=b0: e.tensor_copy(out=wuqr[:, :, b0 + 80:b0 + 96], in_=wuq[:, :, b0 + 64:b0 + 80]), ["m_wuq", "m_wuqr"], ["m_wuqr"])
            sc.dma(wst[:], self.w_ukv[l], reads=["m_wst"], writes=["m_wst"])
            sc.pool(lambda e: e.memset(wkn[:], 0.0), [], ["m_wkn"])
            for h in range(4):
                sc.dve(lambda e, h=h: e.tensor_scalar(out=wkn[:, h * 96:h * 96 + 64], in0=wst[:, h * 128:h * 128 + 64], scalar1=ml[:, 2:3], scalar2=None, op0=ALU.mult),
                       ["m_wst", "mls", "m_wkn"], ["m_wkn"])
                sc.dve(lambda e, h=h: e.tensor_scalar(out=wv[:, h * 64:h * 64 + 64], in0=wst[:, h * 128 + 64:h * 128 + 128], scalar1=ml[:, 2:3], scalar2=None, op0=ALU.mult),
                       ["m_wst", "mls"], ["m_wv"])
            sc.dve(lambda e: e.tensor_copy(out=sel[:], in_=self.mlc[0:32, 8:104]), ["mlc"], ["m_sel"])
            sc.pool(lambda e: e.memset(ones96[:], 1.0), [], ["m_ones96"])
            for i in range(2):
                sc.pool(lambda e, i=i: e.memset(vx[i][:], 1.0), [], [f"m_vx{i}"])
            nfin = 0
            for t in range(S // T):
                t0 = t * T
                sc.dma(cq[:], self.pT[1408:1664, t0:t0 + T].rearrange("(k p) s -> p k s", p=128), writes=["m_cq"])
                sc.dma(ckv[:], self.pT[1664:1792, t0:t0 + T], writes=["m_ckv"])
                sc.dma(kr[:], self.pT[1792:1824, t0:t0 + T], writes=["m_kr"])
                sc.dma(krt[:], self.pT[3372:3404, t0:t0 + T], writes=["m_krt"])
                sc.dma(cosb[:], self.cosT[:, t0:t0 + T], writes=["m_cos"])
                sc.dma(sinb[:], self.sinT[:, t0:t0 + T], writes=["m_sin"])
                sc.act(lambda e: e.activation(out=sq2[:], in_=cq[:], func=AF.Square), ["m_cq"], ["m_sq2"])
                sc.act(lambda e: e.activation(out=sq1[:], in_=ckv[:], func=AF.Square), ["m_ckv"], ["m_sq1"])
                p, ptok = self.psum()
                for kc in range(2):
                    sc.pe(lambda e, p=p, kc=kc: e.matmul(p[:], lhsT=self.ones_bf[:], rhs=sq2[:, kc, :], start=(kc == 0), stop=(kc == 1)), ["m_sq2", "ones_bf"], [ptok])
                sc.act(lambda e, p=p: e.activation(out=rq[:], in_=p[:], func=AF.Sqrt, bias=NORM_EPS, scale=1.0 / 256), [ptok], ["m_rq"])
                sc.dve(lambda e: e.reciprocal(out=rq[:], in_=rq[:]), ["m_rq"], ["m_rq"])
                p, ptok = self.psum()
                sc.pe(lambda e, p=p: e.matmul(p[:], lhsT=self.ones_bf[:], rhs=sq1[:], start=True, stop=True), ["m_sq1", "ones_bf"], [ptok])
                sc.act(lambda e, p=p: e.activation(out=rkv[:], in_=p[:], func=AF.Sqrt, bias=NORM_EPS, scale=1.0 / 128), [ptok], ["m_rkv"])
                sc.dve(lambda e: e.reciprocal(out=rkv[:], in_=rkv[:]), ["m_rkv"], ["m_rkv"])
                for kc in range(2):
                    sc.dve(lambda e, kc=kc: e.tensor_tensor(out=cqn[:, kc, :], in0=cq[:, kc, :], in1=rq[:], op=ALU.mult), ["m_cq", "m_rq"], ["m_cqn"])
                sc.dve(lambda e: e.tensor_tensor(out=ckvn[:], in0=ckv[:], in1=rkv[:], op=ALU.mult), ["m_ckv", "m_rkv"], ["m_ckvn"])
                sc.act(lambda e: e.copy(out=krb[:], in_=kr[:]), ["m_kr"], ["m_krb"])
                sc.act(lambda e: e.copy(out=krtb[:], in_=krt[:]), ["m_krt"], ["m_krtb"])
                for h in range(4):
                    hc = slice(h * 96, (h + 1) * 96)
                    for isk in range(2):
                        pr, prt = self.psum()
                        pro, prot = self.psum()
                        if isk == 0:
                            for kc in range(2):
                                sc.pe(lambda e, pr=pr, kc=kc, hc=hc: e.matmul(pr[0:96, :], lhsT=wuq[:, kc, hc], rhs=cqn[:, kc, :], start=(kc == 0), stop=(kc == 1)),
                                      ["m_wuq", "m_cqn"], [prt])
                            for kc in range(2):
                                sc.pe(lambda e, pro=pro, kc=kc, hc=hc: e.matmul(pro[0:96, :], lhsT=wuqr[:, kc, hc], rhs=cqn[:, kc, :], start=(kc == 0), stop=(kc == 1)),
                                      ["m_wuqr", "m_cqn"], [prot])
                            gcol, gpcol, fscale = ml[0:96, 3:4], ml[0:96, 4:5], float(96 ** -0.5)
                        else:
                            sc.pe(lambda e, pr=pr, hc=hc: e.matmul(pr[0:96, :], lhsT=wkn[:, hc], rhs=ckvn[:], start=True, stop=False), ["m_wkn", "m_ckvn"], [prt])
                            sc.pe(lambda e, pr=pr: e.matmul(pr[0:96, :], lhsT=sel[:], rhs=krb[:], start=False, stop=True), ["m_sel", "m_krb"], [prt])
                            sc.pe(lambda e, pro=pro: e.matmul(pro[0:96, :], lhsT=sel[:], rhs=krtb[:], start=True, stop=True), ["m_sel", "m_krtb"], [prot])
                            gcol, gpcol, fscale = ml[0:96, 5:6], ml[0:96, 6:7], 1.0
                        sc.act(lambda e, pr=pr: e.copy(out=raw[:], in_=pr[0:96, :]), [prt], ["m_raw"])
                        sc.act(lambda e, pro=pro: e.copy(out=rot[:], in_=pro[0:96, :]), [prot], ["m_rot"])
                        sc.act(lambda e: e.activation(out=sq96[:], in_=raw[:], func=AF.Square), ["m_raw"], ["m_sq96"])
                        p, ptok = self.psum()
                        sc.pe(lambda e, p=p: e.matmul(p[0:96, :], lhsT=ones96[:], rhs=sq96[:], start=True, stop=True), ["m_sq96", "m_ones96"], [ptok])
                        sc.act(lambda e, p=p: e.activation(out=rs96[:], in_=p[0:96, :], func=AF.Sqrt, bias=NORM_EPS, scale=1.0 / 96), [ptok], ["m_rs96"])
                        sc.dve(lambda e: e.reciprocal(out=rs96[:], in_=rs96[:]), ["m_rs96"], ["m_rs96"])
                        sc.dve(lambda e, gcol=gcol: e.scalar_tensor_tensor(out=raw[:], in0=raw[:], scalar=gcol, in1=cosb[:], op0=ALU.mult, op1=ALU.mult),
                               ["m_raw", "mls", "m_cos"], ["m_raw"])
                        sc.dve(lambda e, gpcol=gpcol: e.scalar_tensor_tensor(out=rot[:], in0=rot[:], scalar=gpcol, in1=sinb[:], op0=ALU.mult, op1=ALU.mult),
                               ["m_rot", "mls", "m_sin"], ["m_rot"])
                        sc.dve(lambda e: e.tensor_tensor(out=raw[:], in0=raw[:], in1=rot[:], op=ALU.add), ["m_raw", "m_rot"], ["m_raw"])
                        sc.dve(lambda e: e.tensor_tensor(out=raw[:], in0=raw[:], in1=rs96[:], op=ALU.mult), ["m_raw", "m_rs96"], ["m_raw"])
                        fi = nfin % 2
                        nfin += 1
                        sc.act(lambda e, fi=fi, fscale=fscale: e.mul(out=fin[fi][:], in_=raw[:], mul=fscale), ["m_raw"], [f"m_fin{fi}"])
                        dstT = self.qfT if isk == 0 else self.kfT
                        sc.dma(dstT[h, :, t0:t0 + T], fin[fi][:], reads=[f"m_fin{fi}"], writes=[("qk", isk, h, t)], q="pool")
                for n in range(4):
                    vi = (t * 4 + n) % 2
                    p, ptok = self.psum()
                    sc.pe(lambda e, p=p, n=n: e.matmul(p[:, 0:256], lhsT=ckvn[:, n * 128:(n + 1) * 128], rhs=wv[:], start=True, stop=True), ["m_ckvn", "m_wv"], [ptok])
                    sc.act(lambda e, p=p, vi=vi: e.copy(out=vx[vi][:, :, 0:64], in_=p[:, 0:256].rearrange("p (h d) -> p h d", d=64)), [ptok], [f"m_vx{vi}"])
                    r0 = t0 + n * 128
                    sc.dma(self.vxT[:, r0:r0 + 128, :].rearrange("h p d -> p h d"), vx[vi][:], reads=[f"m_vx{vi}"], writes=[("vx", t, n)], q="pool")
            sc.flush()
        self.ps_n = 6
        psOD = [(self.ps[6 + i], f"psOD{i}") for i in range(2)]
        NB = S // 128
        with contextlib.ExitStack() as st:
            sb = lambda n, s, d=F32: st.enter_context(nc.sbuf_tensor(f"{n}_L{l}", s, d))
            kf = sb("m_kf", [96, S], BF16); qf = sb("m_qf", [96, S], BF16)
            vxa = sb("m_vxa", [128, NB, 128], BF16)
            mk32 = sb("m_mk32", [128, 2048]); mk = sb("m_mk", [128, 2048], BF16)
            pt = [sb(f"m_pt{i}", [128, T], BF16) for i in range(3)]
            od = sb("m_od", [128, T]); den = sb("m_den", [64, T]); yb = [sb(f"m_yb{i}", [64, T], BF16) for i in range(2)]
            sc.dma(mk32[:], self.mmask, writes=["m_mk32"])
            sc.dve(lambda e: e.tensor_copy(out=mk[:], in_=mk32[:]), ["m_mk32"], ["m_mk"])
            npt = 0
            ng = 0
            for h in range(4):
                sc.dma(kf[:], self.kfT[h], writes=["m_kf"])
                sc.dma(qf[:], self.qfT[h], writes=["m_qf"])
                sc.dma(vxa[:], self.vxT[h].rearrange("(n p) d -> p n d", p=128), writes=["m_vxa"])
                for g in range(S // T):
                    po, pot = psOD[ng % 2]
                    nkb = 4 * g + 4
                    for kb in range(nkb):
                        ps_, pst = self.psum()
                        sc.pe(lambda e, ps_=ps_, kb=kb, g=g: e.matmul(ps_[:], lhsT=kf[:, kb * 128:(kb + 1) * 128], rhs=qf[:, g * T:(g + 1) * T], start=True, stop=True),
                              ["m_kf", "m_qf"], [pst])
                        pi = npt % 3
                        npt += 1
                        sc.act(lambda e, ps_=ps_, pi=pi: e.activation(out=pt[pi][:], in_=ps_[:], func=AF.Exp), [pst], [f"m_pt{pi}"])
                        j = kb - 4 * g
                        if j >= 0:
                            sc.dve(lambda e, pi=pi, j=j: e.tensor_tensor(out=pt[pi][:], in0=pt[pi][:], in1=mk[:, j * T:(j + 1) * T], op=ALU.mult),
                                    [f"m_pt{pi}", "m_mk"], [f"m_pt{pi}"])
                        sc.pe(lambda e, po=po, kb=kb, pi=pi, nkb=nkb: e.matmul(po[:], lhsT=vxa[:, kb, :], rhs=pt[pi][:], start=(kb == 0), stop=(kb == nkb - 1)),
                              ["m_vxa", f"m_pt{pi}"], [pot])
                    sc.act(lambda e, po=po: e.copy(out=od[:], in_=po[:]), [pot], ["m_od"])
                    sc.dve(lambda e: e.tensor_copy(out=den[:], in_=od[64:128, :]), ["m_od"], ["m_den"])
                    sc.dve(lambda e: e.reciprocal(out=den[:], in_=den[:]), ["m_den"], ["m_den"])
                    yi = ng % 2
                    sc.dve(lambda e, yi=yi: e.tensor_tensor(out=yb[yi][:], in0=od[0:64, :], in1=den[:], op=ALU.mult), ["m_od", "m_den"], [f"m_yb{yi}"])
                    y0 = 384 + h * 64
                    sc.dma(self.yT[y0:y0 + 64, g * T:(g + 1) * T], yb[yi][:], reads=[f"m_yb{yi}"], writes=[("yT", y0, g)], q="pool")
                    ng += 1
            sc.flush()
        self.ps_lo, self.ps_n = 0, 8

    def phase_rwkv(self, l):
        nc, sc, S = self.nc, self.sc, self.S
        C = self.C
        ident, onesbd = C[:, 0:128], C[:, 256:384]
        mstrict, mincl, nmstrict = C[:, 768:896], C[:, 896:1024], C[:, 384:512]
        ident6 = C[:, 1792:2560]
        segm = self.segm
        self.ps_n = 5
        psO = [(self.ps[5 + i], f"psO{i}") for i in range(3)]
        rs_ = self.rws[:, l * 32:(l + 1) * 32]
        MU, W0, A0, KK_, KA, RK, GG, GB = 0, 11, 14, 17, 20, 23, 26, 29
        with contextlib.ExitStack() as st:
            sb = lambda n, s, d=F32: st.enter_context(nc.sbuf_tensor(f"{n}_L{l}", s, d))
            T = 512
            wst = sb("r_wst", [128, 1152])
            wbf = sb("r_wbf", [128, 1152], BF16)
            xin = [sb(f"r_xin{i}", [128, 513]) for i in range(2)]
            dtmp = sb("r_dtmp", [128, T])
            f3 = lambda nm, d=F32: [sb(f"r_{nm}{i}", [128, T], d) for i in range(3)]
            rf, kraw, vf, gf_, epos = (f3(n_) for n_ in ("rf", "kraw", "vf", "gf", "epos"))
            af, gc, kk, kmod, bvec, eneg = ([sb(f"r_{n_}", [128, T])] * 3 for n_ in ("af", "gc", "kk", "kmod", "bvec", "eneg"))
            ap32, Bh32, Kh32, bonus = f3("ap32"), f3("Bh32"), f3("Kh32"), f3("bonus")
            apb, rbar, btb, ktb = f3("apb", BF16), f3("rbar", BF16), f3("btb", BF16), f3("ktb", BF16)
            btz = [[sb(f"r_btz{i}_{hp}", [128, T], BF16) for hp in range(2)] for i in range(3)]
            ktz = [[sb(f"r_ktz{i}_{hp}", [128, T], BF16) for hp in range(2)] for i in range(3)]
            z9, zg = sb("r_z9", [128, T]), sb("r_zg", [128, T])
            act9, sigzg = sb("r_act9", [128, T], BF16), sb("r_sigzg", [128, T], BF16)
            tA, tB = sb("r_tA", [128, T]), sb("r_tB", [128, T])
            sqt = sb("r_sq", [128, T], BF16)
            H32 = sb("r_H32", [128, 384]); Hbd = sb("r_Hbd", [128, 384], BF16)
            B = []
            for pb in range(2):
                d = {}
                for nm, shp, dtp in [("Mm", [128, 768], F32), ("Mt", [128, 768], F32), ("R", [128, 768], F32), ("Pa", [128, 768], F32),
                                     ("Pta", [128, 768], F32), ("Rbf", [128, 768], BF16), ("AqT", [128, 768], BF16), ("AqkT", [128, 768], BF16),
                                     ("AakT", [128, 768], BF16), ("Kez", [128, 768], BF16), ("Vz", [128, 768], BF16), ("Vtm", [128, 384], BF16),
                                     ("Xb", [128, 384], BF16), ("Ubz", [128, 768], BF16), ("WtT", [128, 384], BF16), ("Utb", [128, 384], F32),
                                     ("qbar", [128, 384], BF16), ("Bhc0", [128, 384], BF16), ("Bhc1", [128, 384], BF16),
                                     ("Khc0", [128, 384], BF16), ("Khc1", [128, 384], BF16), ("BKtm", [128, 256], F32)]:
                    d[nm] = sb(f"r_{nm}{pb}", shp, dtp)
                B.append(d)
            sc.dma(wst[:], self.rww[:, l * 1152:(l + 1) * 1152], writes=["r_wst"])
            sc.dve(lambda e: e.tensor_copy(out=wbf[:], in_=wst[:]), ["r_wst"], ["r_wbf"])
            sc.dve(lambda e: e.memset(H32[:], 0.0), [], [("r_H32", h) for h in range(6)])
            sc.pool(lambda e: e.memset(Hbd[:], 0.0), [], [("r_Hbd", h) for h in range(6)])
            for i in range(3):
                for hp in range(2):
                    sc.pool(lambda e, i=i, hp=hp: e.memset(btz[i][hp][:], 0.0), [], [f"r_btz{i}"])
                    sc.pool(lambda e, i=i, hp=hp: e.memset(ktz[i][hp][:], 0.0), [], [f"r_ktz{i}"])
            for pb in range(2):
                for nm in ("Kez", "Vz", "Ubz"):
                    sc.pool(lambda e, pb=pb, nm=nm: e.memset(B[pb][nm][:], 0.0), [], [(f"r_{nm}{pb}", h) for h in range(6)])
            nblk = 0
            pend_loop = []
            for sbi in range(S // T):
                t0 = sbi * T
                dests = rf + kraw + vf + [z9, zg]
                dtoks = [f"r_rf{i}" for i in range(3)] + [f"r_kraw{i}" for i in range(3)] + [f"r_vf{i}" for i in range(3)] + ["r_z9", "r_zg"]
                for rc in range(11):
                    xi, xt = xin[rc % 2], f"r_xin{rc % 2}"
                    if sbi == 0:
                        sc.pool(lambda e, xi=xi: e.memset(xi[:, 0:1], 0.0), [], [xt])
                        sc.dma(xi[:, 1:513], self.pT[rc * 128:(rc + 1) * 128, 0:T], reads=[xt], writes=[xt])
                    else:
                        sc.dma(xi[:], self.pT[rc * 128:(rc + 1) * 128, t0 - 1:t0 + T], writes=[xt])
                    sc.dve(lambda e, xi=xi: e.tensor_tensor(out=dtmp[:], in0=xi[:, 0:512], in1=xi[:, 1:513], op=ALU.subtract), [xt], ["r_dtmp"])
                    dst = dests[rc]
                    sc.dve(lambda e, xi=xi, dst=dst, rc=rc: e.scalar_tensor_tensor(out=dst[:], in0=dtmp[:], scalar=rs_[:, MU + rc:MU + rc + 1], in1=xi[:, 1:513],
                                                                            op0=ALU.mult, op1=ALU.add), ["r_dtmp", xt, "rws"], [dtoks[rc]])
                sc.act(lambda e: e.activation(out=act9[0:64, :], in_=z9[0:64, :], func=AF.Tanh), ["r_z9"], ["r_act9"])
                sc.act(lambda e: e.copy(out=act9[64:128, :], in_=z9[64:128, :]), ["r_z9"], ["r_act9"])
                sc.act(lambda e: e.activation(out=sigzg[:], in_=zg[:], func=AF.Sigmoid), ["r_zg"], ["r_sigzg"])
                for c in range(3):
                    cc = slice(c * 128, (c + 1) * 128)
                    p, ptok = self.psum()
                    sc.pe(lambda e, p=p, cc=cc: e.matmul(p[:], lhsT=wbf[:, cc], rhs=act9[:], start=True, stop=True), ["r_wbf", "r_act9"], [ptok])
                    sc.act(lambda e, p=p, c=c: e.activation(out=gc[c][:], in_=p[:], func=AF.Sigmoid, bias=rs_[:, W0 + c:W0 + c + 1]), [ptok, "rws"], ["r_gc"])
                    sc.dve(lambda e, c=c: e.tensor_scalar(out=tA[:], in0=gc[c][:], scalar1=-0.6065306597126334, scalar2=None, op0=ALU.mult), ["r_gc"], ["r_tA"])
                    p2, p2tok = self.psum()
                    sc.pe(lambda e, p2=p2, c=c: e.matmul(p2[:], lhsT=wbf[:, 384 + c * 128:384 + (c + 1) * 128], rhs=act9[:], start=True, stop=True), ["r_wbf", "r_act9"], [p2tok])
                    sc.act(lambda e, p2=p2, c=c: e.activation(out=af[c][:], in_=p2[:], func=AF.Sigmoid, bias=rs_[:, A0 + c:A0 + c + 1]), [p2tok, "rws"], ["r_af"])
                    p3, p3tok = self.psum()
                    sc.pe(lambda e, p3=p3, c=c: e.matmul(p3[:], lhsT=wbf[:, 768 + c * 128:768 + (c + 1) * 128], rhs=sigzg[:], start=True, stop=True), ["r_wbf", "r_sigzg"], [p3tok])
                    sc.act(lambda e, p3=p3, c=c: e.copy(out=gf_[c][:], in_=p3[:]), [p3tok], [f"r_gf{c}"])
                    sc.dve(lambda e, c=c: e.tensor_tensor_scan(out=gc[c][:], data0=segm[:], data1=tA[:], initial=0.0, op0=ALU.mult, op1=ALU.add),
                           ["r_tA", "segm", "r_gc"], ["r_gc"])
                    sc.act(lambda e, c=c: e.activation(out=epos[c][:], in_=gc[c][:], func=AF.Exp), ["r_gc"], [f"r_epos{c}"])
                    sc.act(lambda e, c=c: e.activation(out=eneg[c][:], in_=gc[c][:], func=AF.Exp, scale=-1.0), ["r_gc"], ["r_eneg"])
                    sc.dve(lambda e, c=c: e.tensor_tensor(out=tB[:], in0=gc[c][:], in1=tA[:], op=ALU.subtract), ["r_gc", "r_tA"], ["r_tB"])
                    sc.act(lambda e: e.activation(out=tB[:], in_=tB[:], func=AF.Exp), ["r_tB"], ["r_tB"])
                    sc.dve(lambda e, c=c: e.tensor_scalar(out=kk[c][:], in0=kraw[c][:], scalar1=rs_[:, KK_ + c:KK_ + c + 1], scalar2=None, op0=ALU.mult),
                           [f"r_kraw{c}", "rws"], ["r_kk"])
                    sc.act(lambda e, c=c: e.activation(out=sqt[:], in_=kk[c][:], func=AF.Square), ["r_kk"], ["r_sq"])
                    p4, p4tok = self.psum()
                    sc.pe(lambda e, p4=p4: e.matmul(p4[:], lhsT=self.onesbd_bf[:], rhs=sqt[:], start=True, stop=True), ["r_sq", "onesbd_bf"], [p4tok])
                    sc.act(lambda e, p4=p4: e.activation(out=tA[:], in_=p4[:], func=AF.Sqrt, bias=1e-6, scale=1.0), [p4tok, "r_tA"], ["r_tA"])
                    sc.dve(lambda e: e.reciprocal(out=tA[:], in_=tA[:]), ["r_tA"], ["r_tA"])
                    sc.dve(lambda e, c=c: e.tensor_tensor(out=kk[c][:], in0=kk[c][:], in1=tA[:], op=ALU.mult), ["r_kk", "r_tA"], ["r_kk"])
                    sc.dve(lambda e, c=c: e.tensor_tensor(out=ap32[c][:], in0=kk[c][:], in1=tB[:], op=ALU.mult), ["r_kk", "r_tB"], [f"r_ap32{c}"])
                    sc.act(lambda e, c=c: e.copy(out=apb[c][:], in_=ap32[c][:]), [f"r_ap32{c}"], [f"r_apb{c}"])
                    sc.dve(lambda e, c=c: e.tensor_scalar(out=tA[:], in0=af[c][:], scalar1=-1.0, scalar2=None, op0=ALU.add), ["r_af", "r_tA"], ["r_tA"])
                    sc.dve(lambda e, c=c: e.tensor_scalar(out=tA[:], in0=tA[:], scalar1=rs_[:, KA + c:KA + c + 1], scalar2=None, op0=ALU.mult), ["rws", "r_tA"], ["r_tA"])
                    sc.dve(lambda e, c=c: e.scalar_tensor_tensor(out=kmod[c][:], in0=tA[:], scalar=1.0, in1=kraw[c][:], op0=ALU.add, op1=ALU.mult),
                           ["r_tA", f"r_kraw{c}"], ["r_kmod"])
                    sc.dve(lambda e, c=c: e.tensor_tensor(out=bvec[c][:], in0=kk[c][:], in1=af[c][:], op=ALU.mult), ["r_kk", "r_af"], ["r_bvec"])
                    sc.dve(lambda e, c=c: e.scalar_tensor_tensor(out=sqt[:], in0=rf[c][:], scalar=rs_[:, RK + c:RK + c + 1], in1=kmod[c][:], op0=ALU.mult, op1=ALU.mult),
                           [f"r_rf{c}", "rws", "r_kmod", "r_sq"], ["r_sq"])
                    p5, p5tok = self.psum()
                    sc.pe(lambda e, p5=p5: e.matmul(p5[:], lhsT=self.onesbd_bf[:], rhs=sqt[:], start=True, stop=True), ["r_sq", "onesbd_bf"], [p5tok])
                    sc.dve(lambda e, p5=p5, c=c: e.tensor_tensor(out=bonus[c][:], in0=p5[:], in1=vf[c][:], op=ALU.mult), [p5tok, f"r_vf{c}"], [f"r_bonus{c}"])
                    sc.dve(lambda e, c=c: e.tensor_tensor(out=rbar[c][:], in0=rf[c][:], in1=epos[c][:], op=ALU.mult), [f"r_rf{c}", f"r_epos{c}"], [f"r_rbar{c}"])
                    sc.dve(lambda e, c=c: e.tensor_tensor(out=btb[c][:], in0=bvec[c][:], in1=eneg[c][:], op=ALU.mult), ["r_bvec", "r_eneg"], [f"r_btb{c}"])
                    sc.dve(lambda e, c=c: e.tensor_tensor(out=ktb[c][:], in0=kmod[c][:], in1=eneg[c][:], op=ALU.mult), ["r_kmod", "r_eneg"], [f"r_ktb{c}"])
                    for hp in range(2):
                        hr = slice(hp * 64, hp * 64 + 64)
                        sc.act(lambda e, c=c, hp=hp, hr=hr: e.copy(out=btz[c][hp][hr, :], in_=btb[c][hr, :]), [f"r_btb{c}"], [f"r_btz{c}"])
                        sc.act(lambda e, c=c, hp=hp, hr=hr: e.copy(out=ktz[c][hp][hr, :], in_=ktb[c][hr, :]), [f"r_ktb{c}"], [f"r_ktz{c}"])
                    for j in range(8):
                        js = slice(j * 64, (j + 1) * 64)
                        eng = sc.dve
                        eng(lambda e, c=c, j=j, js=js: e.tensor_scalar(out=tA[:, js], in0=eneg[c][:, js], scalar1=epos[c][:, j * 64 + 63:j * 64 + 64], scalar2=None,
                                                                        op0=ALU.mult), ["r_eneg", f"r_epos{c}", "r_tA"], ["r_tA"])
                    sc.dve(lambda e, c=c: e.tensor_tensor(out=Bh32[c][:], in0=bvec[c][:], in1=tA[:], op=ALU.mult), ["r_bvec", "r_tA"], [f"r_Bh32{c}"])
                    sc.dve(lambda e, c=c: e.tensor_tensor(out=Kh32[c][:], in0=kmod[c][:], in1=tA[:], op=ALU.mult), ["r_kmod", "r_tA"], [f"r_Kh32{c}"])
                for n in range(4 if self.stage >= 3 else 0):
                    pb = nblk % 2
                    nblk += 1
                    b = B[pb]
                    tk = lambda nm, pb=pb: f"r_{nm}{pb}"
                    cs = slice(n * 128, (n + 1) * 128)
                    sc.cap_begin(); self.ps_lo, self.ps_n = 0, 4
                    for rc in range(3):
                        pk, tkk = self.psum()
                        for q_, (src, stok) in enumerate([(ap32, "r_ap32"), (vf, "r_vf"), (Bh32, "r_Bh32"), (Kh32, "r_Kh32")]):
                            sc.pe(lambda e, pk=pk, q_=q_, src=src, rc=rc, cs=cs: e.matmul(pk[:, q_ * 128:(q_ + 1) * 128], lhsT=src[rc][:, cs], rhs=ident, start=True, stop=True),
                                  [f"{stok}{rc}", "C"], [tkk])
                        pc = slice(rc * 128, (rc + 1) * 128)
                        sc.act(lambda e, pk=pk, pc=pc, b=b: e.copy(out=b["Vtm"][:, pc], in_=pk[:, 128:256]), [tkk], [(tk("Vtm"), rc)])
                        for hp in range(2):
                            h = 2 * rc + hp
                            zc = slice(h * 128 + hp * 64, h * 128 + hp * 64 + 64)
                            hs = slice(h * 64, (h + 1) * 64)
                            sc.act(lambda e, pk=pk, zc=zc, hp=hp, b=b: e.copy(out=b["Kez"][:, zc], in_=pk[:, hp * 64:hp * 64 + 64]), [tkk], [(tk("Kez"), h)])
                            sc.act(lambda e, pk=pk, zc=zc, hp=hp, b=b: e.copy(out=b["Vz"][:, zc], in_=pk[:, 128 + hp * 64:128 + hp * 64 + 64]), [tkk], [(tk("Vz"), h)])
                        sc.act(lambda e, pk=pk, b=b: e.copy(out=b["BKtm"][:], in_=pk[:, 256:512]), [tkk], [tk("BKtm")])
                        for hp in range(2):
                            h = 2 * rc + hp
                            hs = slice(h * 64, (h + 1) * 64)
                            for c in range(2):
                                ind = C[:, 256 + 64 * c:257 + 64 * c]
                                sc.dve(lambda e, hs=hs, hp=hp, c=c, ind=ind, b=b: e.tensor_scalar(
                                    out=b[f"Bhc{c}"][:, hs], in0=b["BKtm"][:, hp * 64:hp * 64 + 64], scalar1=ind, scalar2=None, op0=ALU.mult),
                                    [tk("BKtm"), "C"], [(tk(f"Bhc{c}"), h)])
                                sc.dve(lambda e, hs=hs, hp=hp, c=c, ind=ind, b=b: e.tensor_scalar(
                                    out=b[f"Khc{c}"][:, hs], in0=b["BKtm"][:, 128 + hp * 64:128 + hp * 64 + 64], scalar1=ind, scalar2=None, op0=ALU.mult),
                                    [tk("BKtm"), "C"], [(tk(f"Khc{c}"), h)])
                    banks = [[self.psum(), self.psum()] for _ in range(2)]
                    def dst(k, h):
                        (pa, ta), (pb_, tb) = banks[k]
                        return (pa[:, h * 128:(h + 1) * 128], ta) if h < 4 else (pb_[:, (h - 4) * 128:(h - 3) * 128], tb)
                    for h in range(6):
                        rc, hp = h // 2, h % 2
                        o1, to1 = dst(0, h)
                        sc.pe(lambda e, o1=o1, rc=rc, hp=hp, cs=cs: e.matmul(o1, lhsT=btz[rc][hp][:, cs], rhs=apb[rc][:, cs], start=True, stop=True),
                              [f"r_btz{rc}", f"r_apb{rc}"], [to1])
                        o2, to2 = dst(1, h)
                        sc.pe(lambda e, o2=o2, rc=rc, hp=hp, cs=cs: e.matmul(o2, lhsT=ktz[rc][hp][:, cs], rhs=apb[rc][:, cs], start=True, stop=True),
                              [f"r_ktz{rc}", f"r_apb{rc}"], [to2])
                    for h in range(6):
                        hc = slice(h * 128, (h + 1) * 128)
                        o1, to1 = dst(0, h)
                        sc.dve(lambda e, b=b, hc=hc, o1=o1: e.tensor_tensor(out=b["Mm"][:, hc], in0=o1, in1=nmstrict, op=ALU.mult),
                               [to1, "C"], [(tk("Mm"), h)])
                        o2, to2 = dst(1, h)
                        sc.dve(lambda e, b=b, hc=hc, o2=o2: e.tensor_tensor(out=b["AakT"][:, hc], in0=o2, in1=nmstrict, op=ALU.mult),
                               [to2, "C"], [(tk("AakT"), h)])
                    banks = [[self.psum(), self.psum()] for _ in range(2)]
                    for h in range(6):
                        rc, hp = h // 2, h % 2
                        o1, to1 = dst(0, h)
                        sc.pe(lambda e, o1=o1, rc=rc, hp=hp, cs=cs: e.matmul(o1, lhsT=btz[rc][hp][:, cs], rhs=rbar[rc][:, cs], start=True, stop=True),
                              [f"r_btz{rc}", f"r_rbar{rc}"], [to1])
                        o2, to2 = dst(1, h)
                        sc.pe(lambda e, o2=o2, rc=rc, hp=hp, cs=cs: e.matmul(o2, lhsT=ktz[rc][hp][:, cs], rhs=rbar[rc][:, cs], start=True, stop=True),
                              [f"r_ktz{rc}", f"r_rbar{rc}"], [to2])
                    for h in range(6):
                        hc = slice(h * 128, (h + 1) * 128)
                        o1, to1 = dst(0, h)
                        sc.dve(lambda e, b=b, hc=hc, o1=o1: e.tensor_tensor(out=b["AqT"][:, hc], in0=o1, in1=mincl, op=ALU.mult), [to1, "C"], [(tk("AqT"), h)])
                        o2, to2 = dst(1, h)
                        sc.dve(lambda e, b=b, hc=hc, o2=o2: e.tensor_tensor(out=b["AqkT"][:, hc], in0=o2, in1=mincl, op=ALU.mult), [to2, "C"], [(tk("AqkT"), h)])
                    self.neumann(b, tk, ident, ident6)
                    sc.act(lambda e, b=b: e.copy(out=b["Rbf"][:], in_=b["R"][:]), [tk("R")], [tk("Rbf")])
                    pW, tW = self.psum()
                    pX, tX = self.psum()
                    for h in range(6):
                        rc, hp = h // 2, h % 2
                        hs = slice(h * 64, (h + 1) * 64); hc = slice(h * 128, (h + 1) * 128)
                        sc.pe(lambda e, pW=pW, b=b, rc=rc, hp=hp, hc=hc: e.matmul(
                            pW[:, rc * 128:(rc + 1) * 128], lhsT=b["Kez"][:, hc], rhs=b["Rbf"][:, hc], start=(hp == 0), stop=(hp == 1)),
                            [(tk("Kez"), h), tk("Rbf")], [tW])
                        sc.pe(lambda e, pX=pX, b=b, hs=hs, hc=hc: e.matmul(pX[:, hs], lhsT=b["AakT"][:, hc], rhs=b["Vtm"][:, hs], start=True, stop=True),
                              [(tk("AakT"), h), (tk("Vtm"), h // 2)], [tX])
                    sc.act(lambda e, pW=pW, b=b: e.mul(out=b["WtT"][:], in_=pW[:, 0:384], mul=-1.0), [tW], [tk("WtT")])
                    sc.act(lambda e, pX=pX, b=b: e.copy(out=b["Xb"][:], in_=pX[:, 0:384]), [tX], [tk("Xb")])
                    pU, tU = self.psum()
                    for h in range(6):
                        hs = slice(h * 64, (h + 1) * 64); hc = slice(h * 128, (h + 1) * 128)
                        sc.pe(lambda e, pU=pU, b=b, hs=hs, hc=hc: e.matmul(pU[:, hs], lhsT=b["Rbf"][:, hc], rhs=b["Xb"][:, hs], start=True, stop=True),
                              [tk("Rbf"), tk("Xb")], [tU])
                    sc.act(lambda e, pU=pU, b=b: e.copy(out=b["Utb"][:], in_=pU[:, 0:384]), [tU], [tk("Utb")])
                    for rc in range(3):
                        sc.act(lambda e, b=b, rc=rc, cs=cs: e.copy(out=b["qbar"][:, rc * 128:(rc + 1) * 128], in_=rbar[rc][:, cs]), [f"r_rbar{rc}"], [tk("qbar")])
                    gl_ap = lambda h, c, n=n: epos[h // 2][(h % 2) * 64:(h % 2) * 64 + 64, n * 128 + c * 64 + 63:n * 128 + c * 64 + 64]
                    pre_ops = sc.cap_end()
                    sc.cap_begin(); self.ps_lo, self.ps_n = 4, 1
                    self.chunk_loop(b, tk, n, psO, H32, Hbd, "r", gl_ap, [f"r_epos{i}" for i in range(3)], beta=None, extra=True)
                    loop_ops = sc.cap_end()
                    self.ps_lo, self.ps_n = 0, 5
                    sc.replay(pend_loop, pre_ops)
                    pend_loop = loop_ops
                    if n == 3:
                        sc.replay(pend_loop)
                        pend_loop = []
                for rc in range(3 if self.stage >= 6 else 0):
                    po, pot = psO[rc]
                    sc.act(lambda e, po=po: e.copy(out=tA[:], in_=po[:]), [pot, "r_tA"], ["r_tA"])
                    p, ptok = self.psum()
                    sc.pe(lambda e, p=p: e.matmul(p[:], lhsT=onesbd, rhs=tA[:], start=True, stop=True), ["r_tA", "C"], [ptok])
                    sc.dve(lambda e, p=p: e.scalar_tensor_tensor(out=tA[:], in0=p[:], scalar=-1.0 / 64, in1=tA[:], op0=ALU.mult, op1=ALU.add), [ptok, "r_tA"], ["r_tA"])
                    sc.act(lambda e: e.activation(out=tB[:], in_=tA[:], func=AF.Square), ["r_tA", "r_tB"], ["r_tB"])
                    p2, p2tok = self.psum()
                    sc.pe(lambda e, p2=p2: e.matmul(p2[:], lhsT=onesbd, rhs=tB[:], start=True, stop=True), ["r_tB", "C"], [p2tok])
                    sc.act(lambda e, p2=p2: e.activation(out=tB[:], in_=p2[:], func=AF.Sqrt, bias=64e-5, scale=1.0 / 64), [p2tok, "r_tB"], ["r_tB"])
                    sc.dve(lambda e: e.reciprocal(out=tB[:], in_=tB[:]), ["r_tB"], ["r_tB"])
                    sc.dve(lambda e: e.tensor_tensor(out=tA[:], in0=tA[:], in1=tB[:], op=ALU.mult), ["r_tA", "r_tB"], ["r_tA"])
                    sc.dve(lambda e, rc=rc: e.tensor_scalar(out=tA[:], in0=tA[:], scalar1=rs_[:, GG + rc:GG + rc + 1], scalar2=None, op0=ALU.mult), ["r_tA", "rws"], ["r_tA"])
                    sc.dve(lambda e, rc=rc: e.tensor_scalar(out=tA[:], in0=tA[:], scalar1=rs_[:, GB + rc:GB + rc + 1], scalar2=None, op0=ALU.add), ["r_tA", "rws"], ["r_tA"])
                    sc.dve(lambda e, rc=rc: e.tensor_tensor(out=tA[:], in0=tA[:], in1=bonus[rc][:], op=ALU.add), ["r_tA", f"r_bonus{rc}"], ["r_tA"])
                    sc.dve(lambda e, rc=rc: e.tensor_tensor(out=sqt[:], in0=tA[:], in1=gf_[rc][:], op=ALU.mult), ["r_tA", f"r_gf{rc}", "r_sq"], ["r_sq"])
                    sc.dma(self.yT[rc * 128:(rc + 1) * 128, t0:t0 + T], sqt[:], reads=["r_sq"], writes=[("yT", rc, sbi)], q="pool")
            sc.flush()
        self.ps_lo, self.ps_n = 0, 8

    def chunk_loop(self, b, tk, n, psO, H32, Hbd, pf, gl_ap, gl_toks, beta=None, extra=False):
        sc = self.sc
        for c in range(2):
            tr = slice(c * 64, c * 64 + 64)
            pSU, tSU = self.psum()
            for rc in range(3):
                pc = slice(rc * 128, (rc + 1) * 128)
                sc.pe(lambda e, pSU=pSU, pc=pc: e.matmul(pSU[:, pc], lhsT=b["WtT"][:, pc], rhs=Hbd[:, pc], start=True, stop=True),
                      [tk("WtT"), (f"{pf}_Hbd", 2 * rc), (f"{pf}_Hbd", 2 * rc + 1)], [tSU])
            for h in range(6):
                rc, hp = h // 2, h % 2
                hs = slice(h * 64, (h + 1) * 64)
                src = pSU[tr, rc * 128 + hp * 64:rc * 128 + hp * 64 + 64]
                dst = b["Ubz"][tr, h * 128 + hp * 64:h * 128 + hp * 64 + 64]
                if beta is not None:
                    sc.dve(lambda e, src=src, dst=dst, h=h, hs=hs, tr=tr: e.scalar_tensor_tensor(
                        out=dst, in0=src, scalar=beta[tr, n, h:h + 1], in1=b["Utb"][tr, hs], op0=ALU.mult, op1=ALU.add),
                        [tSU, f"{pf}_beta", tk("Utb")], [(tk("Ubz"), h)])
                else:
                    sc.dve(lambda e, src=src, dst=dst, hs=hs, tr=tr: e.tensor_tensor(out=dst, in0=src, in1=b["Utb"][tr, hs], op=ALU.add),
                           [tSU, tk("Utb")], [(tk("Ubz"), h)])
            pSH, tSH = self.psum()
            for rc in range(3):
                pc = slice(rc * 128, (rc + 1) * 128)
                po, pot = psO[rc]
                oc = slice(n * 128 + c * 64, n * 128 + c * 64 + 64)
                qc = slice(rc * 128 + c * 64, rc * 128 + c * 64 + 64)
                hbt = [(f"{pf}_Hbd", 2 * rc), (f"{pf}_Hbd", 2 * rc + 1)]
                sc.pe(lambda e, po=po, pc=pc, oc=oc, qc=qc: e.matmul(po[:, oc], lhsT=Hbd[:, pc], rhs=b["qbar"][:, qc], start=True, stop=False),
                      hbt + [tk("qbar")], [pot])
                for hp in range(2):
                    h = 2 * rc + hp
                    hc = slice(h * 128, (h + 1) * 128)
                    ac = slice(h * 128 + c * 64, h * 128 + c * 64 + 64)
                    last = (hp == 1) and not extra
                    sc.pe(lambda e, po=po, hc=hc, oc=oc, ac=ac, last=last: e.matmul(po[:, oc], lhsT=b["Ubz"][:, hc], rhs=b["AqT"][:, ac], start=False, stop=last),
                          [(tk("Ubz"), h), (tk("AqT"), h)], [pot])
                    if extra:
                        sc.pe(lambda e, po=po, hc=hc, oc=oc, ac=ac, hp=hp: e.matmul(po[:, oc], lhsT=b["Vz"][:, hc], rhs=b["AqkT"][:, ac], start=False, stop=(hp == 1)),
                              [(tk("Vz"), h), (tk("AqkT"), h)], [pot])
                nmm = 4 if extra else 2
                k = 0
                for hp in range(2):
                    h = 2 * rc + hp
                    hc = slice(h * 128, (h + 1) * 128)
                    sc.pe(lambda e, pSH=pSH, pc=pc, hc=hc, c=c, k=k, nmm=nmm: e.matmul(pSH[:, pc], lhsT=b[f"Bhc{c}"][:, pc], rhs=b["Ubz"][:, hc], start=(k == 0), stop=(k == nmm - 1)),
                          [(tk(f"Bhc{c}"), h), (tk(f"Bhc{c}"), h ^ 1), (tk("Ubz"), h)], [tSH])
                    k += 1
                    if extra:
                        sc.pe(lambda e, pSH=pSH, pc=pc, hc=hc, c=c, k=k, nmm=nmm: e.matmul(pSH[:, pc], lhsT=b[f"Khc{c}"][:, pc], rhs=b["Vz"][:, hc], start=False, stop=(k == nmm - 1)),
                              [(tk(f"Khc{c}"), h), (tk(f"Khc{c}"), h ^ 1), (tk("Vz"), h)], [tSH])
                        k += 1
            for h in range(6):
                rc, hp = h // 2, h % 2
                rows = slice(hp * 64, hp * 64 + 64)
                cols = slice(rc * 128 + hp * 64, rc * 128 + hp * 64 + 64)
                gl = gl_ap(h, c)
                sc.dve(lambda e, pSH=pSH, rows=rows, cols=cols, gl=gl: e.scalar_tensor_tensor(
                    out=H32[rows, cols], in0=H32[rows, cols], scalar=gl, in1=pSH[rows, cols], op0=ALU.mult, op1=ALU.add),
                    [tSH, (f"{pf}_H32", h)] + list(gl_toks), [(f"{pf}_H32", h)])
                sc.act(lambda e, rows=rows, cols=cols: e.copy(out=Hbd[rows, cols], in_=H32[rows, cols]), [(f"{pf}_H32", h)], [(f"{pf}_Hbd", h)])

    def neumann(self, b, tk, ident, ident6):
        sc = self.sc
        allM = [(tk("Mm"), h) for h in range(6)]
        pa, ta = self.psum(); pb_, tb = self.psum()
        for h in range(6):
            o = pa[:, h * 128:(h + 1) * 128] if h < 4 else pb_[:, (h - 4) * 128:(h - 3) * 128]
            sc.pe(lambda e, o=o, h=h: e.matmul(o, lhsT=b["Mm"][:, h * 128:(h + 1) * 128], rhs=ident, start=True, stop=True), [(tk("Mm"), h), "C"], [ta if h < 4 else tb])
        sc.act(lambda e, pa=pa: e.copy(out=b["Mt"][:, 0:512], in_=pa[:, 0:512]), [ta], [tk("Mt")])
        sc.act(lambda e, pb_=pb_: e.copy(out=b["Mt"][:, 512:768], in_=pb_[:, 0:256]), [tb], [tk("Mt")])
        sc.dve(lambda e: e.tensor_tensor(out=b["R"][:], in0=b["Mm"][:], in1=ident6, op=ALU.add), allM + ["C"], [tk("R")])
        P, Pt, Ptok, Pttok = b["Mm"], b["Mt"], allM, [tk("Mt")]
        for lev in range(5):
            last = lev == 4
            pa, ta = self.psum(); pb_, tb = self.psum()
            for h in range(6):
                hc = slice(h * 128, (h + 1) * 128)
                o = pa[:, hc] if h < 4 else pb_[:, (h - 4) * 128:(h - 3) * 128]
                sc.pe(lambda e, o=o, hc=hc, P=P, Pt=Pt: e.matmul(o, lhsT=P[:, hc], rhs=Pt[:, hc], start=True, stop=True),
                      list(Ptok) + list(Pttok), [ta if h < 4 else tb])
            n2t = b["Pta"] if lev % 2 == 0 else b["Mt"]
            n2ttok = tk("Pta") if lev % 2 == 0 else tk("Mt")
            sc.act(lambda e, pa=pa, n2t=n2t: e.copy(out=n2t[:, 0:512], in_=pa[:, 0:512]), [ta], [n2ttok])
            sc.act(lambda e, pb_=pb_, n2t=n2t: e.copy(out=n2t[:, 512:768], in_=pb_[:, 0:256]), [tb], [n2ttok])
            if not last:
                pc, tc = self.psum(); pd, td = self.psum()
                for h in range(6):
                    hc = slice(h * 128, (h + 1) * 128)
                    o = pc[:, hc] if h < 4 else pd[:, (h - 4) * 128:(h - 3) * 128]
                    sc.pe(lambda e, o=o, hc=hc, P=P, Pt=Pt: e.matmul(o, lhsT=Pt[:, hc], rhs=P[:, hc], start=True, stop=True),
                          list(Ptok) + list(Pttok), [tc if h < 4 else td])
                n2 = b["Pa"] if lev % 2 == 0 else b["Mm"]
                n2tok = tk("Pa") if lev % 2 == 0 else tk("Mm_all")
            pe_, te = self.psum(); pf, tf = self.psum()
            for h in range(6):
                hc = slice(h * 128, (h + 1) * 128)
                o = pe_[:, hc] if h < 4 else pf[:, (h - 4) * 128:(h - 3) * 128]
                sc.pe(lambda e, o=o, hc=hc, n2t=n2t: e.matmul(o, lhsT=n2t[:, hc], rhs=b["R"][:, hc], start=True, stop=True),
                      [n2ttok, tk("R")], [te if h < 4 else tf])
            if not last:
                sc.act(lambda e, pc=pc, n2=n2: e.copy(out=n2[:, 0:512], in_=pc[:, 0:512]), [tc] + (list(Ptok) if n2 is P else []), [n2tok] + (list(Ptok) if n2 is P else []))
                sc.act(lambda e, pd=pd, n2=n2: e.copy(out=n2[:, 512:768], in_=pd[:, 0:256]), [td], [n2tok] + (list(Ptok) if n2 is P else []))
            sc.dve(lambda e, pe_=pe_: e.tensor_tensor(out=b["R"][:, 0:512], in0=b["R"][:, 0:512], in1=pe_[:, 0:512], op=ALU.add), [te, tk("R")], [tk("R")])
            sc.dve(lambda e, pf=pf: e.tensor_tensor(out=b["R"][:, 512:768], in0=b["R"][:, 512:768], in1=pf[:, 0:256], op=ALU.add), [tf, tk("R")], [tk("R")])
            if not last:
                P, Pt = n2, n2t
                Ptok, Pttok = [n2tok], [n2ttok]


    def build(self, mixers=True):
        self.consts()
        h_src = self.xT
        for l in range(self.L):
            self.phase_a(l, h_src)
            if "pT" in self.dbg:
                self.sc.dma(self.dbg_out["pT"], self.pT, reads=[], writes=[])
                self.sc.flush()
            self.phase_b(l)
            if "yT" in self.dbg:
                self.sc.dma(self.dbg_out["yT"], self.yT, reads=[], writes=[])
                self.sc.flush()
            h_dst = self.oT if l == self.L - 1 else self.hT[l % 2]
            self.phase_c(l, h_src, h_dst)
            h_src = h_dst
        return self.nc

    def phase_b(self, l):
        if len(self.mix) < 3:
            self.phase_stub(l)
        if "gdn" in self.mix:
            self.phase_gdn(l)
        if "rwkv" in self.mix:
            self.phase_rwkv(l)
        if "mla" in self.mix:
            if l == 0:
                self.mla_tables()
            self.phase_mla(l)

    def phase_stub(self, l):
        nc, sc, S = self.nc, self.sc, self.S
        with contextlib.ExitStack() as st:
            z = st.enter_context(nc.sbuf_tensor(f"stZ_L{l}", [128, 8, 512], BF16))
            sc.pool(lambda e: e.memset(z[:], 0.0), [], ["stZ"])
            for t in range(S // 512):
                t0 = t * 512
                sc.dma(self.yT[:, t0:t0 + 512].rearrange("(k p) s -> p k s", p=128), z[:], reads=["stZ"])
            sc.flush()


def _consts():
    C = np.zeros((128, NCST), np.float32)
    i = np.arange(128)
    same = (i[:, None] // 64) == (i[None, :] // 64)
    incl = same & (i[:, None] <= i[None, :])
    strict = same & (i[:, None] < i[None, :])
    C[:, 0:128] = np.eye(128)
    C[:, 128:256] = 1.0
    C[:, 256:384] = same
    C[:, 384:512] = -1.0 * strict
    C[:, 512:640] = np.where(incl, 0.0, -30000.0)
    C[:, 640:768] = 1.0 - np.eye(128)
    C[:, 768:896] = strict
    C[:, 896:1024] = incl
    C[:, 1024:1792] = np.tile(1.0 - np.eye(128), (1, 6))
    C[:, 1792:2560] = np.tile(np.eye(128), (1, 6))
    return C


def rwkv_host(mu, w0, w2, a0, a2, g2, k_k, k_a, r_k, gn_g, gn_b):
    L = mu.shape[0]
    c3 = lambda v: v.reshape(L, 3, 128).transpose(2, 0, 1)
    rws = np.zeros((128, L, 32), np.float32)
    rws[:, :, 0:11] = mu.reshape(L, 11, 128).transpose(2, 0, 1)
    for off, v in ((11, w0), (14, a0), (17, k_k), (20, k_a), (23, r_k.reshape(L, 384)), (26, gn_g), (29, gn_b)):
        rws[:, :, off:off + 3] = c3(v)
    rww = np.zeros((128, L, 1152), np.float32)
    rww[0:64, :, 0:384] = w2.transpose(1, 0, 2)
    rww[64:128, :, 384:768] = a2.transpose(1, 0, 2)
    rww[:, :, 768:1152] = g2.transpose(1, 0, 2)
    segm = np.ones((128, 512), np.float32)
    segm[:, ::64] = 0.0
    return {"rws": np.ascontiguousarray(rws.reshape(128, L * 32)), "rww": np.ascontiguousarray(rww.reshape(128, L * 1152)), "segm": segm}


def mla_host(q_norm_g, kv_norm_g, q_qk_g, k_qk_g, positions_row, S):
    L = q_norm_g.shape[0]
    mls = np.zeros((128, L, 8), np.float32)
    mls[:, :, 0:2] = q_norm_g.reshape(L, 2, 128).transpose(2, 0, 1)
    mls[:, :, 2] = kv_norm_g.T
    perm = np.concatenate([np.arange(64), 64 + (np.arange(32) + 16) % 32])
    mls[0:96, :, 3] = q_qk_g.T
    mls[0:96, :, 4] = q_qk_g[:, perm].T
    mls[0:96, :, 5] = k_qk_g.T
    mls[0:96, :, 6] = k_qk_g[:, perm].T
    mlc = np.zeros((128, 104), np.float32)
    inv_freq = (10000.0 ** (-np.arange(0, 32, 2, dtype=np.float32) / 32)).astype(np.float32)
    mlc[64:96, 0] = np.tile(inv_freq, 2)
    mlc[0:32, 8 + 64:8 + 96] = np.eye(32, dtype=np.float32)
    kk = np.arange(128)[:, None]
    qo = np.arange(512)[None, :]
    mm = np.concatenate([((2 * j + (kk >= 64)) <= (qo // 64)).astype(np.float32) for j in range(4)], axis=1)
    pos96 = np.ascontiguousarray(np.broadcast_to(positions_row.astype(np.int32)[None, :], (96, S)))
    return {"mls": np.ascontiguousarray(mls.reshape(128, L * 8)), "mlc": mlc, "mmask": np.ascontiguousarray(mm), "pos96": pos96}


def kernel(**inputs):
    x = np.asarray(inputs["x"], np.float32)
    B, S, _ = x.shape
    L = int(inputs["w_in"].shape[0])
    f = lambda k: np.ascontiguousarray(np.asarray(inputs[k], np.float32))
    col8 = lambda g: np.ascontiguousarray(g.reshape(L, 8, 128).transpose(2, 0, 1).reshape(128, L * 8))
    conv_w, a_log, dtb, og = f("gdn_conv_w"), f("gdn_a_log"), f("gdn_dt_bias"), f("gdn_o_norm_g")
    gsm = np.zeros((128, L, 13), np.float32)
    gsm[:, :, 0:6] = dtb[None]
    gsm[:, :, 6:12] = a_log[None]
    gsm[:, :, 12] = np.tile(og, (1, 2)).T
    shared = {
        "w_in": f("w_in"), "w_out": f("w_out"), "w_ff1": f("w_ff1"), "w_ff2": f("w_ff2"),
        "g_mix": col8(f("ln_mix_g")), "g_ffn": col8(f("ln_ffn_g")), "cst": _consts(),
        "cw": np.ascontiguousarray(conv_w.reshape(L, 4, 9, 128).transpose(3, 0, 2, 1).reshape(128, L * 36)),
        "gsm": np.ascontiguousarray(gsm.reshape(128, L * 13)),
        "w_uq": f("mla_w_uq"), "w_ukv": f("mla_w_ukv"),
    }
    shared.update(rwkv_host(f("rwkv_mu"), f("rwkv_w0"), f("rwkv_w2"), f("rwkv_a0"), f("rwkv_a2"), f("rwkv_g2"), f("rwkv_k_k"),
                            f("rwkv_k_a"), f("rwkv_r_k"), f("rwkv_gn_g"), f("rwkv_gn_b")))
    positions = np.asarray(inputs["positions"]).astype(np.int32)
    prog = Prog(S, L)
    nc = prog.build()
    in_maps = []
    for b in range(B):
        m = dict(shared, xT=np.ascontiguousarray(x[b].T))
        m.update(mla_host(f("mla_q_norm_g"), f("mla_kv_norm_g"), f("mla_q_qk_g"), f("mla_k_qk_g"), positions[b], S))
        in_maps.append(m)
    res = run_bass_kernel_spmd(nc, in_maps, core_ids=list(range(B)))
    return np.stack([np.ascontiguousarray(r["oT"].T) for r in res.results], axis=0).astype(np.float32)
```

```python
import contextlib
import numpy as np
import concourse.bass as bass
import concourse.mybir as mybir
from concourse.bass_utils import run_bass_kernel_spmd

F32 = mybir.dt.float32
BF16 = mybir.dt.bfloat16
I32 = mybir.dt.int32
AF = mybir.ActivationFunctionType
ALU = mybir.AluOpType
AX = mybir.AxisListType

D = 1024
DFF = 4096
P_IN = 3372
P_EXT = P_IN + 32
NORM_EPS = 1e-6
NCST = 128 * 8 + 768 * 2

ENGS = ("pe", "act", "dve", "pool", "sp")
CENG = ("pe", "act", "dve", "pool")
BLK = 8192
NROT = 8
NDMA = 12


class Op:
    __slots__ = ("eng", "fn", "reads", "writes", "idx", "deps", "signal", "dma",
                 "k", "dslot", "dval")

    def __init__(self, eng, fn, reads, writes, dma):
        self.eng, self.fn, self.reads, self.writes, self.dma = eng, fn, reads, writes, dma
        self.deps = ()
        self.signal = False
        self.k = -1


class Sched:
    def __init__(self, nc):
        self.nc = nc
        self.ops = []
        self.last_w = {}
        self.readers = {}
        self.nsig = {e: 0 for e in ENGS}
        self.ndma = {e: 0 for e in ENGS}
        self.waited_c = {e: {} for e in ENGS}
        self.waited_d = {e: {} for e in ENGS}
        self.nbar = 0
        self.ntot = {e: 0 for e in ENGS}
        self.csem = {e: [nc.semaphore(f"c_{e}_{i}").__enter__() for i in range(NROT)] for e in CENG}
        self.dsem = {e: [nc.semaphore(f"d_{e}_{i}").__enter__() for i in range(NDMA)]
                     for e in ("sp", "pool")}
        self.bsem = nc.semaphore("bar").__enter__()
        self.cap = None

    def cap_begin(self):
        self.cap = []

    def cap_end(self):
        c, self.cap = self.cap, None
        return c

    def replay(self, a, b=()):
        na, nb = len(a), len(b)
        i = j = 0
        while i < na or j < nb:
            if j >= nb or (i < na and i * nb <= j * na):
                self.add(*a[i]); i += 1
            else:
                self.add(*b[j]); j += 1

    def add(self, eng, fn, reads=(), writes=(), dma=False):
        if self.cap is not None:
            self.cap.append((eng, fn, tuple(reads), tuple(writes), dma))
            return None
        op = Op(eng, fn, tuple(reads), tuple(writes), dma)
        if dma:
            op.signal = True
        op.idx = len(self.ops)
        deps = set()
        lw, rd = self.last_w, self.readers
        for r in op.reads:
            p = lw.get(r)
            if p is not None:
                deps.add(p)
        for w in op.writes:
            p = lw.get(w)
            if p is not None:
                deps.add(p)
            for q in rd.get(w, ()):
                deps.add(q)
        ops = self.ops
        keep = []
        for d in deps:
            p = ops[d]
            if p.eng == eng and not p.dma and not dma:
                if eng == "pe":
                    continue
            keep.append(d)
        op.deps = tuple(sorted(keep))
        for d in op.deps:
            ops[d].signal = True
        for r in op.reads:
            rd.setdefault(r, []).append(op.idx)
        for w in op.writes:
            lw[w] = op.idx
            rd[w] = []
        ops.append(op)
        return op

    def pe(self, fn, reads=(), writes=()):
        return self.add("pe", fn, reads, writes)

    def act(self, fn, reads=(), writes=()):
        return self.add("act", fn, reads, writes)

    def dve(self, fn, reads=(), writes=()):
        return self.add("dve", fn, reads, writes)

    def pool(self, fn, reads=(), writes=()):
        return self.add("pool", fn, reads, writes)

    def dma(self, out, in_, reads=(), writes=(), q="sp", **kw):
        return self.add(q, lambda e: e.dma_start(out=out, in_=in_, **kw), reads, writes, dma=True)

    def _csig(self, op):
        j = op.k // BLK
        return self.csem[op.eng][j % NROT], (j // NROT) * BLK + (op.k % BLK) + 1

    def flush(self):
        nc = self.nc
        ops = self.ops
        per = {e: [] for e in ENGS}
        for op in ops:
            per[op.eng].append(op)
        for e in CENG:
            for op in reversed(per[e]):
                if not op.dma:
                    op.signal = True
                    break
        for op in ops:
            if op.dma:
                n = self.ndma[op.eng]
                op.dslot = n % NDMA
                op.dval = 16 * (n // NDMA + 1)
                self.ndma[op.eng] = n + 1
            elif op.signal:
                op.k = self.nsig[op.eng]
                self.nsig[op.eng] += 1
        for e in ENGS:
            self.ntot[e] += len(per[e])
        self.nbar += 1
        nbar = self.nbar
        dsem, bsem = self.dsem, self.bsem

        def run(engname, e):
            waited_c = self.waited_c[engname]
            waited_d = self.waited_d[engname]

            def wait_for(p):
                if p.dma:
                    key = (p.eng, p.dslot)
                    if waited_d.get(key, 0) >= p.dval:
                        return
                    waited_d[key] = p.dval
                    e.wait_ge(dsem[p.eng][p.dslot], p.dval)
                else:
                    if waited_c.get(p.eng, -1) >= p.k:
                        return
                    waited_c[p.eng] = p.k
                    s, v = self._csig(p)
                    e.wait_ge(s, v)

            last_c = None
            for op in per[engname]:
                for d in op.deps:
                    wait_for(ops[d])
                if op.dma:
                    if op.dval > 16:
                        key = (op.eng, op.dslot)
                        if waited_d.get(key, 0) < op.dval - 16:
                            waited_d[key] = op.dval - 16
                            e.wait_ge(dsem[op.eng][op.dslot], op.dval - 16)
                    op.fn(e).then_inc(dsem[op.eng][op.dslot], 16)
                else:
                    ins = op.fn(e)
                    if op.signal:
                        s, v = self._csig(op)
                        ins.then_inc(s, 1)
                        last_c = op
            if last_c is not None:
                wait_for(last_c)
            lastd = {}
            for op in per[engname]:
                if op.dma:
                    lastd[op.dslot] = op
            for op in lastd.values():
                wait_for(op)
            e.sem_inc(bsem, 1)
            e.wait_ge(bsem, 5 * nbar)

        with nc.Block() as block:
            block.tensor(lambda e: run("pe", e))
            block.scalar(lambda e: run("act", e))
            block.vector(lambda e: run("dve", e))
            block.gpsimd(lambda e: run("pool", e))
            block.sync(lambda e: run("sp", e))
        self.ops = []
        self.last_w = {}
        self.readers = {}


IN_CHUNKS = ([(i * 128, 128) for i in range(14)] + [(1792, 32)] +
             [(1824 + i * 128, 128) for i in range(12)] + [(3360, 12), (3372, 32)])


class Prog:
    def __init__(self, S, L, dbg=(), mix=("gdn", "rwkv", "mla")):
        self.S, self.L, self.dbg = S, L, set(dbg)
        self.mix = set(mix)
        self.stage = 9
        self.sub = 9
        nc = self.nc = bass.Bass("TRN2", target_bir_lowering=False)
        self.sc = Sched(nc)
        dt = nc.dram_tensor
        self.xT = dt("xT", [D, S], F32, kind="ExternalInput").ap()
        self.w_in = dt("w_in", [L, D, P_IN], F32, kind="ExternalInput").ap()
        self.w_out = dt("w_out", [L, D, D], F32, kind="ExternalInput").ap()
        self.w_ff1 = dt("w_ff1", [L, D, DFF], F32, kind="ExternalInput").ap()
        self.w_ff2 = dt("w_ff2", [L, DFF, D], F32, kind="ExternalInput").ap()
        self.g_mix = dt("g_mix", [128, L * 8], F32, kind="ExternalInput").ap()
        self.g_ffn = dt("g_ffn", [128, L * 8], F32, kind="ExternalInput").ap()
        self.oT = dt("oT", [D, S], F32, kind="ExternalOutput").ap()
        self.cst = dt("cst", [128, NCST], F32, kind="ExternalInput").ap()
        self.cw_d = dt("cw", [128, L * 36], F32, kind="ExternalInput").ap()
        self.gsm_d = dt("gsm", [128, L * 13], F32, kind="ExternalInput").ap()
        self.baT = dt("baT", [S, 12], F32).ap()
        self.rws_d = dt("rws", [128, L * 32], F32, kind="ExternalInput").ap()
        self.rww = dt("rww", [128, L * 1152], F32, kind="ExternalInput").ap()
        self.segm_d = dt("segm", [128, 512], F32, kind="ExternalInput").ap()
        self.w_uq = dt("w_uq", [L, 256, 384], F32, kind="ExternalInput").ap()
        self.w_ukv = dt("w_ukv", [L, 128, 512], F32, kind="ExternalInput").ap()
        self.mls_d = dt("mls", [128, L * 8], F32, kind="ExternalInput").ap()
        self.mlc_d = dt("mlc", [128, 104], F32, kind="ExternalInput").ap()
        self.pos96 = dt("pos96", [96, S], I32, kind="ExternalInput").ap()
        self.mmask = dt("mmask", [128, 2048], F32, kind="ExternalInput").ap()
        self.cosT = dt("cosT", [96, S], F32).ap()
        self.sinT = dt("sinT", [96, S], F32).ap()
        self.qfT = dt("qfT", [4, 96, S], BF16).ap()
        self.kfT = dt("kfT", [4, 96, S], BF16).ap()
        self.vxT = dt("vxT", [4, S, 128], BF16).ap()
        self.pT = dt("pT", [P_EXT, S], F32).ap()
        self.yT = dt("yT", [D, S], BF16).ap()
        self.hT = [dt(f"hT{i}", [D, S], F32).ap() for i in range(2)]
        self.dbg_out = {}
        if "pT" in self.dbg:
            self.dbg_out["pT"] = dt("dbg_pT", [P_EXT, S], F32, kind="ExternalOutput").ap()
        if "yT" in self.dbg:
            self.dbg_out["yT"] = dt("dbg_yT", [D, S], BF16, kind="ExternalOutput").ap()
        self.ps = [nc.alloc_psum_tensor(f"ps{i}", [128, 512], F32) for i in range(8)]
        self.ps_i = 0
        self.ps_lo, self.ps_n = 0, 8
        self.ps_lo = 0

    def psum(self):
        i = self.ps_lo + self.ps_i % self.ps_n
        self.ps_i += 1
        return self.ps[i], ("ps", i)

    def consts(self):
        nc, sc = self.nc, self.sc
        L = self.L
        self.ones_bf = nc.alloc_sbuf_tensor("ones_bf", [128, 128], BF16)
        self.gm = nc.alloc_sbuf_tensor("gm", [128, L * 8], F32)
        self.gf = nc.alloc_sbuf_tensor("gf", [128, L * 8], F32)
        sc.pool(lambda e: e.memset(self.ones_bf[:], 1.0), [], ["ones_bf"])
        self.C = nc.alloc_sbuf_tensor("cstt", [128, NCST], F32)
        self.cw = nc.alloc_sbuf_tensor("cwt", [128, L * 36], F32)
        self.gsm = nc.alloc_sbuf_tensor("gsmt", [128, L * 13], F32)
        self.onesbd_bf = nc.alloc_sbuf_tensor("onesbd_bf", [128, 128], BF16)
        self.rws = nc.alloc_sbuf_tensor("rwst", [128, L * 32], F32)
        self.segm = nc.alloc_sbuf_tensor("segmt", [128, 512], F32)
        sc.dma(self.rws[:], self.rws_d, writes=["rws"])
        sc.dma(self.segm[:], self.segm_d, writes=["segm"])
        self.mls = nc.alloc_sbuf_tensor("mlst", [128, L * 8], F32)
        self.mlc = nc.alloc_sbuf_tensor("mlct", [128, 104], F32)
        sc.dma(self.mls[:], self.mls_d, writes=["mls"])
        sc.dma(self.mlc[:], self.mlc_d, writes=["mlc"])
        sc.dma(self.C[:], self.cst, writes=["C"])
        sc.dma(self.cw[:], self.cw_d, writes=["cw"])
        sc.dma(self.gsm[:], self.gsm_d, writes=["gsm"])
        sc.dve(lambda e: e.tensor_copy(out=self.onesbd_bf[:], in_=self.C[:, 256:384]), ["C"], ["onesbd_bf"])
        sc.dma(self.gm[:], self.g_mix, writes=["gm"])
        sc.dma(self.gf[:], self.g_ffn, writes=["gf"])
        sc.flush()

    def rms_stats(self, hx, hx_tok, sq, sq_tok, rs, rs_tok, T):
        sc = self.sc
        sc.act(lambda e: e.activation(out=sq[:, :, 0:T], in_=hx[:, :, 0:T], func=AF.Square), [hx_tok], [sq_tok])
        p, ptok = self.psum()
        for kc in range(8):
            sc.pe(lambda e, kc=kc: e.matmul(p[:, 0:T], lhsT=self.ones_bf[:], rhs=sq[:, kc, 0:T],
                                              start=(kc == 0), stop=(kc == 7)), [sq_tok, "ones_bf"], [ptok])
        sc.act(lambda e: e.activation(out=rs[:, 0:T], in_=p[:, 0:T], func=AF.Sqrt, bias=NORM_EPS, scale=1.0 / D),
               [ptok], [rs_tok])
        sc.dve(lambda e: e.reciprocal(out=rs[:, 0:T], in_=rs[:, 0:T]), [rs_tok], [rs_tok])

    def load_weight(self, dst, dst_name, src, KC, cols, gcol, gtok, stg, col_off=0):
        sc = self.sc
        n = 0
        CH = stg[0][0].shape[1]
        for kc in range(KC):
            for c0 in range(0, cols, CH):
                c1 = min(cols, c0 + CH)
                st, sttok = stg[n % len(stg)]
                n += 1
                sc.dma(st[:, 0:c1 - c0], src[kc * 128:(kc + 1) * 128, c0:c1], writes=[sttok])
                if n % 3 == 0:
                    if gcol is not None:
                        sc.act(lambda e, st=st, kc=kc, c0=c0, c1=c1: e.mul(out=dst[:, kc, col_off + c0:col_off + c1], in_=st[:, 0:c1 - c0],
                                                                           mul=gcol[:, kc:kc + 1]), [sttok, gtok], [(dst_name, kc)])
                    else:
                        sc.act(lambda e, st=st, kc=kc, c0=c0, c1=c1: e.copy(out=dst[:, kc, col_off + c0:col_off + c1], in_=st[:, 0:c1 - c0]),
                               [sttok], [(dst_name, kc)])
                elif gcol is not None:
                    sc.dve(lambda e, st=st, kc=kc, c0=c0, c1=c1: e.tensor_scalar(
                        out=dst[:, kc, col_off + c0:col_off + c1], in0=st[:, 0:c1 - c0],
                        scalar1=gcol[:, kc:kc + 1], scalar2=None, op0=ALU.mult),
                        [sttok, gtok], [(dst_name, kc)])
                else:
                    sc.dve(lambda e, st=st, kc=kc, c0=c0, c1=c1: e.tensor_copy(
                        out=dst[:, kc, col_off + c0:col_off + c1], in_=st[:, 0:c1 - c0]),
                        [sttok], [(dst_name, kc)])

    def phase_a(self, l, h_src):
        nc, sc, S = self.nc, self.sc, self.S
        with contextlib.ExitStack() as st:
            sb = lambda n, s, d=F32: st.enter_context(nc.sbuf_tensor(f"{n}_L{l}", s, d))
            wi = sb("wi", [128, 8, P_EXT], BF16)
            stg = [(sb(f"stgA{i}", [128, 2048]), f"stgA{i}") for i in range(2)]
            hx = [sb(f"hxA{i}", [128, 8, 512]) for i in range(2)]
            sq = [sb(f"sqA{i}", [128, 8, 512], BF16) for i in range(2)]
            xb = [sb(f"xbA{i}", [128, 8, 512], BF16) for i in range(2)]
            rs = [sb(f"rsA{i}", [128, 512]) for i in range(2)]
            ev = [sb(f"evA{i}", [128, 512]) for i in range(4)]
            bat = [sb(f"batA{i}", [128, 64]) for i in range(2)]
            gcol = self.gm[:, l * 8:(l + 1) * 8]
            self.load_weight(wi, "wi", self.w_in[l], 8, P_IN, gcol, "gm", stg)
            allwi = [("wi", kc) for kc in range(8)]
            sc.dve(lambda e: e.tensor_scalar(out=wi[:, :, 3372:3388], in0=wi[:, :, 1808:1824], scalar1=-1.0,
                                              scalar2=None, op0=ALU.mult), allwi, allwi)
            sc.dve(lambda e: e.tensor_copy(out=wi[:, :, 3388:3404], in_=wi[:, :, 1792:1808]), allwi, allwi)
            nev = 0
            for t in range(S // 512):
                b = t % 2
                t0 = t * 512
                sc.dma(hx[b][:], h_src[:, t0:t0 + 512].rearrange("(k p) s -> p k s", p=128), writes=[f"hxA{b}"])
                self.rms_stats(hx[b], f"hxA{b}", sq[b], f"sqA{b}", rs[b], f"rsA{b}", 512)
                for kc in range(8):
                    eng = sc.dve if kc % 2 == 0 else sc.pool
                    eng(lambda e, kc=kc, b=b: e.tensor_tensor(out=xb[b][:, kc, :], in0=hx[b][:, kc, :], in1=rs[b][:],
                                                               op=ALU.mult), [f"hxA{b}", f"rsA{b}"], [(f"xbA{b}", kc)])
                p, ptok = self.psum()
                for n in range(4):
                    for kc in range(8):
                        sc.pe(lambda e, p=p, kc=kc, n=n, b=b: e.matmul(
                            p[:, n * 16:n * 16 + 12], lhsT=xb[b][:, kc, n * 128:(n + 1) * 128], rhs=wi[:, kc, 3360:3372],
                            start=(kc == 0), stop=(kc == 7)), [("wi", kc), (f"xbA{b}", kc)], [ptok])
                sc.dve(lambda e, p=p, b=b: e.tensor_copy(out=bat[b][:].rearrange("p (n c) -> p n c", c=16)[:, :, 0:12], in_=p[:, 0:64].rearrange("p (n c) -> p n c", c=16)[:, :, 0:12]), [ptok], [f"batA{b}"])
                sc.dma(self.baT[t0:t0 + 512, :].rearrange("(n p) c -> p n c", p=128),
                       bat[b][:].rearrange("p (n c) -> p n c", c=16)[:, :, 0:12], reads=[f"batA{b}"], writes=[("baT", t)], q="pool")
                for (c0, m) in IN_CHUNKS:
                    p, ptok = self.psum()
                    for kc in range(8):
                        sc.pe(lambda e, p=p, kc=kc, c0=c0, m=m, b=b: e.matmul(
                            p[0:m, :], lhsT=wi[:, kc, c0:c0 + m], rhs=xb[b][:, kc, :], start=(kc == 0), stop=(kc == 7)),
                            [("wi", kc), (f"xbA{b}", kc)], [ptok])
                    e_i = nev % 4
                    evt = ev[e_i]
                    if nev % 2 == 0:
                        sc.dve(lambda e, p=p, m=m, evt=evt: e.tensor_copy(out=evt[0:m, :], in_=p[0:m, :]), [ptok], [f"evA{e_i}"])
                    else:
                        sc.act(lambda e, p=p, m=m, evt=evt: e.copy(out=evt[0:m, :], in_=p[0:m, :]), [ptok], [f"evA{e_i}"])
                    nev += 1
                    sc.dma(self.pT[c0:c0 + m, t0:t0 + 512], evt[0:m, :], reads=[f"evA{e_i}"], writes=[("pT", c0, t)], q="pool")
            sc.flush()

    def phase_c(self, l, h_src, h_dst):
        nc, sc, S = self.nc, self.sc, self.S
        T = 256
        with contextlib.ExitStack() as st:
            sb = lambda n, s, d=F32: st.enter_context(nc.sbuf_tensor(f"{n}_L{l}", s, d))
            wo = sb("wo", [128, 8, D], BF16)
            w1 = sb("w1", [128, 8, DFF], BF16)
            w2 = sb("w2", [128, 32, D], BF16)
            hid = sb("hid", [128, 32, T], BF16)
            hx = sb("hxC", [128, 8, T])
            sq = sb("sqC", [128, 8, T], BF16)
            xb = sb("xbC", [128, 8, T], BF16)
            yb = sb("ybC", [128, 8, T], BF16)
            rs = sb("rsC", [128, T])
            relu_t = [sb(f"reluC{i}", [128, T]) for i in range(2)]
            stg = [(sb(f"stgC{i}", [128, 1024]), f"stgC{i}") for i in range(2)]
            self.load_weight(wo, "wo", self.w_out[l], 8, D, None, None, stg)
            self.load_weight(w1, "w1", self.w_ff1[l], 8, DFF, self.gf[:, l * 8:(l + 1) * 8], "gf", stg)
            self.load_weight(w2, "w2", self.w_ff2[l], 32, D, None, None, stg)
            for t in range(S // T):
                t0 = t * T
                sc.dma(hx[:], h_src[:, t0:t0 + T].rearrange("(k p) s -> p k s", p=128), writes=["hxC"])
                sc.dma(yb[:], self.yT[:, t0:t0 + T].rearrange("(k p) s -> p k s", p=128), writes=["ybC"], q="pool")
                for oc in range(8):
                    p, ptok = self.psum()
                    for kc in range(8):
                        sc.pe(lambda e, p=p, kc=kc, oc=oc: e.matmul(
                            p[:, 0:T], lhsT=wo[:, kc, oc * 128:(oc + 1) * 128], rhs=yb[:, kc, :], start=(kc == 0), stop=(kc == 7)),
                            [("wo", kc), "ybC"], [ptok])
                    sc.dve(lambda e, p=p, oc=oc: e.tensor_tensor(out=hx[:, oc, :], in0=hx[:, oc, :], in1=p[:, 0:T], op=ALU.add),
                           [ptok, ("hxC", oc), "hxC"], [("hxC", oc)])
                hx_all = ["hxC"] + [("hxC", oc) for oc in range(8)]
                sc.act(lambda e: e.activation(out=sq[:], in_=hx[:], func=AF.Square), hx_all, ["sqC"])
                p, ptok = self.psum()
                for kc in range(8):
                    sc.pe(lambda e, p=p, kc=kc: e.matmul(p[:, 0:T], lhsT=self.ones_bf[:], rhs=sq[:, kc, :],
                                                          start=(kc == 0), stop=(kc == 7)), ["sqC", "ones_bf"], [ptok])
                sc.act(lambda e, p=p: e.activation(out=rs[:], in_=p[:, 0:T], func=AF.Sqrt, bias=NORM_EPS, scale=1.0 / D),
                       [ptok], ["rsC"])
                sc.dve(lambda e: e.reciprocal(out=rs[:], in_=rs[:]), ["rsC"], ["rsC"])
                for kc in range(8):
                    eng = sc.dve if kc % 2 == 0 else sc.pool
                    eng(lambda e, kc=kc: e.tensor_tensor(out=xb[:, kc, :], in0=hx[:, kc, :], in1=rs[:], op=ALU.mult),
                        hx_all + ["rsC"], [("xbC", kc)])
                for oc in range(32):
                    p, ptok = self.psum()
                    for kc in range(8):
                        sc.pe(lambda e, p=p, kc=kc, oc=oc: e.matmul(
                            p[:, 0:T], lhsT=w1[:, kc, oc * 128:(oc + 1) * 128], rhs=xb[:, kc, :], start=(kc == 0), stop=(kc == 7)),
                            [("w1", kc), ("xbC", kc)], [ptok])
                    rl, rltok = relu_t[oc % 2], f"reluC{oc % 2}"
                    sc.act(lambda e, p=p, rl=rl: e.activation(out=rl[:], in_=p[:, 0:T], func=AF.Relu), [ptok], [rltok])
                    sc.pool(lambda e, oc=oc, rl=rl: e.tensor_tensor(out=hid[:, oc, :], in0=rl[:], in1=rl[:], op=ALU.mult),
                            [rltok], [("hid", oc)])
                for oc in range(8):
                    p, ptok = self.psum()
                    for kc in range(32):
                        sc.pe(lambda e, p=p, kc=kc, oc=oc: e.matmul(
                            p[:, 0:T], lhsT=w2[:, kc, oc * 128:(oc + 1) * 128], rhs=hid[:, kc, :], start=(kc == 0), stop=(kc == 31)),
                            [("w2", kc), ("hid", kc)], [ptok])
                    sc.dve(lambda e, p=p, oc=oc: e.tensor_tensor(out=hx[:, oc, :], in0=hx[:, oc, :], in1=p[:, 0:T], op=ALU.add),
                           [ptok, ("hxC", oc), "hxC"], [("hxC", oc)])
                sc.dma(h_dst[:, t0:t0 + T].rearrange("(k p) s -> p k s", p=128), hx[:], reads=hx_all, writes=[("hdst", t)], q="pool")
            sc.flush()

    def phase_gdn(self, l):
        nc, sc, S = self.nc, self.sc, self.S
        C = self.C
        ident, ones, onesbd = C[:, 0:128], C[:, 128:256], C[:, 256:384]
        mneg, useg = C[:, 512:640], C[:, 896:1024]
        offd6, ident6 = C[:, 1024:1792], C[:, 1792:2560]
        self.ps_n = 5
        psO = [(self.ps[5 + i], f"psO{i}") for i in range(3)]
        cwl = self.cw[:, l * 36:(l + 1) * 36]
        gs = self.gsm[:, l * 13:(l + 1) * 13]
        with contextlib.ExitStack() as st:
            sb = lambda n, s, d=F32: st.enter_context(nc.sbuf_tensor(f"{n}_L{l}", s, d))
            xin = [sb(f"g_xin{i}", [128, 515]) for i in range(2)]
            acc = [sb(f"g_acc{i}", [128, 512]) for i in range(2)]
            qf32 = [sb(f"g_qf{i}", [128, 512]) for i in range(3)]
            kf32 = [sb(f"g_kf{i}", [128, 512]) for i in range(3)]
            vf32 = [sb(f"g_vf{i}", [128, 512]) for i in range(3)]
            qfb = [sb(f"g_qb{i}", [128, 512], BF16) for i in range(3)]
            kfb = [sb(f"g_kb{i}", [128, 512], BF16) for i in range(3)]
            sqt = sb("g_sq", [128, 512], BF16)
            rinv = sb("g_rinv", [128, 512])
            batm = sb("g_batm", [128, 4, 12])
            beta = sb("g_beta", [128, 4, 6]); nbeta = sb("g_nbeta", [128, 4, 6]); gtm = sb("g_gtm", [128, 4, 6])
            tmp46 = sb("g_tmp46", [128, 4, 6])
            nA = sb("g_nA", [128, 6])
            H32 = sb("g_H32", [128, 3 * 128]); Hbd = sb("g_Hbd", [128, 3 * 128], BF16)
            kz = [[sb(f"g_kz{i}_{hp}", [128, 512], BF16) for hp in range(2)] for i in range(3)]
            osb = sb("g_osb", [128, 512]); gate = sb("g_gate", [128, 512]); ybf = sb("g_ybf", [128, 512], BF16)
            B = []
            for pb in range(2):
                d = {}
                for nm, shp, dtp in [("gcc", [128, 6], F32), ("gct", [128, 6], F32), ("egc", [128, 6], F32), ("eend", [128, 6], F32),
                                     ("GU", [128, 768], F32), ("Egc", [128, 768], F32), ("dd", [128, 768], F32), ("DmT", [128, 768], F32),
                                     ("DmS", [128, 768], F32), ("Ktm", [128, 384], F32), ("Vtm", [128, 384], BF16),
                                     ("Kez", [128, 768], BF16), ("Bhc0", [128, 384], BF16), ("Bhc1", [128, 384], BF16),
                                     ("eendc0", [128, 6], F32), ("eendc1", [128, 6], F32), ("Mm", [128, 768], F32), ("Mt", [128, 768], F32),
                                     ("R", [128, 768], F32), ("Pa", [128, 768], F32), ("Pta", [128, 768], F32), ("Rbf", [128, 768], BF16),
                                     ("AqT", [128, 768], BF16), ("WtT", [128, 384], BF16), ("Utb", [128, 384], F32),
                                     ("qbar", [128, 384], BF16), ("Ubz", [128, 768], BF16)]:
                    d[nm] = sb(f"g_{nm}{pb}", shp, dtp)
                B.append(d)
            sc.act(lambda e: e.activation(out=nA[:], in_=gs[:, 6:12], func=AF.Exp), ["gsm"], ["g_nA"])
            sc.dve(lambda e: e.tensor_scalar(out=nA[:], in0=nA[:], scalar1=-1.0, scalar2=None, op0=ALU.mult), ["g_nA"], ["g_nA"])
            sc.dve(lambda e: e.memset(H32[:], 0.0), [], [("g_H32", h) for h in range(6)])
            sc.pool(lambda e: e.memset(Hbd[:], 0.0), [], [("g_Hbd", h) for h in range(6)])
            for i in range(3):
                for hp in range(2):
                    sc.pool(lambda e, i=i, hp=hp: e.memset(kz[i][hp][:], 0.0), [], [f"g_kz{i}"])
            for pb in range(2):
                sc.pool(lambda e, pb=pb: e.memset(B[pb]["Kez"][:], 0.0), [], [(f"g_Kez{pb}", h) for h in range(6)])
                sc.pool(lambda e, pb=pb: e.memset(B[pb]["Ubz"][:], 0.0), [], [(f"g_Ubz{pb}", h) for h in range(6)])
            nblk = 0
            pend_loop = []
            for sbi in range(S // 512):
                t0 = sbi * 512
                for rc in range(9):
                    xi = xin[rc % 2]; xt = f"g_xin{rc % 2}"
                    ac = acc[rc % 2]; at_ = f"g_acc{rc % 2}"
                    r0 = 1824 + rc * 128
                    if sbi == 0:
                        sc.pool(lambda e, xi=xi: e.memset(xi[:, 0:3], 0.0), [], [xt])
                        sc.dma(xi[:, 3:515], self.pT[r0:r0 + 128, 0:512], reads=[xt], writes=[xt])
                    else:
                        sc.dma(xi[:], self.pT[r0:r0 + 128, t0 - 3:t0 + 512], writes=[xt])
                    cb = rc * 4
                    sc.dve(lambda e, xi=xi, ac=ac, cb=cb: e.tensor_scalar(out=ac[:], in0=xi[:, 3:515], scalar1=cwl[:, cb + 3:cb + 4],
                                                                         scalar2=None, op0=ALU.mult), [xt, "cw"], [at_])
                    for j in range(3):
                        sc.dve(lambda e, xi=xi, ac=ac, cb=cb, j=j: e.scalar_tensor_tensor(
                            out=ac[:], in0=xi[:, j:j + 512], scalar=cwl[:, cb + j:cb + j + 1], in1=ac[:], op0=ALU.mult, op1=ALU.add),
                            [xt, "cw", at_], [at_])
                    kind, i3 = rc // 3, rc % 3
                    if kind == 2:
                        sc.act(lambda e, ac=ac, i3=i3: e.activation(out=vf32[i3][:], in_=ac[:], func=AF.Silu), [at_], [f"g_vf{i3}"])
                        continue
                    sc.act(lambda e, ac=ac: e.activation(out=ac[:], in_=ac[:], func=AF.Silu), [at_], [at_])
                    sc.act(lambda e, ac=ac: e.activation(out=sqt[:], in_=ac[:], func=AF.Square), [at_], ["g_sq"])
                    p, ptok = self.psum()
                    sc.pe(lambda e, p=p: e.matmul(p[:], lhsT=self.onesbd_bf[:], rhs=sqt[:], start=True, stop=True), ["g_sq", "onesbd_bf"], [ptok])
                    sc.act(lambda e, p=p: e.activation(out=rinv[:], in_=p[:], func=AF.Sqrt, bias=1e-6, scale=1.0), [ptok], ["g_rinv"])
                    sc.dve(lambda e: e.reciprocal(out=rinv[:], in_=rinv[:]), ["g_rinv"], ["g_rinv"])
                    if kind == 0:
                        sc.dve(lambda e, ac=ac, i3=i3: e.scalar_tensor_tensor(out=qf32[i3][:], in0=ac[:], scalar=0.125, in1=rinv[:],
                                                                              op0=ALU.mult, op1=ALU.mult), [at_, "g_rinv"], [f"g_qf{i3}"])
                        sc.act(lambda e, i3=i3: e.copy(out=qfb[i3][:], in_=qf32[i3][:]), [f"g_qf{i3}"], [f"g_qb{i3}"])
                    else:
                        sc.dve(lambda e, ac=ac, i3=i3: e.tensor_tensor(out=kf32[i3][:], in0=ac[:], in1=rinv[:], op=ALU.mult),
                               [at_, "g_rinv"], [f"g_kf{i3}"])
                        sc.act(lambda e, i3=i3: e.copy(out=kfb[i3][:], in_=kf32[i3][:]), [f"g_kf{i3}"], [f"g_kb{i3}"])
                        for hp in range(2):
                            sc.act(lambda e, i3=i3, hp=hp: e.copy(out=kz[i3][hp][hp * 64:hp * 64 + 64, :], in_=kf32[i3][hp * 64:hp * 64 + 64, :]),
                                    [f"g_kf{i3}"], [f"g_kz{i3}"])
                sc.dma(batm[:], self.baT[t0:t0 + 512, :].rearrange("(n p) c -> p n c", p=128), writes=["g_batm"])
                sc.act(lambda e: e.activation(out=beta[:], in_=batm[:, :, 0:6], func=AF.Sigmoid), ["g_batm"], ["g_beta"])
                sc.dve(lambda e: e.tensor_scalar(out=nbeta[:], in0=beta[:], scalar1=-1.0, scalar2=None, op0=ALU.mult), ["g_beta"], ["g_nbeta"])
                for n in range(4):
                    sc.dve(lambda e, n=n: e.tensor_tensor(out=tmp46[:, n, :], in0=batm[:, n, 6:12], in1=gs[:, 0:6], op=ALU.add),
                           ["g_batm", "gsm"], ["g_tmp46"])
                sc.act(lambda e: e.activation(out=tmp46[:], in_=tmp46[:], func=AF.Exp), ["g_tmp46"], ["g_tmp46"])
                sc.act(lambda e: e.activation(out=tmp46[:], in_=tmp46[:], func=AF.Ln, bias=1.0), ["g_tmp46"], ["g_tmp46"])
                for n in range(4):
                    sc.dve(lambda e, n=n: e.tensor_tensor(out=gtm[:, n, :], in0=tmp46[:, n, :], in1=nA[:], op=ALU.mult),
                           ["g_tmp46", "g_nA"], ["g_gtm"])
                for n in range(4 if self.stage >= 2 else 0):
                    pb = nblk % 2
                    nblk += 1
                    b = B[pb]
                    tk = lambda nm, pb=pb: f"g_{nm}{pb}"
                    cs = slice(n * 128, (n + 1) * 128)
                    g_n = gtm[:, n, :]
                    sc.cap_begin(); self.ps_lo, self.ps_n = 0, 4
                    p1, t1 = self.psum()
                    sc.pe(lambda e, p1=p1, g_n=g_n: e.matmul(p1[:, 0:6], lhsT=useg, rhs=g_n, start=True, stop=True), ["g_gtm", "C"], [t1])
                    sc.pe(lambda e, p1=p1, g_n=g_n: e.matmul(p1[:, 8:14], lhsT=onesbd, rhs=g_n, start=True, stop=True), ["g_gtm", "C"], [t1])
                    sc.dve(lambda e, p1=p1, b=b: e.tensor_copy(out=b["gcc"][:], in_=p1[:, 0:6]), [t1], [tk("gcc")])
                    sc.dve(lambda e, p1=p1, b=b: e.tensor_tensor(out=b["gct"][:], in0=p1[:, 8:14], in1=b["gcc"][:], op=ALU.subtract),
                           [t1, tk("gcc")], [tk("gct")])
                    sc.act(lambda e, b=b: e.activation(out=b["egc"][:], in_=b["gcc"][:], func=AF.Exp), [tk("gcc")], [tk("egc")])
                    sc.act(lambda e, b=b: e.activation(out=b["eend"][:], in_=b["gct"][:], func=AF.Exp), [tk("gct")], [tk("eend")])
                    for h in range(6):
                        eng = sc.dve
                        eng(lambda e, b=b, h=h, g_n=g_n: e.tensor_scalar(out=b["GU"][:, h * 128:(h + 1) * 128], in0=useg, scalar1=g_n[:, h:h + 1],
                                                                       scalar2=None, op0=ALU.mult), ["C", "g_gtm"], [(tk("GU"), h)])
                    p2, t2 = self.psum()
                    p3, t3 = self.psum()
                    allGU = [(tk("GU"), h) for h in range(6)]
                    sc.pe(lambda e, p2=p2, b=b: e.matmul(p2[:, 0:512], lhsT=ones, rhs=b["GU"][:, 0:512], start=True, stop=True), allGU + ["C"], [t2])
                    sc.pe(lambda e, p3=p3, b=b: e.matmul(p3[:, 0:256], lhsT=ones, rhs=b["GU"][:, 512:768], start=True, stop=True), allGU + ["C"], [t3])
                    sc.act(lambda e, p2=p2, b=b: e.activation(out=b["Egc"][:, 0:512], in_=p2[:, 0:512], func=AF.Exp), [t2], [tk("Egc")])
                    sc.act(lambda e, p3=p3, b=b: e.activation(out=b["Egc"][:, 512:768], in_=p3[:, 0:256], func=AF.Exp), [t3], [tk("Egc")])
                    for h in range(6):
                        src = p2[:, h * 128:(h + 1) * 128] if h < 4 else p3[:, (h - 4) * 128:(h - 3) * 128]
                        sc.dve(lambda e, b=b, h=h, src=src: e.scalar_tensor_tensor(
                            out=b["dd"][:, h * 128:(h + 1) * 128], in0=src, scalar=b["gcc"][:, h:h + 1], in1=mneg,
                            op0=ALU.subtract, op1=ALU.add), [t2, t3, tk("gcc"), "C"], [tk("dd")])
                    sc.act(lambda e, b=b: e.activation(out=b["DmT"][:], in_=b["dd"][:], func=AF.Exp), [tk("dd")], [tk("DmT")])
                    sc.dve(lambda e, b=b: e.tensor_tensor(out=b["DmS"][:], in0=b["DmT"][:], in1=offd6, op=ALU.mult), [tk("DmT"), "C"], [tk("DmS")])
                    for rc in range(3):
                        pk, tkk = self.psum()
                        sc.pe(lambda e, pk=pk, rc=rc, cs=cs: e.matmul(pk[:, 0:128], lhsT=kf32[rc][:, cs], rhs=ident, start=True, stop=True), [f"g_kf{rc}", "C"], [tkk])
                        sc.pe(lambda e, pk=pk, rc=rc, cs=cs: e.matmul(pk[:, 128:256], lhsT=vf32[rc][:, cs], rhs=ident, start=True, stop=True), [f"g_vf{rc}", "C"], [tkk])
                        sc.act(lambda e, pk=pk, rc=rc, b=b: e.copy(out=b["Ktm"][:, rc * 128:(rc + 1) * 128], in_=pk[:, 0:128]), [tkk], [(tk("Ktm"), rc)])
                        sc.act(lambda e, pk=pk, rc=rc, b=b: e.copy(out=b["Vtm"][:, rc * 128:(rc + 1) * 128], in_=pk[:, 128:256]), [tkk], [(tk("Vtm"), rc)])
                    for c in range(2):
                        sc.dve(lambda e, b=b, c=c: e.tensor_scalar(out=b[f"eendc{c}"][:], in0=b["eend"][:], scalar1=C[:, 256 + 64 * c:257 + 64 * c],
                                                                   scalar2=None, op0=ALU.mult), [tk("eend"), "C"], [tk(f"eendc{c}")])
                    for h in range(6):
                        eng = sc.dve
                        hs = slice(h * 64, (h + 1) * 64)
                        kz_c = slice(h * 128 + (h % 2) * 64, h * 128 + (h % 2) * 64 + 64)
                        eng(lambda e, b=b, h=h, hs=hs, kz_c=kz_c: e.tensor_scalar(out=b["Kez"][:, kz_c], in0=b["Ktm"][:, hs], scalar1=b["egc"][:, h:h + 1],
                                                                       scalar2=None, op0=ALU.mult), [(tk("Ktm"), h // 2), tk("egc")], [(tk("Kez"), h)])
                        for c in range(2):
                            eng(lambda e, b=b, h=h, hs=hs, c=c: e.tensor_scalar(out=b[f"Bhc{c}"][:, hs], in0=b["Ktm"][:, hs], scalar1=b[f"eendc{c}"][:, h:h + 1],
                                                                           scalar2=None, op0=ALU.mult), [(tk("Ktm"), h // 2), tk(f"eendc{c}")], [(tk(f"Bhc{c}"), h)])
                    pKa, tKa = self.psum(); pKb, tKb = self.psum()
                    pQa, tQa = self.psum(); pQb, tQb = self.psum()
                    def bank(h, pa, ta, pb_, tb):
                        return (pa[:, h * 128:(h + 1) * 128], ta) if h < 4 else (pb_[:, (h - 4) * 128:(h - 3) * 128], tb)
                    for h in range(6):
                        rc, hp = h // 2, h % 2
                        o1, to1 = bank(h, pKa, tKa, pKb, tKb)
                        sc.pe(lambda e, o1=o1, rc=rc, hp=hp, cs=cs: e.matmul(o1, lhsT=kz[rc][hp][:, cs], rhs=kfb[rc][:, cs], start=True, stop=True),
                              [f"g_kb{rc}", f"g_kz{rc}"], [to1])
                        o2, to2 = bank(h, pQa, tQa, pQb, tQb)
                        sc.pe(lambda e, o2=o2, rc=rc, hp=hp, cs=cs: e.matmul(o2, lhsT=kz[rc][hp][:, cs], rhs=qfb[rc][:, cs], start=True, stop=True),
                              [f"g_kz{rc}", f"g_qb{rc}"], [to2])
                    for h in range(6):
                        hc = slice(h * 128, (h + 1) * 128)
                        o1, to1 = bank(h, pKa, tKa, pKb, tKb)
                        sc.dve(lambda e, b=b, h=h, hc=hc, o1=o1, n=n: e.scalar_tensor_tensor(
                            out=b["Mm"][:, hc], in0=o1, scalar=nbeta[:, n, h:h + 1], in1=b["DmS"][:, hc], op0=ALU.mult, op1=ALU.mult),
                            [to1, "g_nbeta", tk("DmS")], [(tk("Mm"), h)])
                        o2, to2 = bank(h, pQa, tQa, pQb, tQb)
                        sc.dve(lambda e, b=b, hc=hc, o2=o2: e.tensor_tensor(out=b["AqT"][:, hc], in0=o2, in1=b["DmT"][:, hc], op=ALU.mult),
                               [to2, tk("DmT")], [(tk("AqT"), h)])
                    self.neumann(b, tk, ident, ident6)
                    sc.act(lambda e, b=b: e.copy(out=b["Rbf"][:], in_=b["R"][:]), [tk("R")], [tk("Rbf")])
                    pW, tW = self.psum()
                    pU, tU = self.psum()
                    for h in range(6):
                        rc, hp = h // 2, h % 2
                        hs = slice(h * 64, (h + 1) * 64); hc = slice(h * 128, (h + 1) * 128)
                        sc.pe(lambda e, pW=pW, b=b, rc=rc, hp=hp, hc=hc: e.matmul(
                            pW[:, rc * 128:(rc + 1) * 128], lhsT=b["Kez"][:, hc], rhs=b["Rbf"][:, hc], start=(hp == 0), stop=(hp == 1)),
                            [(tk("Kez"), h), tk("Rbf")], [tW])
                        sc.pe(lambda e, pU=pU, b=b, hs=hs, hc=hc: e.matmul(pU[:, hs], lhsT=b["Rbf"][:, hc], rhs=b["Vtm"][:, hs], start=True, stop=True),
                              [(tk("Vtm"), h // 2), tk("Rbf")], [tU])
                    sc.act(lambda e, pW=pW, b=b: e.mul(out=b["WtT"][:], in_=pW[:, 0:384], mul=-1.0), [tW], [tk("WtT")])
                    for h in range(6):
                        hs = slice(h * 64, (h + 1) * 64)
                        sc.dve(lambda e, pU=pU, b=b, h=h, hs=hs, n=n: e.tensor_scalar(out=b["Utb"][:, hs], in0=pU[:, hs], scalar1=beta[:, n, h:h + 1],
                                                                                 scalar2=None, op0=ALU.mult), [tU, "g_beta"], [tk("Utb")])
                    for h in range(6):
                        rc, hp = h // 2, h % 2
                        rows = slice(hp * 64, hp * 64 + 64)
                        eng = sc.dve
                        eng(lambda e, b=b, rc=rc, rows=rows, h=h, cs=cs: e.tensor_tensor(
                            out=b["qbar"][rows, rc * 128:(rc + 1) * 128], in0=qf32[rc][rows, cs], in1=b["Egc"][rows, h * 128:(h + 1) * 128], op=ALU.mult),
                            [f"g_qf{rc}", tk("Egc")], [tk("qbar")])
                    gl_ap = lambda h, c, b=b: b["Egc"][(h % 2) * 64:(h % 2) * 64 + 64, h * 128 + c * 64 + 63:h * 128 + c * 64 + 64]
                    pre_ops = sc.cap_end()
                    sc.cap_begin(); self.ps_lo, self.ps_n = 4, 1
                    self.chunk_loop(b, tk, n, psO, H32, Hbd, "g", gl_ap, [tk("Egc")], beta=beta)
                    loop_ops = sc.cap_end()
                    self.ps_lo, self.ps_n = 0, 5
                    sc.replay(pend_loop, pre_ops)
                    pend_loop = loop_ops
                    if n == 3:
                        sc.replay(pend_loop)
                        pend_loop = []
                for rc in range(3 if self.stage >= 6 else 0):
                    po, pot = psO[rc]
                    sc.act(lambda e, po=po: e.copy(out=osb[:], in_=po[:]), [pot], ["g_osb"])
                    sc.act(lambda e: e.activation(out=sqt[:], in_=osb[:], func=AF.Square), ["g_osb"], ["g_sq"])
                    p, ptok = self.psum()
                    sc.pe(lambda e, p=p: e.matmul(p[:], lhsT=self.onesbd_bf[:], rhs=sqt[:], start=True, stop=True), ["g_sq", "onesbd_bf"], [ptok])
                    sc.act(lambda e, p=p: e.activation(out=rinv[:], in_=p[:], func=AF.Sqrt, bias=NORM_EPS, scale=1.0 / 64), [ptok], ["g_rinv"])
                    sc.dve(lambda e: e.reciprocal(out=rinv[:], in_=rinv[:]), ["g_rinv"], ["g_rinv"])
                    r0 = 2976 + rc * 128
                    sc.dma(gate[:], self.pT[r0:r0 + 128, t0:t0 + 512], writes=["g_gate"])
                    sc.act(lambda e: e.activation(out=gate[:], in_=gate[:], func=AF.Silu), ["g_gate"], ["g_gate"])
                    sc.dve(lambda e: e.tensor_tensor(out=osb[:], in0=osb[:], in1=rinv[:], op=ALU.mult), ["g_osb", "g_rinv"], ["g_osb"])
                    sc.dve(lambda e: e.scalar_tensor_tensor(out=ybf[:], in0=osb[:], scalar=gs[:, 12:13], in1=gate[:], op0=ALU.mult, op1=ALU.mult),
                           ["g_osb", "gsm", "g_gate"], ["g_ybf"])
                    y0 = 640 + rc * 128
                    sc.dma(self.yT[y0:y0 + 128, t0:t0 + 512], ybf[:], reads=["g_ybf"], writes=[("yT", y0, sbi)], q="pool")
            sc.flush()
        self.ps_lo, self.ps_n = 0, 8

    def mla_tables(self):
        nc, sc, S = self.nc, self.sc, self.S
        with contextlib.ExitStack() as st:
            T = 2048 if S % 2048 == 0 else 512
            pi_ = st.enter_context(nc.sbuf_tensor("m_posi", [96, T], I32))
            pf = st.enter_context(nc.sbuf_tensor("m_posf", [96, T], F32))
            t1 = st.enter_context(nc.sbuf_tensor("m_tt1", [96, T], F32))
            t2 = st.enter_context(nc.sbuf_tensor("m_tt2", [96, T], F32))
            ifq = self.mlc[0:96, 0:1]
            for t in range(S // T):
                t0 = t * T
                sc.dma(pi_[:], self.pos96[:, t0:t0 + T], writes=["m_posi"])
                sc.dve(lambda e: e.tensor_copy(out=pf[:], in_=pi_[:]), ["m_posi"], ["m_posf"])
                sc.dve(lambda e: e.tensor_scalar(out=pf[:], in0=pf[:], scalar1=ifq, scalar2=None, op0=ALU.mult), ["m_posf", "mlc"], ["m_posf"])
                sc.dve(lambda e: e.tensor_scalar(out=pf[:], in0=pf[:], scalar1=float(1.0 / (2.0 * np.pi)), scalar2=None, op0=ALU.mult), ["m_posf"], ["m_posf"])
                sc.dve(lambda e: e.tensor_copy(out=pi_[:], in_=pf[:]), ["m_posf", "m_posi"], ["m_posi"])
                sc.dve(lambda e: e.tensor_copy(out=t1[:], in_=pi_[:]), ["m_posi"], ["m_tt1"])
                sc.dve(lambda e: e.tensor_tensor(out=pf[:], in0=pf[:], in1=t1[:], op=ALU.subtract), ["m_posf", "m_tt1"], ["m_posf"])
                sc.act(lambda e: e.activation(out=t1[:], in_=pf[:], func=AF.Sin, scale=float(np.pi)), ["m_posf"], ["m_tt1"])
                sc.act(lambda e: e.activation(out=t2[:], in_=pf[:], func=AF.Sin, scale=float(np.pi / 2)), ["m_posf"], ["m_tt2"])
                sc.dve(lambda e: e.tensor_tensor(out=t2[:], in0=t2[:], in1=t2[:], op=ALU.mult), ["m_tt2"], ["m_tt2"])
                sc.dve(lambda e: e.tensor_scalar(out=t2[:], in0=t2[:], scalar1=-2.0, scalar2=1.0, op0=ALU.mult, op1=ALU.add), ["m_tt2"], ["m_tt2"])
                sc.dve(lambda e: e.scalar_tensor_tensor(out=t2[:], in0=t1[:], scalar=2.0, in1=t2[:], op0=ALU.mult, op1=ALU.mult), ["m_tt1", "m_tt2"], ["m_tt2"])
                sc.dma(self.sinT[:, t0:t0 + T], t2[:], reads=["m_tt2"], writes=[("tab", 1, t)], q="pool")
                sc.dve(lambda e: e.tensor_tensor(out=t1[:], in0=t1[:], in1=t1[:], op=ALU.mult), ["m_tt1"], ["m_tt1"])
                sc.dve(lambda e: e.tensor_scalar(out=t1[:], in0=t1[:], scalar1=-2.0, scalar2=1.0, op0=ALU.mult, op1=ALU.add), ["m_tt1"], ["m_tt1"])
                sc.dma(self.cosT[:, t0:t0 + T], t1[:], reads=["m_tt1"], writes=[("tab", 0, t)], q="pool")
            sc.flush()

    def phase_mla(self, l):
        nc, sc, S = self.nc, self.sc, self.S
        C = self.C
        T = 512
        ml = self.mls[:, l * 8:(l + 1) * 8]
        with contextlib.ExitStack() as st:
            sb = lambda n, s, d=F32: st.enter_context(nc.sbuf_tensor(f"{n}_L{l}", s, d))
            wst = sb("m_wst", [128, 512])
            wuq = sb("m_wuq", [128, 2, 384], BF16); wuqr = sb("m_wuqr", [128, 2, 384], BF16)
            wkn = sb("m_wkn", [128, 384], BF16); wv = sb("m_wv", [128, 256], BF16)
            sel = sb("m_sel", [32, 96], BF16); ones96 = sb("m_ones96", [96, 96], BF16)
            cq = sb("m_cq", [128, 2, T]); ckv = sb("m_ckv", [128, T]); kr = sb("m_kr", [32, T]); krt = sb("m_krt", [32, T])
            sq2 = sb("m_sq2", [128, 2, T], BF16); sq1 = sb("m_sq1", [128, T], BF16)
            rq = sb("m_rq", [128, T]); rkv = sb("m_rkv", [128, T])
            cqn = sb("m_cqn", [128, 2, T], BF16); ckvn = sb("m_ckvn", [128, T], BF16)
            krb = sb("m_krb", [32, T], BF16); krtb = sb("m_krtb", [32, T], BF16)
            cosb = sb("m_cos", [96, T]); sinb = sb("m_sin", [96, T])
            raw2 = [sb(f"m_raw{i}", [96, T]) for i in range(2)]; rot2 = [sb(f"m_rot{i}", [96, T]) for i in range(2)]
            sq962 = [sb(f"m_sq96{i}", [96, T], BF16) for i in range(2)]; rs962 = [sb(f"m_rs96{i}", [96, T]) for i in range(2)]
            fin = [sb(f"m_fin{i}", [96, T], BF16) for i in range(2)]
            vx = [sb(f"m_vx{i}", [128, 4, 128], BF16) for i in range(2)]
            sc.dma(wst[:, 0:384], self.w_uq[l, 0:128, :], writes=["m_wst"])
            sc.dve(lambda e: e.tensor_scalar(out=wuq[:, 0, :], in0=wst[:, 0:384], scalar1=ml[:, 0:1], scalar2=None, op0=ALU.mult), ["m_wst", "mls"], ["m_wuq"])
            sc.dma(wst[:, 0:384], self.w_uq[l, 128:256, :], reads=["m_wst"], writes=["m_wst"])
            sc.dve(lambda e: e.tensor_scalar(out=wuq[:, 1, :], in0=wst[:, 0:384], scalar1=ml[:, 1:2], scalar2=None, op0=ALU.mult), ["m_wst", "mls"], ["m_wuq"])
            sc.pool(lambda e: e.memset(wuqr[:], 0.0), [], ["m_wuqr"])
            for h in range(4):
                b0 = h * 96
                sc.dve(lambda e, b0=b0: e.tensor_scalar(out=wuqr[:, :, b0 + 64:b0 + 80], in0=wuq[:, :, b0 + 80:b0 + 96], scalar1=-1.0, scalar2=None, op0=ALU.mult),
                       ["m_wuq", "m_wuqr"], ["m_wuqr"])
                sc.dve(lambda e, b0=b0: e.tensor_copy(out=wuqr[:, :, b0 + 80:b0 + 96], in_=wuq[:, :, b0 + 64:b0 + 80]), ["m_wuq", "m_wuqr"], ["m_wuqr"])
            sc.dma(wst[:], self.w_ukv[l], reads=["m_wst"], writes=["m_wst"])
            sc.pool(lambda e: e.memset(wkn[:], 0.0), [], ["m_wkn"])
            for h in range(4):
                sc.dve(lambda e, h=h: e.tensor_scalar(out=wkn[:, h * 96:h * 96 + 64], in0=wst[:, h * 128:h * 128 + 64], scalar1=ml[:, 2:3], scalar2=None, op0=ALU.mult),
                       ["m_wst", "mls", "m_wkn"], ["m_wkn"])
                sc.dve(lambda e, h=h: e.tensor_scalar(out=wv[:, h * 64:h * 64 + 64], in0=wst[:, h * 128 + 64:h * 128 + 128], scalar1=ml[:, 2:3], scalar2=None, op0=ALU.mult),
                       ["m_wst", "mls"], ["m_wv"])
            sc.dve(lambda e: e.tensor_copy(out=sel[:], in_=self.mlc[0:32, 8:104]), ["mlc"], ["m_sel"])
            sc.pool(lambda e: e.memset(ones96[:], 1.0), [], ["m_ones96"])
            for i in range(2):
                sc.pool(lambda e, i=i: e.memset(vx[i][:], 1.0), [], [f"m_vx{i}"])
            nfin = 0
            for t in range(S // T):
                t0 = t * T
                sc.dma(cq[:], self.pT[1408:1664, t0:t0 + T].rearrange("(k p) s -> p k s", p=128), writes=["m_cq"])
                sc.dma(ckv[:], self.pT[1664:1792, t0:t0 + T], writes=["m_ckv"])
                sc.dma(kr[:], self.pT[1792:1824, t0:t0 + T], writes=["m_kr"])
                sc.dma(krt[:], self.pT[3372:3404, t0:t0 + T], writes=["m_krt"])
                sc.dma(cosb[:], self.cosT[:, t0:t0 + T], writes=["m_cos"])
                sc.dma(sinb[:], self.sinT[:, t0:t0 + T], writes=["m_sin"])
                sc.act(lambda e: e.activation(out=sq2[:], in_=cq[:], func=AF.Square), ["m_cq"], ["m_sq2"])
                sc.act(lambda e: e.activation(out=sq1[:], in_=ckv[:], func=AF.Square), ["m_ckv"], ["m_sq1"])
                p, ptok = self.psum()
                for kc in range(2):
                    sc.pe(lambda e, p=p, kc=kc: e.matmul(p[:], lhsT=self.ones_bf[:], rhs=sq2[:, kc, :], start=(kc == 0), stop=(kc == 1)), ["m_sq2", "ones_bf"], [ptok])
                sc.act(lambda e, p=p: e.activation(out=rq[:], in_=p[:], func=AF.Sqrt, bias=NORM_EPS, scale=1.0 / 256), [ptok], ["m_rq"])
                sc.dve(lambda e: e.reciprocal(out=rq[:], in_=rq[:]), ["m_rq"], ["m_rq"])
                p, ptok = self.psum()
                sc.pe(lambda e, p=p: e.matmul(p[:], lhsT=self.ones_bf[:], rhs=sq1[:], start=True, stop=True), ["m_sq1", "ones_bf"], [ptok])
                sc.act(lambda e, p=p: e.activation(out=rkv[:], in_=p[:], func=AF.Sqrt, bias=NORM_EPS, scale=1.0 / 128), [ptok], ["m_rkv"])
                sc.dve(lambda e: e.reciprocal(out=rkv[:], in_=rkv[:]), ["m_rkv"], ["m_rkv"])
                for kc in range(2):
                    sc.dve(lambda e, kc=kc: e.tensor_tensor(out=cqn[:, kc, :], in0=cq[:, kc, :], in1=rq[:], op=ALU.mult), ["m_cq", "m_rq"], ["m_cqn"])
                sc.dve(lambda e: e.tensor_tensor(out=ckvn[:], in0=ckv[:], in1=rkv[:], op=ALU.mult), ["m_ckv", "m_rkv"], ["m_ckvn"])
                sc.act(lambda e: e.copy(out=krb[:], in_=kr[:]), ["m_kr"], ["m_krb"])
                sc.act(lambda e: e.copy(out=krtb[:], in_=krt[:]), ["m_krt"], ["m_krtb"])
                for h in range(4):
                    hc = slice(h * 96, (h + 1) * 96)
                    for isk in range(2):
                        ci = nfin % 2
                        raw, rot, sq96, rs96 = raw2[ci], rot2[ci], sq962[ci], rs962[ci]
                        pr, prt = self.psum()
                        pro, prot = self.psum()
                        if isk == 0:
                            for kc in range(2):
                                sc.pe(lambda e, pr=pr, kc=kc, hc=hc: e.matmul(pr[0:96, :], lhsT=wuq[:, kc, hc], rhs=cqn[:, kc, :], start=(kc == 0), stop=(kc == 1)),
                                      ["m_wuq", "m_cqn"], [prt])
                            for kc in range(2):
                                sc.pe(lambda e, pro=pro, kc=kc, hc=hc: e.matmul(pro[0:96, :], lhsT=wuqr[:, kc, hc], rhs=cqn[:, kc, :], start=(kc == 0), stop=(kc == 1)),
                                      ["m_wuqr", "m_cqn"], [prot])
                            gcol, gpcol, fscale = ml[0:96, 3:4], ml[0:96, 4:5], float(96 ** -0.5)
                        else:
                            sc.pe(lambda e, pr=pr, hc=hc: e.matmul(pr[0:96, :], lhsT=wkn[:, hc], rhs=ckvn[:], start=True, stop=False), ["m_wkn", "m_ckvn"], [prt])
                            sc.pe(lambda e, pr=pr: e.matmul(pr[0:96, :], lhsT=sel[:], rhs=krb[:], start=False, stop=True), ["m_sel", "m_krb"], [prt])
                            sc.pe(lambda e, pro=pro: e.matmul(pro[0:96, :], lhsT=sel[:], rhs=krtb[:], start=True, stop=True), ["m_sel", "m_krtb"], [prot])
                            gcol, gpcol, fscale = ml[0:96, 5:6], ml[0:96, 6:7], 1.0
                        sc.act(lambda e, raw=raw, rot=rot, sq96=sq96, rs96=rs96, pr=pr: e.copy(out=raw[:], in_=pr[0:96, :]), [prt], [f"m_raw{ci}"])
                        sc.act(lambda e, raw=raw, rot=rot, sq96=sq96, rs96=rs96, pro=pro: e.copy(out=rot[:], in_=pro[0:96, :]), [prot], [f"m_rot{ci}"])
                        sc.act(lambda e, raw=raw, rot=rot, sq96=sq96, rs96=rs96: e.activation(out=sq96[:], in_=raw[:], func=AF.Square), [f"m_raw{ci}"], [f"m_sq96{ci}"])
                        p, ptok = self.psum()
                        sc.pe(lambda e, raw=raw, rot=rot, sq96=sq96, rs96=rs96, p=p: e.matmul(p[0:96, :], lhsT=ones96[:], rhs=sq96[:], start=True, stop=True), [f"m_sq96{ci}", "m_ones96"], [ptok])
                        sc.act(lambda e, raw=raw, rot=rot, sq96=sq96, rs96=rs96, p=p: e.activation(out=rs96[:], in_=p[0:96, :], func=AF.Sqrt, bias=NORM_EPS, scale=1.0 / 96), [ptok], [f"m_rs96{ci}"])
                        sc.dve(lambda e, raw=raw, rot=rot, sq96=sq96, rs96=rs96: e.reciprocal(out=rs96[:], in_=rs96[:]), [f"m_rs96{ci}"], [f"m_rs96{ci}"])
                        sc.dve(lambda e, raw=raw, rot=rot, sq96=sq96, rs96=rs96, gcol=gcol: e.scalar_tensor_tensor(out=raw[:], in0=raw[:], scalar=gcol, in1=cosb[:], op0=ALU.mult, op1=ALU.mult),
                               [f"m_raw{ci}", "mls", "m_cos"], [f"m_raw{ci}"])
                        sc.dve(lambda e, raw=raw, rot=rot, sq96=sq96, rs96=rs96, gpcol=gpcol: e.scalar_tensor_tensor(out=rot[:], in0=rot[:], scalar=gpcol, in1=sinb[:], op0=ALU.mult, op1=ALU.mult),
                               [f"m_rot{ci}", "mls", "m_sin"], [f"m_rot{ci}"])
                        sc.dve(lambda e, raw=raw, rot=rot, sq96=sq96, rs96=rs96: e.tensor_tensor(out=raw[:], in0=raw[:], in1=rot[:], op=ALU.add), [f"m_raw{ci}", f"m_rot{ci}"], [f"m_raw{ci}"])
                        sc.dve(lambda e, raw=raw, rot=rot, sq96=sq96, rs96=rs96: e.tensor_tensor(out=raw[:], in0=raw[:], in1=rs96[:], op=ALU.mult), [f"m_raw{ci}", f"m_rs96{ci}"], [f"m_raw{ci}"])
                        fi = nfin % 2
                        nfin += 1
                        sc.act(lambda e, raw=raw, rot=rot, sq96=sq96, rs96=rs96, fi=fi, fscale=fscale: e.mul(out=fin[fi][:], in_=raw[:], mul=fscale), [f"m_raw{ci}"], [f"m_fin{fi}"])
                        dstT = self.qfT if isk == 0 else self.kfT
                        sc.dma(dstT[h, :, t0:t0 + T], fin[fi][:], reads=[f"m_fin{fi}"], writes=[("qk", isk, h, t)], q="pool")
                for n in range(4):
                    vi = (t * 4 + n) % 2
                    p, ptok = self.psum()
                    sc.pe(lambda e, p=p, n=n: e.matmul(p[:, 0:256], lhsT=ckvn[:, n * 128:(n + 1) * 128], rhs=wv[:], start=True, stop=True), ["m_ckvn", "m_wv"], [ptok])
                    sc.act(lambda e, p=p, vi=vi: e.copy(out=vx[vi][:, :, 0:64], in_=p[:, 0:256].rearrange("p (h d) -> p h d", d=64)), [ptok], [f"m_vx{vi}"])
                    r0 = t0 + n * 128
                    sc.dma(self.vxT[:, r0:r0 + 128, :].rearrange("h p d -> p h d"), vx[vi][:], reads=[f"m_vx{vi}"], writes=[("vx", t, n)], q="pool")
            sc.flush()
        self.ps_n = 6
        psOD = [(self.ps[6 + i], f"psOD{i}") for i in range(2)]
        NB = S // 128
        with contextlib.ExitStack() as st:
            sb = lambda n, s, d=F32: st.enter_context(nc.sbuf_tensor(f"{n}_L{l}", s, d))
            kf = sb("m_kf", [96, S], BF16); qf = sb("m_qf", [96, S], BF16)
            vxa = sb("m_vxa", [128, NB, 128], BF16)
            mk32 = sb("m_mk32", [128, 2048]); mk = sb("m_mk", [128, 2048], BF16)
            pt = [sb(f"m_pt{i}", [128, T], BF16) for i in range(3)]
            od = sb("m_od", [128, T]); den = sb("m_den", [64, T]); yb = [sb(f"m_yb{i}", [64, T], BF16) for i in range(2)]
            sc.dma(mk32[:], self.mmask, writes=["m_mk32"])
            sc.dve(lambda e: e.tensor_copy(out=mk[:], in_=mk32[:]), ["m_mk32"], ["m_mk"])
            npt = 0
            ng = 0
            for h in range(4):
                sc.dma(kf[:], self.kfT[h], writes=["m_kf"])
                sc.dma(qf[:], self.qfT[h], writes=["m_qf"])
                sc.dma(vxa[:], self.vxT[h].rearrange("(n p) d -> p n d", p=128), writes=["m_vxa"])
                items = [(g, kb) for g in range(S // T) for kb in range(4 * g + 4)]
                LOOK = 2
                pend = {}

                def emit_st(i):
                    nonlocal npt
                    g, kb = items[i]
                    ps_, pst = self.psum()
                    sc.pe(lambda e, ps_=ps_, kb=kb, g=g: e.matmul(ps_[:], lhsT=kf[:, kb * 128:(kb + 1) * 128], rhs=qf[:, g * T:(g + 1) * T], start=True, stop=True),
                          ["m_kf", "m_qf"], [pst])
                    pi = npt % 3
                    npt += 1
                    sc.act(lambda e, ps_=ps_, pi=pi: e.activation(out=pt[pi][:], in_=ps_[:], func=AF.Exp), [pst], [f"m_pt{pi}"])
                    j = kb - 4 * g
                    if j >= 0:
                        sc.dve(lambda e, pi=pi, j=j: e.tensor_tensor(out=pt[pi][:], in0=pt[pi][:], in1=mk[:, j * T:(j + 1) * T], op=ALU.mult),
                               [f"m_pt{pi}", "m_mk"], [f"m_pt{pi}"])
                    pend[i] = pi

                for i in range(min(LOOK, len(items))):
                    emit_st(i)
                for i, (g, kb) in enumerate(items):
                    if i + LOOK < len(items):
                        emit_st(i + LOOK)
                    pi = pend.pop(i)
                    po, pot = psOD[(ng + g) % 2]
                    nkb = 4 * g + 4
                    sc.pe(lambda e, po=po, kb=kb, pi=pi, nkb=nkb: e.matmul(po[:], lhsT=vxa[:, kb, :], rhs=pt[pi][:], start=(kb == 0), stop=(kb == nkb - 1)),
                          ["m_vxa", f"m_pt{pi}"], [pot])
                    if kb == nkb - 1:
                        sc.act(lambda e, po=po: e.copy(out=od[:], in_=po[:]), [pot], ["m_od"])
                        sc.dve(lambda e: e.tensor_copy(out=den[:], in_=od[64:128, :]), ["m_od"], ["m_den"])
                        sc.dve(lambda e: e.reciprocal(out=den[:], in_=den[:]), ["m_den"], ["m_den"])
                        yi = (ng + g) % 2
                        sc.dve(lambda e, yi=yi: e.tensor_tensor(out=yb[yi][:], in0=od[0:64, :], in1=den[:], op=ALU.mult), ["m_od", "m_den"], [f"m_yb{yi}"])
                        y0 = 384 + h * 64
                        sc.dma(self.yT[y0:y0 + 64, g * T:(g + 1) * T], yb[yi][:], reads=[f"m_yb{yi}"], writes=[("yT", y0, g)], q="pool")
                ng += S // T
            sc.flush()
        self.ps_lo, self.ps_n = 0, 8

    def phase_rwkv(self, l):
        nc, sc, S = self.nc, self.sc, self.S
        C = self.C
        ident, onesbd = C[:, 0:128], C[:, 256:384]
        mstrict, mincl, nmstrict = C[:, 768:896], C[:, 896:1024], C[:, 384:512]
        ident6 = C[:, 1792:2560]
        segm = self.segm
        self.ps_n = 5
        psO = [(self.ps[5 + i], f"psO{i}") for i in range(3)]
        rs_ = self.rws[:, l * 32:(l + 1) * 32]
        MU, W0, A0, KK_, KA, RK, GG, GB = 0, 11, 14, 17, 20, 23, 26, 29
        with contextlib.ExitStack() as st:
            sb = lambda n, s, d=F32: st.enter_context(nc.sbuf_tensor(f"{n}_L{l}", s, d))
            T = 512
            wst = sb("r_wst", [128, 1152])
            wbf = sb("r_wbf", [128, 1152], BF16)
            xin = [sb(f"r_xin{i}", [128, 513]) for i in range(2)]
            dtmp = sb("r_dtmp", [128, T])
            f3 = lambda nm, d=F32: [sb(f"r_{nm}{i}", [128, T], d) for i in range(3)]
            rf, kraw, vf, gf_, epos = (f3(n_) for n_ in ("rf", "kraw", "vf", "gf", "epos"))
            af, gc, kk, kmod, bvec, eneg = ([sb(f"r_{n_}", [128, T])] * 3 for n_ in ("af", "gc", "kk", "kmod", "bvec", "eneg"))
            ap32, Bh32, Kh32, bonus = f3("ap32"), f3("Bh32"), f3("Kh32"), f3("bonus")
            apb, rbar, btb, ktb = f3("apb", BF16), f3("rbar", BF16), f3("btb", BF16), f3("ktb", BF16)
            btz = [[sb(f"r_btz{i}_{hp}", [128, T], BF16) for hp in range(2)] for i in range(3)]
            ktz = [[sb(f"r_ktz{i}_{hp}", [128, T], BF16) for hp in range(2)] for i in range(3)]
            z9, zg = sb("r_z9", [128, T]), sb("r_zg", [128, T])
            act9, sigzg = sb("r_act9", [128, T], BF16), sb("r_sigzg", [128, T], BF16)
            tA, tB = sb("r_tA", [128, T]), sb("r_tB", [128, T])
            sqt = sb("r_sq", [128, T], BF16)
            H32 = sb("r_H32", [128, 384]); Hbd = sb("r_Hbd", [128, 384], BF16)
            B = []
            for pb in range(2):
                d = {}
                for nm, shp, dtp in [("Mm", [128, 768], F32), ("Mt", [128, 768], F32), ("R", [128, 768], F32), ("Pa", [128, 768], F32),
                                     ("Pta", [128, 768], F32), ("Rbf", [128, 768], BF16), ("AqT", [128, 768], BF16), ("AqkT", [128, 768], BF16),
                                     ("AakT", [128, 768], BF16), ("Kez", [128, 768], BF16), ("Vz", [128, 768], BF16), ("Vtm", [128, 384], BF16),
                                     ("Xb", [128, 384], BF16), ("Ubz", [128, 768], BF16), ("WtT", [128, 384], BF16), ("Utb", [128, 384], F32),
                                     ("qbar", [128, 384], BF16), ("Bhc0", [128, 384], BF16), ("Bhc1", [128, 384], BF16),
                                     ("Khc0", [128, 384], BF16), ("Khc1", [128, 384], BF16), ("BKtm", [128, 256], F32)]:
                    d[nm] = sb(f"r_{nm}{pb}", shp, dtp)
                B.append(d)
            sc.dma(wst[:], self.rww[:, l * 1152:(l + 1) * 1152], writes=["r_wst"])
            sc.dve(lambda e: e.tensor_copy(out=wbf[:], in_=wst[:]), ["r_wst"], ["r_wbf"])
            sc.dve(lambda e: e.memset(H32[:], 0.0), [], [("r_H32", h) for h in range(6)])
            sc.pool(lambda e: e.memset(Hbd[:], 0.0), [], [("r_Hbd", h) for h in range(6)])
            for i in range(3):
                for hp in range(2):
                    sc.pool(lambda e, i=i, hp=hp: e.memset(btz[i][hp][:], 0.0), [], [f"r_btz{i}"])
                    sc.pool(lambda e, i=i, hp=hp: e.memset(ktz[i][hp][:], 0.0), [], [f"r_ktz{i}"])
            for pb in range(2):
                for nm in ("Kez", "Vz", "Ubz"):
                    sc.pool(lambda e, pb=pb, nm=nm: e.memset(B[pb][nm][:], 0.0), [], [(f"r_{nm}{pb}", h) for h in range(6)])
            nblk = 0
            pend_loop = []
            for sbi in range(S // T):
                t0 = sbi * T
                dests = rf + kraw + vf + [z9, zg]
                dtoks = [f"r_rf{i}" for i in range(3)] + [f"r_kraw{i}" for i in range(3)] + [f"r_vf{i}" for i in range(3)] + ["r_z9", "r_zg"]
                for rc in range(11):
                    xi, xt = xin[rc % 2], f"r_xin{rc % 2}"
                    if sbi == 0:
                        sc.pool(lambda e, xi=xi: e.memset(xi[:, 0:1], 0.0), [], [xt])
                        sc.dma(xi[:, 1:513], self.pT[rc * 128:(rc + 1) * 128, 0:T], reads=[xt], writes=[xt])
                    else:
                        sc.dma(xi[:], self.pT[rc * 128:(rc + 1) * 128, t0 - 1:t0 + T], writes=[xt])
                    sc.dve(lambda e, xi=xi: e.tensor_tensor(out=dtmp[:], in0=xi[:, 0:512], in1=xi[:, 1:513], op=ALU.subtract), [xt], ["r_dtmp"])
                    dst = dests[rc]
                    sc.dve(lambda e, xi=xi, dst=dst, rc=rc: e.scalar_tensor_tensor(out=dst[:], in0=dtmp[:], scalar=rs_[:, MU + rc:MU + rc + 1], in1=xi[:, 1:513],
                                                                            op0=ALU.mult, op1=ALU.add), ["r_dtmp", xt, "rws"], [dtoks[rc]])
                sc.act(lambda e: e.activation(out=act9[0:64, :], in_=z9[0:64, :], func=AF.Tanh), ["r_z9"], ["r_act9"])
                sc.act(lambda e: e.copy(out=act9[64:128, :], in_=z9[64:128, :]), ["r_z9"], ["r_act9"])
                sc.act(lambda e: e.activation(out=sigzg[:], in_=zg[:], func=AF.Sigmoid), ["r_zg"], ["r_sigzg"])
                for c in range(3):
                    cc = slice(c * 128, (c + 1) * 128)
                    p, ptok = self.psum()
                    sc.pe(lambda e, p=p, cc=cc: e.matmul(p[:], lhsT=wbf[:, cc], rhs=act9[:], start=True, stop=True), ["r_wbf", "r_act9"], [ptok])
                    sc.act(lambda e, p=p, c=c: e.activation(out=gc[c][:], in_=p[:], func=AF.Sigmoid, bias=rs_[:, W0 + c:W0 + c + 1]), [ptok, "rws"], ["r_gc"])
                    sc.dve(lambda e, c=c: e.tensor_scalar(out=tA[:], in0=gc[c][:], scalar1=-0.6065306597126334, scalar2=None, op0=ALU.mult), ["r_gc"], ["r_tA"])
                    p2, p2tok = self.psum()
                    sc.pe(lambda e, p2=p2, c=c: e.matmul(p2[:], lhsT=wbf[:, 384 + c * 128:384 + (c + 1) * 128], rhs=act9[:], start=True, stop=True), ["r_wbf", "r_act9"], [p2tok])
                    sc.act(lambda e, p2=p2, c=c: e.activation(out=af[c][:], in_=p2[:], func=AF.Sigmoid, bias=rs_[:, A0 + c:A0 + c + 1]), [p2tok, "rws"], ["r_af"])
                    p3, p3tok = self.psum()
                    sc.pe(lambda e, p3=p3, c=c: e.matmul(p3[:], lhsT=wbf[:, 768 + c * 128:768 + (c + 1) * 128], rhs=sigzg[:], start=True, stop=True), ["r_wbf", "r_sigzg"], [p3tok])
                    sc.act(lambda e, p3=p3, c=c: e.copy(out=gf_[c][:], in_=p3[:]), [p3tok], [f"r_gf{c}"])
                    sc.dve(lambda e, c=c: e.tensor_tensor_scan(out=gc[c][:], data0=segm[:], data1=tA[:], initial=0.0, op0=ALU.mult, op1=ALU.add),
                           ["r_tA", "segm", "r_gc"], ["r_gc"])
                    sc.act(lambda e, c=c: e.activation(out=epos[c][:], in_=gc[c][:], func=AF.Exp), ["r_gc"], [f"r_epos{c}"])
                    sc.act(lambda e, c=c: e.activation(out=eneg[c][:], in_=gc[c][:], func=AF.Exp, scale=-1.0), ["r_gc"], ["r_eneg"])
                    sc.dve(lambda e, c=c: e.tensor_tensor(out=tB[:], in0=gc[c][:], in1=tA[:], op=ALU.subtract), ["r_gc", "r_tA"], ["r_tB"])
                    sc.act(lambda e: e.activation(out=tB[:], in_=tB[:], func=AF.Exp), ["r_tB"], ["r_tB"])
                    sc.dve(lambda e, c=c: e.tensor_scalar(out=kk[c][:], in0=kraw[c][:], scalar1=rs_[:, KK_ + c:KK_ + c + 1], scalar2=None, op0=ALU.mult),
                           [f"r_kraw{c}", "rws"], ["r_kk"])
                    sc.act(lambda e, c=c: e.activation(out=sqt[:], in_=kk[c][:], func=AF.Square), ["r_kk"], ["r_sq"])
                    p4, p4tok = self.psum()
                    sc.pe(lambda e, p4=p4: e.matmul(p4[:], lhsT=self.onesbd_bf[:], rhs=sqt[:], start=True, stop=True), ["r_sq", "onesbd_bf"], [p4tok])
                    sc.act(lambda e, p4=p4: e.activation(out=tA[:], in_=p4[:], func=AF.Sqrt, bias=1e-6, scale=1.0), [p4tok, "r_tA"], ["r_tA"])
                    sc.dve(lambda e: e.reciprocal(out=tA[:], in_=tA[:]), ["r_tA"], ["r_tA"])
                    sc.dve(lambda e, c=c: e.tensor_tensor(out=kk[c][:], in0=kk[c][:], in1=tA[:], op=ALU.mult), ["r_kk", "r_tA"], ["r_kk"])
                    sc.dve(lambda e, c=c: e.tensor_tensor(out=ap32[c][:], in0=kk[c][:], in1=tB[:], op=ALU.mult), ["r_kk", "r_tB"], [f"r_ap32{c}"])
                    sc.act(lambda e, c=c: e.copy(out=apb[c][:], in_=ap32[c][:]), [f"r_ap32{c}"], [f"r_apb{c}"])
                    sc.dve(lambda e, c=c: e.tensor_scalar(out=tA[:], in0=af[c][:], scalar1=-1.0, scalar2=None, op0=ALU.add), ["r_af", "r_tA"], ["r_tA"])
                    sc.dve(lambda e, c=c: e.tensor_scalar(out=tA[:], in0=tA[:], scalar1=rs_[:, KA + c:KA + c + 1], scalar2=None, op0=ALU.mult), ["rws", "r_tA"], ["r_tA"])
                    sc.dve(lambda e, c=c: e.scalar_tensor_tensor(out=kmod[c][:], in0=tA[:], scalar=1.0, in1=kraw[c][:], op0=ALU.add, op1=ALU.mult),
                           ["r_tA", f"r_kraw{c}"], ["r_kmod"])
                    sc.dve(lambda e, c=c: e.tensor_tensor(out=bvec[c][:], in0=kk[c][:], in1=af[c][:], op=ALU.mult), ["r_kk", "r_af"], ["r_bvec"])
                    sc.dve(lambda e, c=c: e.scalar_tensor_tensor(out=sqt[:], in0=rf[c][:], scalar=rs_[:, RK + c:RK + c + 1], in1=kmod[c][:], op0=ALU.mult, op1=ALU.mult),
                           [f"r_rf{c}", "rws", "r_kmod", "r_sq"], ["r_sq"])
                    p5, p5tok = self.psum()
                    sc.pe(lambda e, p5=p5: e.matmul(p5[:], lhsT=self.onesbd_bf[:], rhs=sqt[:], start=True, stop=True), ["r_sq", "onesbd_bf"], [p5tok])
                    sc.dve(lambda e, p5=p5, c=c: e.tensor_tensor(out=bonus[c][:], in0=p5[:], in1=vf[c][:], op=ALU.mult), [p5tok, f"r_vf{c}"], [f"r_bonus{c}"])
                    sc.dve(lambda e, c=c: e.tensor_tensor(out=rbar[c][:], in0=rf[c][:], in1=epos[c][:], op=ALU.mult), [f"r_rf{c}", f"r_epos{c}"], [f"r_rbar{c}"])
                    sc.dve(lambda e, c=c: e.tensor_tensor(out=btb[c][:], in0=bvec[c][:], in1=eneg[c][:], op=ALU.mult), ["r_bvec", "r_eneg"], [f"r_btb{c}"])
                    sc.dve(lambda e, c=c: e.tensor_tensor(out=ktb[c][:], in0=kmod[c][:], in1=eneg[c][:], op=ALU.mult), ["r_kmod", "r_eneg"], [f"r_ktb{c}"])
                    for hp in range(2):
                        hr = slice(hp * 64, hp * 64 + 64)
                        sc.act(lambda e, c=c, hp=hp, hr=hr: e.copy(out=btz[c][hp][hr, :], in_=btb[c][hr, :]), [f"r_btb{c}"], [f"r_btz{c}"])
                        sc.act(lambda e, c=c, hp=hp, hr=hr: e.copy(out=ktz[c][hp][hr, :], in_=ktb[c][hr, :]), [f"r_ktb{c}"], [f"r_ktz{c}"])
                    for j in range(8):
                        js = slice(j * 64, (j + 1) * 64)
                        eng = sc.dve
                        eng(lambda e, c=c, j=j, js=js: e.tensor_scalar(out=tA[:, js], in0=eneg[c][:, js], scalar1=epos[c][:, j * 64 + 63:j * 64 + 64], scalar2=None,
                                                                        op0=ALU.mult), ["r_eneg", f"r_epos{c}", "r_tA"], ["r_tA"])
                    sc.dve(lambda e, c=c: e.tensor_tensor(out=Bh32[c][:], in0=bvec[c][:], in1=tA[:], op=ALU.mult), ["r_bvec", "r_tA"], [f"r_Bh32{c}"])
                    sc.dve(lambda e, c=c: e.tensor_tensor(out=Kh32[c][:], in0=kmod[c][:], in1=tA[:], op=ALU.mult), ["r_kmod", "r_tA"], [f"r_Kh32{c}"])
                for n in range(4 if self.stage >= 3 else 0):
                    pb = nblk % 2
                    nblk += 1
                    b = B[pb]
                    tk = lambda nm, pb=pb: f"r_{nm}{pb}"
                    cs = slice(n * 128, (n + 1) * 128)
                    sc.cap_begin(); self.ps_lo, self.ps_n = 0, 4
                    for rc in range(3):
                        pk, tkk = self.psum()
                        for q_, (src, stok) in enumerate([(ap32, "r_ap32"), (vf, "r_vf"), (Bh32, "r_Bh32"), (Kh32, "r_Kh32")]):
                            sc.pe(lambda e, pk=pk, q_=q_, src=src, rc=rc, cs=cs: e.matmul(pk[:, q_ * 128:(q_ + 1) * 128], lhsT=src[rc][:, cs], rhs=ident, start=True, stop=True),
                                  [f"{stok}{rc}", "C"], [tkk])
                        pc = slice(rc * 128, (rc + 1) * 128)
                        sc.act(lambda e, pk=pk, pc=pc, b=b: e.copy(out=b["Vtm"][:, pc], in_=pk[:, 128:256]), [tkk], [(tk("Vtm"), rc)])
                        for hp in range(2):
                            h = 2 * rc + hp
                            zc = slice(h * 128 + hp * 64, h * 128 + hp * 64 + 64)
                            hs = slice(h * 64, (h + 1) * 64)
                            sc.act(lambda e, pk=pk, zc=zc, hp=hp, b=b: e.copy(out=b["Kez"][:, zc], in_=pk[:, hp * 64:hp * 64 + 64]), [tkk], [(tk("Kez"), h)])
                            sc.act(lambda e, pk=pk, zc=zc, hp=hp, b=b: e.copy(out=b["Vz"][:, zc], in_=pk[:, 128 + hp * 64:128 + hp * 64 + 64]), [tkk], [(tk("Vz"), h)])
                        sc.act(lambda e, pk=pk, b=b: e.copy(out=b["BKtm"][:], in_=pk[:, 256:512]), [tkk], [tk("BKtm")])
                        for hp in range(2):
                            h = 2 * rc + hp
                            hs = slice(h * 64, (h + 1) * 64)
                            for c in range(2):
                                ind = C[:, 256 + 64 * c:257 + 64 * c]
                                sc.dve(lambda e, hs=hs, hp=hp, c=c, ind=ind, b=b: e.tensor_scalar(
                                    out=b[f"Bhc{c}"][:, hs], in0=b["BKtm"][:, hp * 64:hp * 64 + 64], scalar1=ind, scalar2=None, op0=ALU.mult),
                                    [tk("BKtm"), "C"], [(tk(f"Bhc{c}"), h)])
                                sc.dve(lambda e, hs=hs, hp=hp, c=c, ind=ind, b=b: e.tensor_scalar(
                                    out=b[f"Khc{c}"][:, hs], in0=b["BKtm"][:, 128 + hp * 64:128 + hp * 64 + 64], scalar1=ind, scalar2=None, op0=ALU.mult),
                                    [tk("BKtm"), "C"], [(tk(f"Khc{c}"), h)])
                    banks = [[self.psum(), self.psum()] for _ in range(2)]
                    def dst(k, h):
                        (pa, ta), (pb_, tb) = banks[k]
                        return (pa[:, h * 128:(h + 1) * 128], ta) if h < 4 else (pb_[:, (h - 4) * 128:(h - 3) * 128], tb)
                    for h in range(6):
                        rc, hp = h // 2, h % 2
                        o1, to1 = dst(0, h)
                        sc.pe(lambda e, o1=o1, rc=rc, hp=hp, cs=cs: e.matmul(o1, lhsT=btz[rc][hp][:, cs], rhs=apb[rc][:, cs], start=True, stop=True),
                              [f"r_btz{rc}", f"r_apb{rc}"], [to1])
                        o2, to2 = dst(1, h)
                        sc.pe(lambda e, o2=o2, rc=rc, hp=hp, cs=cs: e.matmul(o2, lhsT=ktz[rc][hp][:, cs], rhs=apb[rc][:, cs], start=True, stop=True),
                              [f"r_ktz{rc}", f"r_apb{rc}"], [to2])
                    for h in range(6):
                        hc = slice(h * 128, (h + 1) * 128)
                        o1, to1 = dst(0, h)
                        sc.dve(lambda e, b=b, hc=hc, o1=o1: e.tensor_tensor(out=b["Mm"][:, hc], in0=o1, in1=nmstrict, op=ALU.mult),
                               [to1, "C"], [(tk("Mm"), h)])
                        o2, to2 = dst(1, h)
                        sc.dve(lambda e, b=b, hc=hc, o2=o2: e.tensor_tensor(out=b["AakT"][:, hc], in0=o2, in1=nmstrict, op=ALU.mult),
                               [to2, "C"], [(tk("AakT"), h)])
                    banks = [[self.psum(), self.psum()] for _ in range(2)]
                    for h in range(6):
                        rc, hp = h // 2, h % 2
                        o1, to1 = dst(0, h)
                        sc.pe(lambda e, o1=o1, rc=rc, hp=hp, cs=cs: e.matmul(o1, lhsT=btz[rc][hp][:, cs], rhs=rbar[rc][:, cs], start=True, stop=True),
                              [f"r_btz{rc}", f"r_rbar{rc}"], [to1])
                        o2, to2 = dst(1, h)
                        sc.pe(lambda e, o2=o2, rc=rc, hp=hp, cs=cs: e.matmul(o2, lhsT=ktz[rc][hp][:, cs], rhs=rbar[rc][:, cs], start=True, stop=True),
                              [f"r_ktz{rc}", f"r_rbar{rc}"], [to2])
                    for h in range(6):
                        hc = slice(h * 128, (h + 1) * 128)
                        o1, to1 = dst(0, h)
                        sc.dve(lambda e, b=b, hc=hc, o1=o1: e.tensor_tensor(out=b["AqT"][:, hc], in0=o1, in1=mincl, op=ALU.mult), [to1, "C"], [(tk("AqT"), h)])
                        o2, to2 = dst(1, h)
                        sc.dve(lambda e, b=b, hc=hc, o2=o2: e.tensor_tensor(out=b["AqkT"][:, hc], in0=o2, in1=mincl, op=ALU.mult), [to2, "C"], [(tk("AqkT"), h)])
                    self.neumann(b, tk, ident, ident6)
                    sc.act(lambda e, b=b: e.copy(out=b["Rbf"][:], in_=b["R"][:]), [tk("R")], [tk("Rbf")])
                    pW, tW = self.psum()
                    pX, tX = self.psum()
                    for h in range(6):
                        rc, hp = h // 2, h % 2
                        hs = slice(h * 64, (h + 1) * 64); hc = slice(h * 128, (h + 1) * 128)
                        sc.pe(lambda e, pW=pW, b=b, rc=rc, hp=hp, hc=hc: e.matmul(
                            pW[:, rc * 128:(rc + 1) * 128], lhsT=b["Kez"][:, hc], rhs=b["Rbf"][:, hc], start=(hp == 0), stop=(hp == 1)),
                            [(tk("Kez"), h), tk("Rbf")], [tW])
                        sc.pe(lambda e, pX=pX, b=b, hs=hs, hc=hc: e.matmul(pX[:, hs], lhsT=b["AakT"][:, hc], rhs=b["Vtm"][:, hs], start=True, stop=True),
                              [(tk("AakT"), h), (tk("Vtm"), h // 2)], [tX])
                    sc.act(lambda e, pW=pW, b=b: e.mul(out=b["WtT"][:], in_=pW[:, 0:384], mul=-1.0), [tW], [tk("WtT")])
                    sc.act(lambda e, pX=pX, b=b: e.copy(out=b["Xb"][:], in_=pX[:, 0:384]), [tX], [tk("Xb")])
                    pU, tU = self.psum()
                    for h in range(6):
                        hs = slice(h * 64, (h + 1) * 64); hc = slice(h * 128, (h + 1) * 128)
                        sc.pe(lambda e, pU=pU, b=b, hs=hs, hc=hc: e.matmul(pU[:, hs], lhsT=b["Rbf"][:, hc], rhs=b["Xb"][:, hs], start=True, stop=True),
                              [tk("Rbf"), tk("Xb")], [tU])
                    sc.act(lambda e, pU=pU, b=b: e.copy(out=b["Utb"][:], in_=pU[:, 0:384]), [tU], [tk("Utb")])
                    for rc in range(3):
                        sc.act(lambda e, b=b, rc=rc, cs=cs: e.copy(out=b["qbar"][:, rc * 128:(rc + 1) * 128], in_=rbar[rc][:, cs]), [f"r_rbar{rc}"], [tk("qbar")])
                    gl_ap = lambda h, c, n=n: epos[h // 2][(h % 2) * 64:(h % 2) * 64 + 64, n * 128 + c * 64 + 63:n * 128 + c * 64 + 64]
                    pre_ops = sc.cap_end()
                    sc.cap_begin(); self.ps_lo, self.ps_n = 4, 1
                    self.chunk_loop(b, tk, n, psO, H32, Hbd, "r", gl_ap, [f"r_epos{i}" for i in range(3)], beta=None, extra=True)
                    loop_ops = sc.cap_end()
                    self.ps_lo, self.ps_n = 0, 5
                    sc.replay(pend_loop, pre_ops)
                    pend_loop = loop_ops
                    if n == 3:
                        sc.replay(pend_loop)
                        pend_loop = []
                for rc in range(3 if self.stage >= 6 else 0):
                    po, pot = psO[rc]
                    sc.act(lambda e, po=po: e.copy(out=tA[:], in_=po[:]), [pot, "r_tA"], ["r_tA"])
                    p, ptok = self.psum()
                    sc.pe(lambda e, p=p: e.matmul(p[:], lhsT=onesbd, rhs=tA[:], start=True, stop=True), ["r_tA", "C"], [ptok])
                    sc.dve(lambda e, p=p: e.scalar_tensor_tensor(out=tA[:], in0=p[:], scalar=-1.0 / 64, in1=tA[:], op0=ALU.mult, op1=ALU.add), [ptok, "r_tA"], ["r_tA"])
                    sc.act(lambda e: e.activation(out=tB[:], in_=tA[:], func=AF.Square), ["r_tA", "r_tB"], ["r_tB"])
                    p2, p2tok = self.psum()
                    sc.pe(lambda e, p2=p2: e.matmul(p2[:], lhsT=onesbd, rhs=tB[:], start=True, stop=True), ["r_tB", "C"], [p2tok])
                    sc.act(lambda e, p2=p2: e.activation(out=tB[:], in_=p2[:], func=AF.Sqrt, bias=64e-5, scale=1.0 / 64), [p2tok, "r_tB"], ["r_tB"])
                    sc.dve(lambda e: e.reciprocal(out=tB[:], in_=tB[:]), ["r_tB"], ["r_tB"])
                    sc.dve(lambda e: e.tensor_tensor(out=tA[:], in0=tA[:], in1=tB[:], op=ALU.mult), ["r_tA", "r_tB"], ["r_tA"])
                    sc.dve(lambda e, rc=rc: e.tensor_scalar(out=tA[:], in0=tA[:], scalar1=rs_[:, GG + rc:GG + rc + 1], scalar2=None, op0=ALU.mult), ["r_tA", "rws"], ["r_tA"])
                    sc.dve(lambda e, rc=rc: e.tensor_scalar(out=tA[:], in0=tA[:], scalar1=rs_[:, GB + rc:GB + rc + 1], scalar2=None, op0=ALU.add), ["r_tA", "rws"], ["r_tA"])
                    sc.dve(lambda e, rc=rc: e.tensor_tensor(out=tA[:], in0=tA[:], in1=bonus[rc][:], op=ALU.add), ["r_tA", f"r_bonus{rc}"], ["r_tA"])
                    sc.dve(lambda e, rc=rc: e.tensor_tensor(out=sqt[:], in0=tA[:], in1=gf_[rc][:], op=ALU.mult), ["r_tA", f"r_gf{rc}", "r_sq"], ["r_sq"])
                    sc.dma(self.yT[rc * 128:(rc + 1) * 128, t0:t0 + T], sqt[:], reads=["r_sq"], writes=[("yT", rc, sbi)], q="pool")
            sc.flush()
        self.ps_lo, self.ps_n = 0, 8

    def chunk_loop(self, b, tk, n, psO, H32, Hbd, pf, gl_ap, gl_toks, beta=None, extra=False):
        sc = self.sc
        for c in range(2):
            tr = slice(c * 64, c * 64 + 64)
            pSU, tSU = self.psum()
            for rc in range(3):
                pc = slice(rc * 128, (rc + 1) * 128)
                sc.pe(lambda e, pSU=pSU, pc=pc: e.matmul(pSU[:, pc], lhsT=b["WtT"][:, pc], rhs=Hbd[:, pc], start=True, stop=True),
                      [tk("WtT"), (f"{pf}_Hbd", 2 * rc), (f"{pf}_Hbd", 2 * rc + 1)], [tSU])
            for h in range(6):
                rc, hp = h // 2, h % 2
                hs = slice(h * 64, (h + 1) * 64)
                src = pSU[tr, rc * 128 + hp * 64:rc * 128 + hp * 64 + 64]
                dst = b["Ubz"][tr, h * 128 + hp * 64:h * 128 + hp * 64 + 64]
                if beta is not None:
                    sc.dve(lambda e, src=src, dst=dst, h=h, hs=hs, tr=tr: e.scalar_tensor_tensor(
                        out=dst, in0=src, scalar=beta[tr, n, h:h + 1], in1=b["Utb"][tr, hs], op0=ALU.mult, op1=ALU.add),
                        [tSU, f"{pf}_beta", tk("Utb")], [(tk("Ubz"), h)])
                else:
                    sc.dve(lambda e, src=src, dst=dst, hs=hs, tr=tr: e.tensor_tensor(out=dst, in0=src, in1=b["Utb"][tr, hs], op=ALU.add),
                           [tSU, tk("Utb")], [(tk("Ubz"), h)])
            pSH, tSH = self.psum()
            for rc in range(3):
                pc = slice(rc * 128, (rc + 1) * 128)
                po, pot = psO[rc]
                oc = slice(n * 128 + c * 64, n * 128 + c * 64 + 64)
                qc = slice(rc * 128 + c * 64, rc * 128 + c * 64 + 64)
                hbt = [(f"{pf}_Hbd", 2 * rc), (f"{pf}_Hbd", 2 * rc + 1)]
                sc.pe(lambda e, po=po, pc=pc, oc=oc, qc=qc: e.matmul(po[:, oc], lhsT=Hbd[:, pc], rhs=b["qbar"][:, qc], start=True, stop=False),
                      hbt + [tk("qbar")], [pot])
                for hp in range(2):
                    h = 2 * rc + hp
                    hc = slice(h * 128, (h + 1) * 128)
                    ac = slice(h * 128 + c * 64, h * 128 + c * 64 + 64)
                    last = (hp == 1) and not extra
                    sc.pe(lambda e, po=po, hc=hc, oc=oc, ac=ac, last=last: e.matmul(po[:, oc], lhsT=b["Ubz"][:, hc], rhs=b["AqT"][:, ac], start=False, stop=last),
                          [(tk("Ubz"), h), (tk("AqT"), h)], [pot])
                    if extra:
                        sc.pe(lambda e, po=po, hc=hc, oc=oc, ac=ac, hp=hp: e.matmul(po[:, oc], lhsT=b["Vz"][:, hc], rhs=b["AqkT"][:, ac], start=False, stop=(hp == 1)),
                              [(tk("Vz"), h), (tk("AqkT"), h)], [pot])
                nmm = 4 if extra else 2
                k = 0
                for hp in range(2):
                    h = 2 * rc + hp
                    hc = slice(h * 128, (h + 1) * 128)
                    sc.pe(lambda e, pSH=pSH, pc=pc, hc=hc, c=c, k=k, nmm=nmm: e.matmul(pSH[:, pc], lhsT=b[f"Bhc{c}"][:, pc], rhs=b["Ubz"][:, hc], start=(k == 0), stop=(k == nmm - 1)),
                          [(tk(f"Bhc{c}"), h), (tk(f"Bhc{c}"), h ^ 1), (tk("Ubz"), h)], [tSH])
                    k += 1
                    if extra:
                        sc.pe(lambda e, pSH=pSH, pc=pc, hc=hc, c=c, k=k, nmm=nmm: e.matmul(pSH[:, pc], lhsT=b[f"Khc{c}"][:, pc], rhs=b["Vz"][:, hc], start=False, stop=(k == nmm - 1)),
                              [(tk(f"Khc{c}"), h), (tk(f"Khc{c}"), h ^ 1), (tk("Vz"), h)], [tSH])
                        k += 1
            for h in range(6):
                rc, hp = h // 2, h % 2
                rows = slice(hp * 64, hp * 64 + 64)
                cols = slice(rc * 128 + hp * 64, rc * 128 + hp * 64 + 64)
                gl = gl_ap(h, c)
                sc.dve(lambda e, pSH=pSH, rows=rows, cols=cols, gl=gl: e.scalar_tensor_tensor(
                    out=H32[rows, cols], in0=H32[rows, cols], scalar=gl, in1=pSH[rows, cols], op0=ALU.mult, op1=ALU.add),
                    [tSH, (f"{pf}_H32", h)] + list(gl_toks), [(f"{pf}_H32", h)])
                sc.act(lambda e, rows=rows, cols=cols: e.copy(out=Hbd[rows, cols], in_=H32[rows, cols]), [(f"{pf}_H32", h)], [(f"{pf}_Hbd", h)])

    def neumann(self, b, tk, ident, ident6):
        sc = self.sc
        allM = [(tk("Mm"), h) for h in range(6)]
        pa, ta = self.psum(); pb_, tb = self.psum()
        for h in range(6):
            o = pa[:, h * 128:(h + 1) * 128] if h < 4 else pb_[:, (h - 4) * 128:(h - 3) * 128]
            sc.pe(lambda e, o=o, h=h: e.matmul(o, lhsT=b["Mm"][:, h * 128:(h + 1) * 128], rhs=ident, start=True, stop=True), [(tk("Mm"), h), "C"], [ta if h < 4 else tb])
        sc.act(lambda e, pa=pa: e.copy(out=b["Mt"][:, 0:512], in_=pa[:, 0:512]), [ta], [tk("Mt")])
        sc.act(lambda e, pb_=pb_: e.copy(out=b["Mt"][:, 512:768], in_=pb_[:, 0:256]), [tb], [tk("Mt")])
        sc.dve(lambda e: e.tensor_tensor(out=b["R"][:], in0=b["Mm"][:], in1=ident6, op=ALU.add), allM + ["C"], [tk("R")])
        P, Pt, Ptok, Pttok = b["Mm"], b["Mt"], allM, [tk("Mt")]
        for lev in range(5):
            last = lev == 4
            pa, ta = self.psum(); pb_, tb = self.psum()
            for h in range(6):
                hc = slice(h * 128, (h + 1) * 128)
                o = pa[:, hc] if h < 4 else pb_[:, (h - 4) * 128:(h - 3) * 128]
                sc.pe(lambda e, o=o, hc=hc, P=P, Pt=Pt: e.matmul(o, lhsT=P[:, hc], rhs=Pt[:, hc], start=True, stop=True),
                      list(Ptok) + list(Pttok), [ta if h < 4 else tb])
            n2t = b["Pta"] if lev % 2 == 0 else b["Mt"]
            n2ttok = tk("Pta") if lev % 2 == 0 else tk("Mt")
            sc.act(lambda e, pa=pa, n2t=n2t: e.copy(out=n2t[:, 0:512], in_=pa[:, 0:512]), [ta], [n2ttok])
            sc.act(lambda e, pb_=pb_, n2t=n2t: e.copy(out=n2t[:, 512:768], in_=pb_[:, 0:256]), [tb], [n2ttok])
            if not last:
                pc, tc = self.psum(); pd, td = self.psum()
                for h in range(6):
                    hc = slice(h * 128, (h + 1) * 128)
                    o = pc[:, hc] if h < 4 else pd[:, (h - 4) * 128:(h - 3) * 128]
                    sc.pe(lambda e, o=o, hc=hc, P=P, Pt=Pt: e.matmul(o, lhsT=Pt[:, hc], rhs=P[:, hc], start=True, stop=True),
                          list(Ptok) + list(Pttok), [tc if h < 4 else td])
                n2 = b["Pa"] if lev % 2 == 0 else b["Mm"]
                n2tok = tk("Pa") if lev % 2 == 0 else tk("Mm_all")
            pe_, te = self.psum(); pf, tf = self.psum()
            for h in range(6):
                hc = slice(h * 128, (h + 1) * 128)
                o = pe_[:, hc] if h < 4 else pf[:, (h - 4) * 128:(h - 3) * 128]
                sc.pe(lambda e, o=o, hc=hc, n2t=n2t: e.matmul(o, lhsT=n2t[:, hc], rhs=b["R"][:, hc], start=True, stop=True),
                      [n2ttok, tk("R")], [te if h < 4 else tf])
            if not last:
                sc.act(lambda e, pc=pc, n2=n2: e.copy(out=n2[:, 0:512], in_=pc[:, 0:512]), [tc] + (list(Ptok) if n2 is P else []), [n2tok] + (list(Ptok) if n2 is P else []))
                sc.act(lambda e, pd=pd, n2=n2: e.copy(out=n2[:, 512:768], in_=pd[:, 0:256]), [td], [n2tok] + (list(Ptok) if n2 is P else []))
            sc.dve(lambda e, pe_=pe_: e.tensor_tensor(out=b["R"][:, 0:512], in0=b["R"][:, 0:512], in1=pe_[:, 0:512], op=ALU.add), [te, tk("R")], [tk("R")])
            sc.dve(lambda e, pf=pf: e.tensor_tensor(out=b["R"][:, 512:768], in0=b["R"][:, 512:768], in1=pf[:, 0:256], op=ALU.add), [tf, tk("R")], [tk("R")])
            if not last:
                P, Pt = n2, n2t
                Ptok, Pttok = [n2tok], [n2ttok]


    def build(self, mixers=True):
        self.consts()
        h_src = self.xT
        for l in range(self.L):
            self.phase_a(l, h_src)
            if "pT" in self.dbg:
                self.sc.dma(self.dbg_out["pT"], self.pT, reads=[], writes=[])
                self.sc.flush()
            self.phase_b(l)
            if "yT" in self.dbg:
                self.sc.dma(self.dbg_out["yT"], self.yT, reads=[], writes=[])
                self.sc.flush()
            h_dst = self.oT if l == self.L - 1 else self.hT[l % 2]
            self.phase_c(l, h_src, h_dst)
            h_src = h_dst
        return self.nc

    def phase_b(self, l):
        if len(self.mix) < 3:
            self.phase_stub(l)
        if "gdn" in self.mix:
            self.phase_gdn(l)
        if "rwkv" in self.mix:
            self.phase_rwkv(l)
        if "mla" in self.mix:
            if l == 0:
                self.mla_tables()
            self.phase_mla(l)

    def phase_stub(self, l):
        nc, sc, S = self.nc, self.sc, self.S
        with contextlib.ExitStack() as st:
            z = st.enter_context(nc.sbuf_tensor(f"stZ_L{l}", [128, 8, 512], BF16))
            sc.pool(lambda e: e.memset(z[:], 0.0), [], ["stZ"])
            for t in range(S // 512):
                t0 = t * 512
                sc.dma(self.yT[:, t0:t0 + 512].rearrange("(k p) s -> p k s", p=128), z[:], reads=["stZ"])
            sc.flush()


def _consts():
    C = np.zeros((128, NCST), np.float32)
    i = np.arange(128)
    same = (i[:, None] // 64) == (i[None, :] // 64)
    incl = same & (i[:, None] <= i[None, :])
    strict = same & (i[:, None] < i[None, :])
    C[:, 0:128] = np.eye(128)
    C[:, 128:256] = 1.0
    C[:, 256:384] = same
    C[:, 384:512] = -1.0 * strict
    C[:, 512:640] = np.where(incl, 0.0, -30000.0)
    C[:, 640:768] = 1.0 - np.eye(128)
    C[:, 768:896] = strict
    C[:, 896:1024] = incl
    C[:, 1024:1792] = np.tile(1.0 - np.eye(128), (1, 6))
    C[:, 1792:2560] = np.tile(np.eye(128), (1, 6))
    return C


def rwkv_host(mu, w0, w2, a0, a2, g2, k_k, k_a, r_k, gn_g, gn_b):
    L = mu.shape[0]
    c3 = lambda v: v.reshape(L, 3, 128).transpose(2, 0, 1)
    rws = np.zeros((128, L, 32), np.float32)
    rws[:, :, 0:11] = mu.reshape(L, 11, 128).transpose(2, 0, 1)
    for off, v in ((11, w0), (14, a0), (17, k_k), (20, k_a), (23, r_k.reshape(L, 384)), (26, gn_g), (29, gn_b)):
        rws[:, :, off:off + 3] = c3(v)
    rww = np.zeros((128, L, 1152), np.float32)
    rww[0:64, :, 0:384] = w2.transpose(1, 0, 2)
    rww[64:128, :, 384:768] = a2.transpose(1, 0, 2)
    rww[:, :, 768:1152] = g2.transpose(1, 0, 2)
    segm = np.ones((128, 512), np.float32)
    segm[:, ::64] = 0.0
    return {"rws": np.ascontiguousarray(rws.reshape(128, L * 32)), "rww": np.ascontiguousarray(rww.reshape(128, L * 1152)), "segm": segm}


def mla_host(q_norm_g, kv_norm_g, q_qk_g, k_qk_g, positions_row, S):
    L = q_norm_g.shape[0]
    mls = np.zeros((128, L, 8), np.float32)
    mls[:, :, 0:2] = q_norm_g.reshape(L, 2, 128).transpose(2, 0, 1)
    mls[:, :, 2] = kv_norm_g.T
    perm = np.concatenate([np.arange(64), 64 + (np.arange(32) + 16) % 32])
    mls[0:96, :, 3] = q_qk_g.T
    mls[0:96, :, 4] = q_qk_g[:, perm].T
    mls[0:96, :, 5] = k_qk_g.T
    mls[0:96, :, 6] = k_qk_g[:, perm].T
    mlc = np.zeros((128, 104), np.float32)
    inv_freq = (10000.0 ** (-np.arange(0, 32, 2, dtype=np.float32) / 32)).astype(np.float32)
    mlc[64:96, 0] = np.tile(inv_freq, 2)
    mlc[0:32, 8 + 64:8 + 96] = np.eye(32, dtype=np.float32)
    kk = np.arange(128)[:, None]
    qo = np.arange(512)[None, :]
    mm = np.concatenate([((2 * j + (kk >= 64)) <= (qo // 64)).astype(np.float32) for j in range(4)], axis=1)
    pos96 = np.ascontiguousarray(np.broadcast_to(positions_row.astype(np.int32)[None, :], (96, S)))
    return {"mls": np.ascontiguousarray(mls.reshape(128, L * 8)), "mlc": mlc, "mmask": np.ascontiguousarray(mm), "pos96": pos96}


def kernel(**inputs):
    x = np.asarray(inputs["x"], np.float32)
    B, S, _ = x.shape
    L = int(inputs["w_in"].shape[0])
    f = lambda k: np.ascontiguousarray(np.asarray(inputs[k], np.float32))
    col8 = lambda g: np.ascontiguousarray(g.reshape(L, 8, 128).transpose(2, 0, 1).reshape(128, L * 8))
    conv_w, a_log, dtb, og = f("gdn_conv_w"), f("gdn_a_log"), f("gdn_dt_bias"), f("gdn_o_norm_g")
    gsm = np.zeros((128, L, 13), np.float32)
    gsm[:, :, 0:6] = dtb[None]
    gsm[:, :, 6:12] = a_log[None]
    gsm[:, :, 12] = np.tile(og, (1, 2)).T
    shared = {
        "w_in": f("w_in"), "w_out": f("w_out"), "w_ff1": f("w_ff1"), "w_ff2": f("w_ff2"),
        "g_mix": col8(f("ln_mix_g")), "g_ffn": col8(f("ln_ffn_g")), "cst": _consts(),
        "cw": np.ascontiguousarray(conv_w.reshape(L, 4, 9, 128).transpose(3, 0, 2, 1).reshape(128, L * 36)),
        "gsm": np.ascontiguousarray(gsm.reshape(128, L * 13)),
        "w_uq": f("mla_w_uq"), "w_ukv": f("mla_w_ukv"),
    }
    shared.update(rwkv_host(f("rwkv_mu"), f("rwkv_w0"), f("rwkv_w2"), f("rwkv_a0"), f("rwkv_a2"), f("rwkv_g2"), f("rwkv_k_k"),
                            f("rwkv_k_a"), f("rwkv_r_k"), f("rwkv_gn_g"), f("rwkv_gn_b")))
    positions = np.asarray(inputs["positions"]).astype(np.int32)
    prog = Prog(S, L)
    nc = prog.build()
    in_maps = []
    for b in range(B):
        m = dict(shared, xT=np.ascontiguousarray(x[b].T))
        m.update(mla_host(f("mla_q_norm_g"), f("mla_kv_norm_g"), f("mla_q_qk_g"), f("mla_k_qk_g"), positions[b], S))
        in_maps.append(m)
    res = run_bass_kernel_spmd(nc, in_maps, core_ids=list(range(B)))
    return np.stack([np.ascontiguousarray(r["oT"].T) for r in res.results], axis=0).astype(np.float32)
```
